# Optimizing a Trainium2 kernel written in Bass

```python
import math
import jax, jax.numpy as jnp
from jax import lax
import numpy as np

D_MODEL = 1024
BATCH = 16
SEQ = 4096
DEPTH = 1

HEAD_DIM = 64
FOX_HEADS = 8
FOX_WIDTH = FOX_HEADS * HEAD_DIM
DIFF_HEADS = 4
DIFF_QK_WIDTH = DIFF_HEADS * 2 * HEAD_DIM
DIFF_V_DIM = 2 * HEAD_DIM
DIFF_V_WIDTH = DIFF_HEADS * DIFF_V_DIM
IN_WIDTH = 3 * FOX_WIDTH + FOX_HEADS + 2 * DIFF_QK_WIDTH + DIFF_V_WIDTH + 2 * D_MODEL
MEM_LEN = 256
CROSS_HEADS = 4
CROSS_WIDTH = CROSS_HEADS * HEAD_DIM
N_GROUPS = 4
EXPERTS_PER_GROUP = 8
N_EXPERTS = N_GROUPS * EXPERTS_PER_GROUP
TOP_K = 2
D_FF_EXPERT = 512
MOE_BLOCK = 256
Q_BLOCK = 128
ROPE_THETA = 10000.0
NORM_EPS = 1e-6
SUBLN_EPS = 1e-5
FGATE_BIAS_INIT = 3.0

kernel_name = 'hybrid_fox_diffattn_hmoe_block'


def _rms_norm(t, g, eps=NORM_EPS):
    tf = t.astype(jnp.float32)
    tf = tf * lax.rsqrt(jnp.mean(tf * tf, axis=-1, keepdims=True) + eps)
    return (tf * g.astype(jnp.float32)).astype(t.dtype)


def _split_heads(t, n):
    b, s, w = t.shape
    return t.reshape(b, s, n, w // n).transpose(0, 2, 1, 3)


def _merge_heads(t):
    b, h, s, d = t.shape
    return t.transpose(0, 2, 1, 3).reshape(b, s, h * d)


def _in_offsets():
    sizes = (FOX_WIDTH, FOX_WIDTH, FOX_WIDTH, FOX_HEADS, DIFF_QK_WIDTH, DIFF_QK_WIDTH,
             DIFF_V_WIDTH, D_MODEL, D_MODEL)
    return [int(o) for o in np.cumsum(sizes)[:-1]]


def _rope_tables(positions, dtype):
    half = HEAD_DIM // 2
    inv_freq = ROPE_THETA ** (-jnp.arange(half, dtype=jnp.float32) * 2.0 / HEAD_DIM)
    ang = positions.astype(jnp.float32)[..., None] * inv_freq
    cos = jnp.cos(ang)[:, :, None, None, :].astype(dtype)
    sin = jnp.sin(ang)[:, :, None, None, :].astype(dtype)
    return cos, sin


def _rope(t, cos, sin):
    half = t.shape[-1] // 2
    t1, t2 = t[..., :half], t[..., half:]
    return jnp.concatenate([t1 * cos - t2 * sin, t2 * cos + t1 * sin], axis=-1)


def _causal_mask(q0, q1):
    qpos = q0 + jnp.arange(q1 - q0)
    kpos = jnp.arange(q1)
    return qpos[:, None] >= kpos[None, :]


def _fox_attention(q, k, v, log_f):
    c = jnp.cumsum(log_f, axis=-1)
    scale = HEAD_DIM ** -0.5
    n_q = q.shape[2]
    outs = []
    for blk in range(n_q // Q_BLOCK):
        q0, q1 = blk * Q_BLOCK, (blk + 1) * Q_BLOCK
        s = jnp.einsum('bhqd,bhkd->bhqk', q[:, :, q0:q1], k[:, :, :q1]).astype(jnp.float32) * scale
        s = s + c[:, :, q0:q1, None] - c[:, :, None, :q1]
        s = jnp.where(_causal_mask(q0, q1), s, -jnp.inf)
        p = jax.nn.softmax(s, axis=-1).astype(v.dtype)
        outs.append(jnp.einsum('bhqk,bhkd->bhqd', p, v[:, :, :q1]))
    return jnp.concatenate(outs, axis=2)


def _diff_attention(q1, q2, k1, k2, v, lam):
    scale = HEAD_DIM ** -0.5
    n_q = q1.shape[2]
    outs = []
    for blk in range(n_q // Q_BLOCK):
        q0, qe = blk * Q_BLOCK, (blk + 1) * Q_BLOCK
        mask = _causal_mask(q0, qe)
        s1 = jnp.einsum('bhqd,bhkd->bhqk', q1[:, :, q0:qe], k1[:, :, :qe]).astype(jnp.float32) * scale
        s2 = jnp.einsum('bhqd,bhkd->bhqk', q2[:, :, q0:qe], k2[:, :, :qe]).astype(jnp.float32) * scale
        p1 = jax.nn.softmax(jnp.where(mask, s1, -jnp.inf), axis=-1)
        p2 = jax.nn.softmax(jnp.where(mask, s2, -jnp.inf), axis=-1)
        a = (p1 - lam * p2).astype(v.dtype)
        outs.append(jnp.einsum('bhqk,bhkd->bhqd', a, v[:, :, :qe]))
    return jnp.concatenate(outs, axis=2)


def _cross_attention(hx, hm, w_q, w_kv, w_o):
    b, m = hm.shape[0], hm.shape[1]
    q = _split_heads(hx @ w_q, CROSS_HEADS)
    kv = (hm @ w_kv).reshape(b, m, 2, CROSS_HEADS, HEAD_DIM)
    k = kv[:, :, 0].transpose(0, 2, 1, 3)
    v = kv[:, :, 1].transpose(0, 2, 1, 3)
    s = jnp.einsum('bhsd,bhmd->bhsm', q, k).astype(jnp.float32) * HEAD_DIM ** -0.5
    p = jax.nn.softmax(s, axis=-1).astype(v.dtype)
    return _merge_heads(jnp.einsum('bhsm,bhmd->bhsd', p, v)) @ w_o


def _routed_experts(t, expert_ids, gate_w, w_gate, w_up, w_down):
    n_tok, d = t.shape
    n_assign = n_tok * TOP_K
    flat_e = expert_ids.reshape(n_assign).astype(jnp.int32)
    flat_tok = jnp.arange(n_assign, dtype=jnp.int32) // TOP_K
    flat_w = gate_w.reshape(n_assign)
    order = jnp.argsort(flat_e)
    e_sorted = flat_e[order]
    counts = jnp.zeros((N_EXPERTS,), jnp.int32).at[flat_e].add(1)
    padded = (counts + MOE_BLOCK - 1) // MOE_BLOCK * MOE_BLOCK
    pad_end = jnp.cumsum(padded)
    pad_start = pad_end - padded
    seg_start = jnp.cumsum(counts) - counts
    dest = pad_start[e_sorted] + jnp.arange(n_assign, dtype=jnp.int32) - seg_start[e_sorted]
    n_blocks = -(-n_assign // MOE_BLOCK) + N_EXPERTS
    n_rows = n_blocks * MOE_BLOCK
    row_tok = jnp.full((n_rows,), n_tok, jnp.int32).at[dest].set(flat_tok[order])
    row_w = jnp.zeros((n_rows,), t.dtype).at[dest].set(flat_w[order])
    block_start = jnp.arange(n_blocks, dtype=jnp.int32) * MOE_BLOCK
    block_expert = jnp.minimum(jnp.searchsorted(pad_end, block_start, side='right'), N_EXPERTS - 1)
    t_pad = jnp.concatenate([t, jnp.zeros((1, d), t.dtype)], axis=0)
    xs = t_pad[row_tok].reshape(n_blocks, MOE_BLOCK, d)

    def expert_block(args):
        xb, e = args
        hb = jax.nn.silu(xb @ w_gate[e]) * (xb @ w_up[e])
        return hb @ w_down[e]

    ys = lax.map(expert_block, (xs, block_expert)).reshape(n_rows, d)
    out = jnp.zeros((n_tok + 1, d), t.dtype).at[row_tok].add(ys * row_w[:, None])
    return out[:n_tok]


def _hier_moe(t, w_group, b_group, w_expert, b_expert, w_gate, w_up, w_down):
    n_tok = t.shape[0]
    group_logits = (t @ w_group + b_group).astype(jnp.float32)
    group_prob = jax.nn.softmax(group_logits, axis=-1)
    g_sel = jnp.argmax(group_logits, axis=-1)
    g_w = jnp.take_along_axis(group_prob, g_sel[:, None], axis=-1)
    exp_logits = (t @ w_expert + b_expert).astype(jnp.float32).reshape(n_tok, N_GROUPS, EXPERTS_PER_GROUP)
    sel_logits = jnp.take_along_axis(exp_logits, g_sel[:, None, None], axis=1)[:, 0]
    top_vals, top_idx = lax.top_k(sel_logits, TOP_K)
    weights = jax.nn.softmax(top_vals, axis=-1) * g_w
    expert_ids = g_sel[:, None].astype(jnp.int32) * EXPERTS_PER_GROUP + top_idx.astype(jnp.int32)
    return _routed_experts(t, expert_ids, weights.astype(t.dtype), w_gate, w_up, w_down)


def setup_inputs(seed: int = 0) -> dict:
    key = jax.random.key(seed)
    ks = jax.random.split(key, 32)
    f32 = jnp.float32
    L = DEPTH

    def nrm(k, shape, fan_in):
        return jax.random.normal(k, shape, f32) * fan_in ** -0.5

    def gain(k, shape):
        return 1.0 + 0.02 * jax.random.normal(k, shape, f32)

    return {
        'x': jax.random.normal(ks[0], (BATCH, SEQ, D_MODEL), f32),
        'mem': jax.random.normal(ks[1], (BATCH, MEM_LEN, D_MODEL), f32),
        'positions': jnp.broadcast_to(jnp.arange(SEQ, dtype=jnp.int32), (BATCH, SEQ)),
        'g_mix': gain(ks[2], (L, D_MODEL)),
        'w_in': nrm(ks[3], (L, D_MODEL, IN_WIDTH), D_MODEL),
        'b_fgate': FGATE_BIAS_INIT + 0.1 * jax.random.normal(ks[4], (L, FOX_HEADS), f32),
        'w_branch_a': nrm(ks[5], (L, FOX_WIDTH, D_MODEL), FOX_WIDTH),
        'w_branch_b': nrm(ks[6], (L, DIFF_V_WIDTH, D_MODEL), DIFF_V_WIDTH),
        'w_out': nrm(ks[7], (L, D_MODEL, D_MODEL), D_MODEL),
        'lambda_q1': 0.1 * jax.random.normal(ks[8], (L, HEAD_DIM), f32),
        'lambda_k1': 0.1 * jax.random.normal(ks[9], (L, HEAD_DIM), f32),
        'lambda_q2': 0.1 * jax.random.normal(ks[10], (L, HEAD_DIM), f32),
        'lambda_k2': 0.1 * jax.random.normal(ks[11], (L, HEAD_DIM), f32),
        'g_diff_sub': gain(ks[12], (L, DIFF_V_DIM)),
        'g_cross': gain(ks[13], (L, D_MODEL)),
        'g_mem': gain(ks[14], (L, D_MODEL)),
        'w_cq': nrm(ks[15], (L, D_MODEL, CROSS_WIDTH), D_MODEL),
        'w_ckv': nrm(ks[16], (L, D_MODEL, 2 * CROSS_WIDTH), D_MODEL),
        'w_co': nrm(ks[17], (L, CROSS_WIDTH, D_MODEL), CROSS_WIDTH),
        'g_ffn': gain(ks[18], (L, D_MODEL)),
        'w_group': nrm(ks[19], (L, D_MODEL, N_GROUPS), D_MODEL),
        'b_group': 0.01 * jax.random.normal(ks[20], (L, N_GROUPS), f32),
        'w_expert': nrm(ks[21], (L, D_MODEL, N_EXPERTS), D_MODEL),
        'b_expert': 0.01 * jax.random.normal(ks[22], (L, N_EXPERTS), f32),
        'w_exp_gate': nrm(ks[23], (L, N_EXPERTS, D_MODEL, D_FF_EXPERT), D_MODEL),
        'w_exp_up': nrm(ks[24], (L, N_EXPERTS, D_MODEL, D_FF_EXPERT), D_MODEL),
        'w_exp_down': nrm(ks[25], (L, N_EXPERTS, D_FF_EXPERT, D_MODEL), D_FF_EXPERT),
        'g_final': gain(ks[26], (D_MODEL,)),
    }


def reference(x, mem, positions, g_mix, w_in, b_fgate, w_branch_a, w_branch_b, w_out,
              lambda_q1, lambda_k1, lambda_q2, lambda_k2, g_diff_sub, g_cross, g_mem,
              w_cq, w_ckv, w_co, g_ffn, w_group, b_group, w_expert, b_expert,
              w_exp_gate, w_exp_up, w_exp_down, g_final):
    b, s, d = x.shape
    cos, sin = _rope_tables(positions, x.dtype)
    offsets = _in_offsets()
    for l in range(DEPTH):
        lambda_init = 0.8 - 0.6 * math.exp(-0.3 * l)
        h = _rms_norm(x, g_mix[l])
        fq, fk, fv, f_logit, dq, dk, dv, gate_a, gate_b = jnp.split(h @ w_in[l], offsets, axis=-1)
        log_f = jax.nn.log_sigmoid((f_logit + b_fgate[l]).astype(jnp.float32)).transpose(0, 2, 1)
        y_a = _fox_attention(_split_heads(fq, FOX_HEADS), _split_heads(fk, FOX_HEADS),
                             _split_heads(fv, FOX_HEADS), log_f)
        y_a = _merge_heads(y_a)
        dq = _rope(dq.reshape(b, s, DIFF_HEADS, 2, HEAD_DIM), cos, sin)
        dk = _rope(dk.reshape(b, s, DIFF_HEADS, 2, HEAD_DIM), cos, sin)
        lam = (jnp.exp(jnp.sum(lambda_q1[l].astype(jnp.float32) * lambda_k1[l].astype(jnp.float32)))
               - jnp.exp(jnp.sum(lambda_q2[l].astype(jnp.float32) * lambda_k2[l].astype(jnp.float32)))
               + lambda_init)
        y_b = _diff_attention(dq[:, :, :, 0].transpose(0, 2, 1, 3), dq[:, :, :, 1].transpose(0, 2, 1, 3),
                              dk[:, :, :, 0].transpose(0, 2, 1, 3), dk[:, :, :, 1].transpose(0, 2, 1, 3),
                              _split_heads(dv, DIFF_HEADS), lam)
        y_b = _merge_heads(_rms_norm(y_b, g_diff_sub[l], SUBLN_EPS) * (1.0 - lambda_init))
        merged = (jax.nn.sigmoid(gate_a) * (y_a @ w_branch_a[l])
                  + jax.nn.sigmoid(gate_b) * (y_b @ w_branch_b[l]))
        x = x + merged @ w_out[l]
        x = x + _cross_attention(_rms_norm(x, g_cross[l]), _rms_norm(mem, g_mem[l]),
                                 w_cq[l], w_ckv[l], w_co[l])
        hm = _rms_norm(x, g_ffn[l]).reshape(b * s, d)
        x = x + _hier_moe(hm, w_group[l], b_group[l], w_expert[l], b_expert[l],
                          w_exp_gate[l], w_exp_up[l], w_exp_down[l]).reshape(b, s, d)
    return _rms_norm(x, g_final)
```

```python
import contextlib
import math
import numpy as np
import ml_dtypes
import concourse.bass as bass
import concourse.mybir as mybir
from concourse.bass_utils import run_bass_kernel_spmd

F32 = mybir.dt.float32
BF16 = mybir.dt.bfloat16
I32 = mybir.dt.int32
ALU = mybir.AluOpType
AF = mybir.ActivationFunctionType
AX = mybir.AxisListType

NCORES = 8
SEQ = 4096
D = 1024
NSEQ = 2
NT = NSEQ * SEQ
INW = 5128
NEXP = 32
MB = 512
NB = NT * 2 // MB + NEXP
NROWS = NB * MB
C_FQ, C_FK, C_FV, C_FL, C_DQ, C_DK, C_DV, C_GA, C_GB = 0, 512, 1024, 1536, 1544, 2056, 2568, 3080, 4104
LAMBDA_INIT = 0.8 - 0.6 * math.exp(0.0)
NDS = 8
DEBUG_OUT = False
MAX_PHASE = 99


class Buf:
    def __init__(self, t):
        self.t = t
        self.w = {}
        self.pw = {}
        self.r = {}


class Sched:
    ENG = ['pe', 'dve', 'act', 'pool', 'sp']

    def __init__(self, nc, es):
        self.nc = nc
        self.es = es
        self.dma_pool = {q: [es.enter_context(nc.semaphore(f"dq_{q}_{i}")) for i in range(NDS)] for q in ('sp', 'pool')}
        self.dma_uses = {}
        for q in self.dma_pool:
            for s in self.dma_pool[q]:
                self.dma_uses[id(s)] = 0
        self.semobj = {}
        for q in self.dma_pool:
            for s in self.dma_pool[q]:
                self.semobj[id(s)] = s
        self.dma_rr = {'sp': 0, 'pool': 0}
        self.nops = 0
        self.phase_idx = 0

    def begin_phase(self, name):
        self.phase_idx += 1
        self.esem = {}
        for e in self.ENG:
            s = self.es.enter_context(self.nc.semaphore(f"{name}_{e}"))
            self.esem[e] = s
            self.semobj[id(s)] = s
        self.cnt = {e: 0 for e in self.ENG}
        self.ops = {e: [] for e in self.ENG}
        self.seen = {e: {} for e in self.ENG}

    def _waits(self, eng, toks):
        out = []
        for (s, v) in toks:
            if v <= 0:
                continue
            if self.seen[eng].get(s, 0) >= v:
                continue
            self.seen[eng][s] = v
            out.append((s, v))
        return out

    def _deps(self, reads, writes, pwrites):
        toks = []
        for b in reads:
            toks.extend(b.w.items())
            toks.extend(b.pw.items())
        for b in writes:
            toks.extend(b.w.items())
            toks.extend(b.pw.items())
            toks.extend(b.r.items())
        for b in pwrites:
            toks.extend(b.w.items())
            toks.extend(b.r.items())
        return toks

    def _commit(self, sid, val, reads, writes, pwrites):
        for b in reads:
            b.r[sid] = val
        for b in writes:
            b.w = {sid: val}
            b.pw = {}
            b.r = {}
        for b in pwrites:
            b.pw[sid] = val

    def op(self, eng, fns, reads=(), writes=(), pwrites=()):
        own = id(self.esem[eng])
        toks = self._deps(reads, writes, pwrites)
        if eng == 'pe':
            toks = [t for t in toks if t[0] != own]
        waits = self._waits(eng, toks)
        self.cnt[eng] += 1
        if callable(fns):
            fns = [fns]
        self.ops[eng].append((waits, fns, own, 1))
        self.nops += len(fns) + len(waits)
        self._commit(own, self.cnt[eng], reads, writes, pwrites)

    def dma(self, q, fn, reads=(), writes=(), pwrites=()):
        pool = self.dma_pool[q]
        i = self.dma_rr[q]
        self.dma_rr[q] = (i + 1) % len(pool)
        s = id(pool[i])
        toks = self._deps(reads, writes, pwrites) + [(s, 16 * self.dma_uses[s])]
        waits = self._waits(q, toks)
        self.dma_uses[s] += 1
        self.ops[q].append((waits, [fn], s, 16))
        self.nops += 1 + len(waits)
        self._commit(s, 16 * self.dma_uses[s], reads, writes, pwrites)

    def end_phase(self):
        final = [(id(self.esem[e]), self.cnt[e]) for e in self.ENG]
        final += [(s, 16 * u) for s, u in self.dma_uses.items()]
        for e in self.ENG:
            waits = self._waits(e, final)
            self.ops[e].append((waits, [], None, 0))
        if self.phase_idx > MAX_PHASE:
            return
        semobj = self.semobj
        ops = self.ops

        def replay(h, lst):
            for waits, fns, sem, inc in lst:
                for (s, v) in waits:
                    h.wait_ge(semobj[s], v)
                for k, fn in enumerate(fns):
                    ins = fn(h)
                    if k == len(fns) - 1 and sem is not None:
                        ins.then_inc(semobj[sem], inc)

        with self.nc.Block() as block:
            @block.tensor
            def _(h):
                replay(h, ops['pe'])

            @block.vector
            def _(h):
                replay(h, ops['dve'])

            @block.scalar
            def _(h):
                replay(h, ops['act'])

            @block.gpsimd
            def _(h):
                replay(h, ops['pool'])

            @block.sync
            def _(h):
                replay(h, ops['sp'])


def MM(out, lhsT, rhs, start=True, stop=True):
    return lambda e: e.matmul(out, lhsT=lhsT, rhs=rhs, start=start, stop=stop)


def TR(out, in_, ident):
    return lambda e: e.transpose(out, in_, ident)


def ACTF(out, in_, func, **kw):
    return lambda e: e.activation(out=out, in_=in_, func=func, **kw)


def TT(out, in0, in1, op):
    return lambda e: e.tensor_tensor(out=out, in0=in0, in1=in1, op=op)


def TS(out, in0, s1, s2, op0, op1=None):
    if op1 is None:
        return lambda e: e.tensor_scalar(out=out, in0=in0, scalar1=s1, scalar2=None, op0=op0)
    return lambda e: e.tensor_scalar(out=out, in0=in0, scalar1=s1, scalar2=s2, op0=op0, op1=op1)


def STT(out, in0, scalar, in1, op0, op1):
    return lambda e: e.scalar_tensor_tensor(out=out, in0=in0, scalar=scalar, in1=in1, op0=op0, op1=op1)


def CP(out, in_):
    return lambda e: (e.tensor_copy(out=out, in_=in_) if hasattr(e, 'tensor_copy') else e.activation(out=out, in_=in_, func=AF.Copy))


def RCP(out, in_):
    return lambda e: e.reciprocal(out=out, in_=in_)


def MSET(ap, c):
    return lambda e: e.memset(ap, c)


def DMA(out, in_):
    return lambda e: e.dma_start(out=out, in_=in_)


def TTR(out, in0, in1, accum):
    return lambda e: e.scalar_tensor_tensor(out=out, in0=in0, scalar=1.0, in1=in1, op0=ALU.mult, op1=ALU.mult, accum_out=accum)


def build_program():
    nc = bass.Bass("TRN2", target_bir_lowering=False)
    io = {}

    def din(name, shape, dt=F32):
        io[name] = nc.dram_tensor(name, shape, dt, kind="ExternalInput").ap()

    din('x', [NT, D]); din('mem', [NSEQ * 256, D]); din('positions', [NSEQ, SEQ], I32)
    din('g_mix', [D]); din('w_in', [D, INW]); din('b_fgate', [8])
    din('w_branch_a', [512, D]); din('w_branch_b', [512, D]); din('w_out', [D, D])
    for n in ('lambda_q1', 'lambda_k1', 'lambda_q2', 'lambda_k2'):
        din(n, [64])
    din('g_diff_sub', [128]); din('g_cross', [D]); din('g_mem', [D])
    din('w_cq', [D, 256]); din('w_ckv', [D, 512]); din('w_co', [256, D]); din('g_ffn', [D])
    din('w_group', [D, 4]); din('b_group', [4]); din('w_expert', [D, 32]); din('b_expert', [32])
    din('w_exp_gate', [NEXP * 128, 4096]); din('w_exp_up', [NEXP * 128, 4096]); din('w_exp_down', [NEXP * 128, 4096])
    din('g_final', [D])
    din('c_ident_bf', [128, 128], BF16); din('c_ident_f', [128, 128]); din('c_tri', [128, 128]); din('c_mask', [128, 128], BF16)
    din('c_invf', [64, 1]); din('c_bstart', [128, NB]); din('c_piota', [128, 1])
    out = nc.dram_tensor('out', [NT, D], F32, kind="ExternalOutput").ap()

    def scr(name, shape, dt):
        return nc.dram_tensor(name, shape, dt, kind=("ExternalOutput" if DEBUG_OUT else "Internal")).ap()

    FQ = scr('FQ', [8, 70, NT], BF16); FK = scr('FK', [8, 70, NT], BF16)
    FV = scr('FV', [NT, 512], BF16); DV = scr('DV', [NT, 512], BF16)
    DQ = scr('DQ', [8, 64, NT], BF16); DK = scr('DK', [8, 64, NT], BF16)
    GA = scr('GA', [D, NT], BF16); GB = scr('GB', [D, NT], BF16)
    YA = scr('YA', [512, NT], BF16); YB = scr('YB', [512, NT], BF16)
    HM = scr('HM', [NT, D], BF16); X2 = scr('X2', [NT, D], F32)
    XS = scr('XS', [NROWS, D], BF16); YS = scr('YS', [NROWS, D], BF16)
    RT = scr('RT', [NT, 8], F32)

    with contextlib.ExitStack() as top:
        S = Sched(nc, top)

        pfx_ctr = [0]

        def mk(es):
            pfx_ctr[0] += 1
            pfx = f"ph{pfx_ctr[0]}_"

            def sb(n, shp, dt):
                return Buf(es.enter_context(nc.sbuf_tensor(pfx + n, shp, dt)))

            def ps(n, shp, dt):
                return Buf(es.enter_context(nc.psum_tensor(pfx + n, shp, dt)))
            return sb, ps

        def rms_tok(xb, gb, hb, junk, ss, rstd, eps_t):
            S.op('dve', TTR(junk.t[:, :], xb.t[:, :], xb.t[:, :], ss.t[:, 0:1]), reads=[xb], writes=[junk, ss])
            S.op('act', ACTF(rstd.t[:, 0:1], ss.t[:, 0:1], AF.Ln, scale=1.0 / D, bias=eps_t.t[:, 0:1]), reads=[ss, eps_t], writes=[rstd])
            S.op('act', ACTF(rstd.t[:, 0:1], rstd.t[:, 0:1], AF.Exp, scale=-0.5), reads=[rstd], writes=[rstd])
            S.op('dve', STT(hb.t[:, :], xb.t[:, :], rstd.t[:, 0:1], gb.t[:, :], ALU.mult, ALU.mult), reads=[xb, rstd, gb], writes=[hb])

        with contextlib.ExitStack() as es:
            sb, ps = mk(es)
            S.begin_phase('p1')
            w = [sb(f'w_in{c}', [128, INW], BF16) for c in range(8)]
            for c in range(8):
                S.dma('pool', DMA(w[c].t[:, :], io['w_in'][c * 128:(c + 1) * 128, :]), writes=[w[c]])
            gmix = sb('gmix', [128, D], F32)
            S.dma('sp', DMA(gmix.t[:, :], io['g_mix'].partition_broadcast(128)), writes=[gmix])
            bfg = sb('bfg', [128, 8], F32)
            S.dma('sp', DMA(bfg.t[:, :], io['b_fgate'].partition_broadcast(128)), writes=[bfg])
            identb = sb('identb', [128, 128], BF16)
            S.dma('sp', DMA(identb.t[:, :], io['c_ident_bf']), writes=[identb])
            identf = sb('identf', [128, 128], F32)
            S.dma('sp', DMA(identf.t[:, :], io['c_ident_f']), writes=[identf])
            invf = sb('invf', [64, 1], F32)
            S.dma('sp', DMA(invf.t[:, :], io['c_invf']), writes=[invf])
            eps_t = sb('eps_t', [128, 1], F32)
            S.op('dve', MSET(eps_t.t[:, :], 1e-6), writes=[eps_t])
            one_t = sb('one_t', [128, 1], F32)
            S.op('dve', MSET(one_t.t[:, :], 1.0), writes=[one_t])
            posi = sb('posi', [64, 512], I32)
            ang = sb('ang', [64, 512], F32)
            tk = sb('tk', [64, 512], I32)
            tf = sb('tf', [64, 512], F32)
            cosT = sb('cosT', [64, 512], F32)
            sinT = sb('sinT', [64, 512], F32)
            logfT = sb('logfT', [8, SEQ], F32)
            cc = sb('cc', [8, 1024], F32)
            carry = sb('carry', [8, 1], F32)
            onesf8 = sb('onesf8', [8, 1024], F32)
            S.op('pool', MSET(onesf8.t[:, :], 1.0), writes=[onesf8])
            onesb8 = sb('onesb8', [8, 1024], BF16)
            S.op('pool', MSET(onesb8.t[:, :], 1.0), writes=[onesb8])
            cparts = [sb(f'cp{i}', [8, 1024], BF16) for i in range(3)]
            nparts = [sb(f'np{i}', [8, 1024], BF16) for i in range(3)]
            cr = sb('cr', [8, 1024], F32)
            c8 = sb('c8', [8, 1024], F32)
            xt = [sb(f'xt{i}', [128, D], F32) for i in range(2)]
            junk = sb('junk', [128, D], BF16)
            ss = sb('ss', [128, 1], F32)
            rstd = sb('rstd', [128, 1], F32)
            hb = [sb(f'hb{i}', [128, D], BF16) for i in range(2)]
            hT = [[sb(f'hT{i}_{s}', [128, 8, 128], BF16) for s in range(4)] for i in range(2)]
            tp = [ps(f'tp{i}', [128, D], BF16) for i in range(2)]
            pt = [ps(f'pt{i}', [128, 512], F32) for i in range(2)]
            pf = [ps(f'pf{i}', [128, 512], F32) for i in range(2)]
            psm = ps('psm', [128, 512], F32)
            vsb = [sb(f'vsb{i}', [128, 512], BF16) for i in range(2)]
            lf = sb('lf', [128, 8], F32)
            lf2 = sb('lf2', [128, 8], F32)
            fqs = [sb(f'fqs{i}', [64, 8, 512], BF16) for i in range(2)]
            gsb = [sb('gsb0', [128, 8, 512], BF16)] * 2
            ra = sb('ra', [64, 512], F32)
            rb = sb('rb', [64, 512], F32)

            def sincos(dst, shift):
                S.op('dve', TS(tf.t[:, :], ang.t[:, :], 1.0 / (2 * math.pi), 0.5 + shift, ALU.mult, ALU.add), reads=[ang], writes=[tf])
                S.op('dve', CP(tk.t[:, :], tf.t[:, :]), reads=[tf], writes=[tk])
                S.op('dve', CP(dst.t[:, :], tk.t[:, :]), reads=[tk], writes=[dst])
                S.op('dve', TT(tf.t[:, :], tf.t[:, :], dst.t[:, :], ALU.subtract), reads=[tf, dst], writes=[tf])
                S.op('dve', TS(dst.t[:, :], tf.t[:, :], 0.0, None, ALU.is_lt), reads=[tf], writes=[dst])
                S.op('dve', TT(tf.t[:, :], tf.t[:, :], dst.t[:, :], ALU.add), reads=[tf, dst], writes=[tf])
                S.op('dve', TS(tf.t[:, :], tf.t[:, :], -0.5, 2 * math.pi, ALU.add, ALU.mult), reads=[tf], writes=[tf])
                S.op('dve', TS(tf.t[:, :], tf.t[:, :], -3.14159, 3.14159, ALU.max, ALU.min), reads=[tf], writes=[tf])
                S.op('act', ACTF(dst.t[:, :], tf.t[:, :], AF.Sin), reads=[tf], writes=[dst])

            it = 0
            for seq in range(NSEQ):
                for st in range(SEQ // 512):
                    T0 = seq * SEQ + st * 512
                    S.dma('sp', DMA(posi.t[:, :], io['positions'][seq, st * 512:(st + 1) * 512].partition_broadcast(64)), writes=[posi])
                    S.op('dve', CP(ang.t[:, :], posi.t[:, :]), reads=[posi], writes=[ang])
                    S.op('dve', TS(ang.t[:, :], ang.t[:, :], invf.t[:, 0:1], None, ALU.mult), reads=[ang, invf], writes=[ang])
                    sincos(sinT, 0.0)
                    sincos(cosT, 0.25)
                    hp = hT[st % 2]
                    for sub in range(4):
                        xb = xt[it % 2]; hbb = hb[it % 2]; tpp = tp[it % 2]
                        r0 = T0 + sub * 128
                        S.dma('sp', DMA(xb.t[:, :], io['x'][r0:r0 + 128, :]), writes=[xb])
                        rms_tok(xb, gmix, hbb, junk, ss, rstd, eps_t)
                        S.op('pe', [TR(tpp.t[:, c * 128:(c + 1) * 128], hbb.t[:, c * 128:(c + 1) * 128], identb.t[:, :]) for c in range(8)],
                             reads=[hbb, identb], writes=[tpp])
                        S.op('act', CP(hp[sub].t[:, :, :], tpp.t[:, :].rearrange("p (c t) -> p c t", c=8)), reads=[tpp], writes=[hp[sub]])
                        for k, (col, dst) in enumerate(((C_FV, FV), (C_DV, DV))):
                            pp = pt[k]; vb = vsb[k]
                            S.op('pe', [MM(pp.t[:, :], hp[sub].t[:, c, :], w[c].t[:, col:col + 512], c == 0, c == 7) for c in range(8)],
                                 reads=[hp[sub]] + w, writes=[pp])
                            S.op('act', CP(vb.t[:, :], pp.t[:, :]), reads=[pp], writes=[vb])
                            S.dma('pool', DMA(dst[r0:r0 + 128, :], vb.t[:, :]), reads=[vb])
                        S.op('pe', [MM(psm.t[:, 0:8], hp[sub].t[:, c, :], w[c].t[:, C_FL:C_FL + 8], c == 0, c == 7) for c in range(8)],
                             reads=[hp[sub]] + w, writes=[psm])
                        S.op('dve', TT(lf.t[:, :], psm.t[:, 0:8], bfg.t[:, :], ALU.add), reads=[psm, bfg], writes=[lf])
                        S.op('act', ACTF(lf.t[:, :], lf.t[:, :], AF.Exp, scale=-1.0), reads=[lf], writes=[lf])
                        S.op('act', ACTF(lf.t[:, :], lf.t[:, :], AF.Ln, bias=one_t.t[:, 0:1]), reads=[lf, one_t], writes=[lf])
                        S.op('dve', TS(lf2.t[:, :], lf.t[:, :], -1.0, None, ALU.mult), reads=[lf], writes=[lf2])
                        S.op('pe', TR(psm.t[0:8, 128:256], lf2.t[:, :], identf.t[:, :]), reads=[lf2, identf], writes=[psm])
                        S.op('dve', CP(logfT.t[:, st * 512 + sub * 128: st * 512 + (sub + 1) * 128], psm.t[0:8, 128:256]), reads=[psm], pwrites=[logfT])
                        it += 1
                    hall = hp
                    fi = 0
                    for k, (col, dst) in enumerate(((C_FQ, FQ), (C_FK, FK))):
                        fb = fqs[k]
                        for hh in range(8):
                            pp = pf[fi % 2]; fi += 1
                            for sub in range(4):
                                S.op('pe', [MM(pp.t[0:64, sub * 128:(sub + 1) * 128], w[c].t[:, col + hh * 64: col + (hh + 1) * 64], hall[sub].t[:, c, :], c == 0, c == 7) for c in range(8)],
                                     reads=[hall[sub]] + w, pwrites=[pp])
                            S.op('act', CP(fb.t[:, hh, :], pp.t[0:64, :]), reads=[pp], pwrites=[fb])
                        S.dma('pool', DMA(dst[:, 0:64, T0:T0 + 512].rearrange("h r t -> r h t"), fb.t[:, :, :]), reads=[fb])
                    for k, (col, dst) in enumerate(((C_DQ, DQ), (C_DK, DK))):
                        fb = fqs[k]
                        for j in range(8):
                            pp = pf[fi % 2]; fi += 1
                            for sub in range(4):
                                S.op('pe', [MM(pp.t[0:64, sub * 128:(sub + 1) * 128], w[c].t[:, col + j * 64: col + (j + 1) * 64], hall[sub].t[:, c, :], c == 0, c == 7) for c in range(8)],
                                     reads=[hall[sub]] + w, pwrites=[pp])
                            cs = slice(0, 512)
                            S.op('dve', TT(ra.t[0:32, :], pp.t[0:32, :], cosT.t[0:32, cs], ALU.mult), reads=[pp, cosT], pwrites=[ra])
                            S.op('dve', TT(rb.t[0:32, :], pp.t[32:64, :], sinT.t[0:32, cs], ALU.mult), reads=[pp, sinT], pwrites=[rb])
                            S.op('dve', TT(ra.t[32:64, :], pp.t[32:64, :], cosT.t[32:64, cs], ALU.mult), reads=[pp, cosT], pwrites=[ra])
                            S.op('dve', TT(rb.t[32:64, :], pp.t[0:32, :], sinT.t[32:64, cs], ALU.mult), reads=[pp, sinT], pwrites=[rb])
                            S.op('pool', TT(fb.t[0:32, j, :], ra.t[0:32, :], rb.t[0:32, :], ALU.subtract), reads=[ra, rb], pwrites=[fb])
                            S.op('pool', TT(fb.t[32:64, j, :], ra.t[32:64, :], rb.t[32:64, :], ALU.add), reads=[ra, rb], pwrites=[fb])
                        S.dma('pool', DMA(dst[:, :, T0:T0 + 512].rearrange("h r t -> r h t"), fb.t[:, :, :]), reads=[fb])
                    for k, (col, dst) in enumerate(((C_GA, GA), (C_GB, GB))):
                        gb_ = gsb[k]
                        for n in range(8):
                            pp = pf[fi % 2]; fi += 1
                            for sub in range(4):
                                S.op('pe', [MM(pp.t[:, sub * 128:(sub + 1) * 128], w[c].t[:, col + n * 128: col + (n + 1) * 128], hall[sub].t[:, c, :], c == 0, c == 7) for c in range(8)],
                                     reads=[hall[sub]] + w, pwrites=[pp])
                            S.op('act', ACTF(gb_.t[:, n, :], pp.t[:, :], AF.Sigmoid), reads=[pp], pwrites=[gb_])
                        S.dma('pool', DMA(dst[:, T0:T0 + 512].rearrange("(n p) t -> p n t", p=128), gb_.t[:, :, :]), reads=[gb_])
                for ch in range(4):
                    lsl = slice(ch * 1024, (ch + 1) * 1024)
                    init = 0.0 if ch == 0 else carry.t[:, 0:1]
                    S.op('dve', (lambda e, lsl=lsl, init=init: e.tensor_tensor_scan(out=cc.t[:, :], data0=onesf8.t[:, :], data1=logfT.t[:, lsl], initial=init, op0=ALU.mult, op1=ALU.add)),
                         reads=[onesf8, logfT, carry], writes=[cc])
                    S.op('dve', CP(carry.t[:, 0:1], cc.t[:, 1023:1024]), reads=[cc], writes=[carry])
                    S.op('dve', TS(c8.t[:, :], cc.t[:, :], 8.0, None, ALU.mult), reads=[cc], writes=[c8])
                    src = c8
                    for i in range(3):
                        S.op('dve', CP(cparts[i].t[:, :], src.t[:, :]), reads=[src], writes=[cparts[i]])
                        S.op('dve', TS(nparts[i].t[:, :], cparts[i].t[:, :], -1.0, None, ALU.mult), reads=[cparts[i]], writes=[nparts[i]])
                        if i < 2:
                            S.op('dve', TT(cr.t[:, :], src.t[:, :], cparts[i].t[:, :], ALU.subtract), reads=[src, cparts[i]], writes=[cr])
                            src = cr
                    sl = slice(seq * SEQ + ch * 1024, seq * SEQ + (ch + 1) * 1024)
                    for i in range(3):
                        S.dma('pool', DMA(FQ[:, 64 + i, sl], cparts[i].t[:, :]), reads=[cparts[i]])
                        S.dma('pool', DMA(FQ[:, 67 + i, sl], onesb8.t[:, :]), reads=[onesb8])
                        S.dma('pool', DMA(FK[:, 64 + i, sl], onesb8.t[:, :]), reads=[onesb8])
                        S.dma('pool', DMA(FK[:, 67 + i, sl], nparts[i].t[:, :]), reads=[nparts[i]])
            S.end_phase()

        with contextlib.ExitStack() as es:
            sb, ps = mk(es)
            S.begin_phase('p2a')
            identb = sb('identb', [128, 128], BF16)
            S.dma('sp', DMA(identb.t[:, :], io['c_ident_bf']), writes=[identb])
            maskb = sb('maskb', [128, 128], BF16)
            S.dma('sp', DMA(maskb.t[:, :], io['c_mask']), writes=[maskb])
            vaug = sb('vaug', [128, 32, 8, 128], BF16)
            S.op('pool', MSET(vaug.t[:, :, :, :], 1.0), writes=[vaug])
            qT = [sb(f'qT{i}', [70, SEQ], BF16) for i in range(2)]
            kT = [sb(f'kT{i}', [70, SEQ], BF16) for i in range(2)]
            sp_ = [ps(f'sp{i}', [128, 512], F32) for i in range(3)]
            op_ = [ps(f'op{i}', [128, 512], F32) for i in range(2)]
            pT = [sb(f'pT{i}', [128, 512], BF16) for i in range(4)]
            rec = sb('rec', [128, 512], F32)
            yab = [sb(f'yab{i}', [64, 512], BF16) for i in range(2)]
            LOOK = 2
            items = []
            n_o = 0
            heads = [(seq, h) for seq in range(NSEQ) for h in range(8)]
            for hi, (seq, h) in enumerate(heads):
                for j in range(8):
                    last = 4 * j + 3
                    for i in range(last + 1):
                        items.append(dict(hi=hi, seq=seq, h=h, j=j, i=i, last=last, ob=n_o % 2, first=(j == 0 and i == 0)))
                    n_o += 1

            def load_v(seq):
                tb = seq * SEQ
                for t in range(32):
                    S.dma('sp', DMA(vaug.t[:, t, :, 0:64], FV[tb + t * 128: tb + (t + 1) * 128, :].rearrange("s (h d) -> s h d", d=64)),
                          pwrites=[vaug])

            def load_head(hi):
                seq, h = heads[hi]
                tb = seq * SEQ
                q = qT[hi % 2]; k_ = kT[hi % 2]
                S.dma('sp', DMA(q.t[:, :], FQ[h, :, tb:tb + SEQ]), writes=[q])
                S.dma('sp', DMA(k_.t[:, :], FK[h, :, tb:tb + SEQ]), writes=[k_])

            def emit_s(n):
                it = items[n]
                if it['first']:
                    if it['hi'] == 0:
                        load_v(0)
                        load_head(0)
                    if it['hi'] + 1 < len(heads):
                        load_head(it['hi'] + 1)
                q = qT[it['hi'] % 2]; k_ = kT[it['hi'] % 2]
                i, j = it['i'], it['j']
                off = max(0, i - 4 * j) * 128
                diag = i >= 4 * j
                spb = sp_[n % len(sp_)]
                fns = [MM(spb.t[:, off:512], k_.t[0:70, i * 128:(i + 1) * 128], q.t[0:70, j * 512 + off:(j + 1) * 512], True, not diag)]
                if diag:
                    fns.append(MM(spb.t[:, off:off + 128], identb.t[:, :], maskb.t[:, :], False, True))
                S.op('pe', fns, reads=[k_, q, identb, maskb], writes=[spb])

            def emit_rest(n):
                it = items[n]
                i, j, h, seq = it['i'], it['j'], it['h'], it['seq']
                tb = seq * SEQ
                off = max(0, i - 4 * j) * 128
                spb = sp_[n % len(sp_)]; pb = pT[n % len(pT)]; ob = op_[it['ob']]
                S.op('act', ACTF(pb.t[:, off:512], spb.t[:, off:512], AF.Exp, scale=0.125), reads=[spb], writes=[pb])
                S.op('pe', MM(ob.t[:, off:512], vaug.t[:, i, h, :], pb.t[:, off:512], i == 0, i == it['last']),
                     reads=[vaug, pb], writes=[ob] if i == 0 else [], pwrites=[] if i == 0 else [ob])
                if i == it['last']:
                    yb = yab[it['ob']]
                    S.op('dve', RCP(rec.t[64:128, :], ob.t[64:128, :]), reads=[ob], writes=[rec])
                    S.op('dve', TT(yb.t[0:64, :], ob.t[0:64, :], rec.t[64:128, :], ALU.mult), reads=[ob, rec], writes=[yb])
                    S.dma('pool', DMA(YA[h * 64:(h + 1) * 64, tb + j * 512: tb + (j + 1) * 512], yb.t[:, :]), reads=[yb])
                    if j == 7 and h == 7 and it['hi'] + 1 < len(heads):
                        load_v(seq + 1)

            for n in range(min(LOOK, len(items))):
                emit_s(n)
            for n in range(len(items)):
                if n + LOOK < len(items):
                    nxt = items[n + LOOK]
                    emit_s(n + LOOK)
                emit_rest(n)
            S.end_phase()

        with contextlib.ExitStack() as es:
            sb, ps = mk(es)
            S.begin_phase('p2b')
            identb = sb('identb', [128, 128], BF16)
            S.dma('sp', DMA(identb.t[:, :], io['c_ident_bf']), writes=[identb])
            maskb = sb('maskb', [128, 128], BF16)
            S.dma('sp', DMA(maskb.t[:, :], io['c_mask']), writes=[maskb])
            onesb = sb('onesb', [128, 128], BF16)
            S.op('pool', MSET(onesb.t[:, :], 1.0), writes=[onesb])
            lqa = sb('lqa', [128, 64], F32); lka = sb('lka', [128, 64], F32); lj = sb('lj', [128, 64], F32)
            l1 = sb('l1', [128, 1], F32); l2 = sb('l2', [128, 1], F32); nlam = sb('nlam', [128, 1], F32)
            for (qa, ka, dst) in (('lambda_q1', 'lambda_k1', l1), ('lambda_q2', 'lambda_k2', l2)):
                S.dma('sp', DMA(lqa.t[:, :], io[qa].partition_broadcast(128)), writes=[lqa])
                S.dma('sp', DMA(lka.t[:, :], io[ka].partition_broadcast(128)), writes=[lka])
                S.op('dve', TTR(lj.t[:, :], lqa.t[:, :], lka.t[:, :], dst.t[:, 0:1]), reads=[lqa, lka], writes=[lj, dst])
                S.op('act', ACTF(dst.t[:, 0:1], dst.t[:, 0:1], AF.Exp), reads=[dst], writes=[dst])
            S.op('dve', TT(nlam.t[:, :], l2.t[:, :], l1.t[:, :], ALU.subtract), reads=[l1, l2], writes=[nlam])
            S.op('dve', TS(nlam.t[:, :], nlam.t[:, :], -LAMBDA_INIT, None, ALU.add), reads=[nlam], writes=[nlam])
            gsub = sb('gsub', [128, 1], F32)
            S.dma('sp', DMA(gsub.t[:, :], io['g_diff_sub'].rearrange("(p o) -> p o", o=1)), writes=[gsub])
            S.op('dve', TS(gsub.t[:, :], gsub.t[:, :], 1.0 - LAMBDA_INIT, None, ALU.mult), reads=[gsub], writes=[gsub])
            eps5 = sb('eps5', [128, 1], F32)
            S.op('dve', MSET(eps5.t[:, :], 1e-5), writes=[eps5])
            dvt = sb('dvt', [128, 32, 512], BF16)
            qk = [[sb(f'qk{i}_{m}', [64, SEQ], BF16) for m in range(4)] for i in range(2)]
            sp_ = [ps(f'sp{i}', [128, 512], F32) for i in range(3)]
            OD = [ps(f'od{i}', [128, 512], F32) for i in range(4)]
            pss = ps('pss', [128, 512], F32)
            pT = [sb(f'pT{i}', [128, 512], BF16) for i in range(4)]
            r1 = sb('r1', [128, 512], F32); a1 = sb('a1', [128, 512], F32); a2 = sb('a2', [128, 512], F32)
            sq = sb('sq', [128, 512], BF16); rs = sb('rs', [128, 512], F32)
            ybb = [sb(f'ybb{i}', [128, 512], BF16) for i in range(2)]
            LOOK = 2
            items = []
            heads = [(seq, hd) for seq in range(NSEQ) for hd in range(4)]
            for hi, (seq, hd) in enumerate(heads):
                for j in range(8):
                    last = 4 * j + 3
                    for i in range(last + 1):
                        for comp in range(2):
                            items.append(dict(hi=hi, seq=seq, hd=hd, j=j, i=i, comp=comp, last=last, first=(j == 0 and i == 0 and comp == 0)))
            n_y = [0]

            def load_v(seq):
                tb = seq * SEQ
                for t4 in range(4):
                    S.dma('sp', DMA(dvt.t[:, t4 * 8:(t4 + 1) * 8, :], DV[tb + t4 * 1024: tb + (t4 + 1) * 1024, :].rearrange("(t s) d -> s t d", s=128)),
                          pwrites=[dvt])

            def load_head(hi):
                seq, hd = heads[hi]
                tb = seq * SEQ
                q1, q2, k1, k2 = qk[hi % 2]
                S.dma('sp', DMA(q1.t[:, :], DQ[hd * 2, :, tb:tb + SEQ]), writes=[q1])
                S.dma('sp', DMA(q2.t[:, :], DQ[hd * 2 + 1, :, tb:tb + SEQ]), writes=[q2])
                S.dma('sp', DMA(k1.t[:, :], DK[hd * 2, :, tb:tb + SEQ]), writes=[k1])
                S.dma('sp', DMA(k2.t[:, :], DK[hd * 2 + 1, :, tb:tb + SEQ]), writes=[k2])

            def emit_s(n):
                it = items[n]
                if it['first']:
                    if it['hi'] == 0:
                        load_v(0)
                        load_head(0)
                    if it['hi'] + 1 < len(heads):
                        load_head(it['hi'] + 1)
                q1, q2, k1, k2 = qk[it['hi'] % 2]
                qq, kk = (q1, k1) if it['comp'] == 0 else (q2, k2)
                i, j = it['i'], it['j']
                off = max(0, i - 4 * j) * 128
                diag = i >= 4 * j
                spb = sp_[n % len(sp_)]
                fns = [MM(spb.t[:, off:512], kk.t[0:64, i * 128:(i + 1) * 128], qq.t[0:64, j * 512 + off:(j + 1) * 512], True, not diag)]
                if diag:
                    fns.append(MM(spb.t[:, off:off + 128], identb.t[:, :], maskb.t[:, :], False, True))
                S.op('pe', fns, reads=[kk, qq, identb, maskb], writes=[spb])

            def emit_rest(n):
                it = items[n]
                i, j, hd, seq, comp = it['i'], it['j'], it['hd'], it['seq'], it['comp']
                tb = seq * SEQ
                off = max(0, i - 4 * j) * 128
                spb = sp_[n % len(sp_)]; pb = pT[n % len(pT)]
                S.op('act', ACTF(pb.t[:, off:512], spb.t[:, off:512], AF.Exp, scale=0.125), reads=[spb], writes=[pb])
                ob = OD[comp * 2]; db = OD[comp * 2 + 1]
                S.op('pe', [MM(ob.t[:, off:512], dvt.t[:, i, hd * 128:(hd + 1) * 128], pb.t[:, off:512], i == 0, i == it['last']),
                            MM(db.t[:, off:512], onesb.t[:, :], pb.t[:, off:512], i == 0, i == it['last'])],
                     reads=[dvt, pb, onesb], writes=[ob, db] if i == 0 else [], pwrites=[] if i == 0 else [ob, db])
                if i == it['last'] and comp == 1:
                    S.op('dve', RCP(r1.t[:, :], OD[1].t[:, :]), reads=[OD[1]], writes=[r1])
                    S.op('dve', TT(a1.t[:, :], OD[0].t[:, :], r1.t[:, :], ALU.mult), reads=[OD[0], r1], writes=[a1])
                    S.op('dve', RCP(r1.t[:, :], OD[3].t[:, :]), reads=[OD[3]], writes=[r1])
                    S.op('dve', TT(a2.t[:, :], OD[2].t[:, :], r1.t[:, :], ALU.mult), reads=[OD[2], r1], writes=[a2])
                    S.op('dve', STT(a1.t[:, :], a2.t[:, :], nlam.t[:, 0:1], a1.t[:, :], ALU.mult, ALU.add), reads=[a2, nlam, a1], writes=[a1])
                    S.op('act', ACTF(sq.t[:, :], a1.t[:, :], AF.Square), reads=[a1], writes=[sq])
                    S.op('pe', MM(pss.t[:, :], onesb.t[:, :], sq.t[:, :], True, True), reads=[onesb, sq], writes=[pss])
                    S.op('act', ACTF(rs.t[:, :], pss.t[:, :], AF.Ln, scale=1.0 / 128, bias=eps5.t[:, 0:1]), reads=[pss, eps5], writes=[rs])
                    S.op('act', ACTF(rs.t[:, :], rs.t[:, :], AF.Exp, scale=-0.5), reads=[rs], writes=[rs])
                    yb = ybb[n_y[0] % 2]; n_y[0] += 1
                    S.op('dve', STT(yb.t[:, :], a1.t[:, :], gsub.t[:, 0:1], rs.t[:, :], ALU.mult, ALU.mult), reads=[a1, gsub, rs], writes=[yb])
                    S.dma('pool', DMA(YB[hd * 128:(hd + 1) * 128, tb + j * 512: tb + (j + 1) * 512], yb.t[:, :]), reads=[yb])
                    if j == 7 and hd == 3 and it['hi'] + 1 < len(heads):
                        load_v(seq + 1)

            for n in range(min(LOOK, len(items))):
                emit_s(n)
            for n in range(len(items)):
                if n + LOOK < len(items):
                    emit_s(n + LOOK)
                emit_rest(n)
            S.end_phase()

        with contextlib.ExitStack() as es:
            sb, ps = mk(es)
            S.begin_phase('p3')
            identb = sb('identb', [128, 128], BF16)
            S.dma('sp', DMA(identb.t[:, :], io['c_ident_bf']), writes=[identb])
            identf = sb('identf', [128, 128], F32)
            S.dma('sp', DMA(identf.t[:, :], io['c_ident_f']), writes=[identf])
            trif = sb('trif', [128, 128], F32)
            S.dma('sp', DMA(trif.t[:, :], io['c_tri']), writes=[trif])
            onesf = sb('onesf', [128, 128], F32)
            S.op('pool', MSET(onesf.t[:, :], 1.0), writes=[onesf])
            eps_t = sb('eps_t', [128, 1], F32)
            S.op('dve', MSET(eps_t.t[:, :], 1e-6), writes=[eps_t])
            wa = sb('wa', [128, 4, D], BF16); wb = sb('wb', [128, 4, D], BF16); wo = sb('wo', [128, 8, D], BF16)
            wcq = sb('wcq', [128, 8, 256], BF16); wckv = sb('wckv', [128, 8, 512], BF16); wco = sb('wco', [64, 4, D], BF16)
            S.dma('pool', DMA(wa.t[:, :, :], io['w_branch_a'].rearrange("(c p) n -> p c n", p=128)), writes=[wa])
            S.dma('pool', DMA(wb.t[:, :, :], io['w_branch_b'].rearrange("(c p) n -> p c n", p=128)), writes=[wb])
            S.dma('pool', DMA(wo.t[:, :, :], io['w_out'].rearrange("(c p) n -> p c n", p=128)), writes=[wo])
            S.dma('pool', DMA(wcq.t[:, :, :], io['w_cq'].rearrange("(c p) n -> p c n", p=128)), writes=[wcq])
            S.dma('pool', DMA(wckv.t[:, :, :], io['w_ckv'].rearrange("(c p) n -> p c n", p=128)), writes=[wckv])
            S.dma('pool', DMA(wco.t[:, :, :], io['w_co'].rearrange("(h d) n -> d h n", d=64)), writes=[wco])
            wr = sb('wr', [128, 8, 36], F32)
            S.dma('sp', DMA(wr.t[:, :, 0:4], io['w_group'].rearrange("(c p) n -> p c n", p=128)), pwrites=[wr])
            S.dma('sp', DMA(wr.t[:, :, 4:36], io['w_expert'].rearrange("(c p) n -> p c n", p=128)), pwrites=[wr])
            brt = sb('brt', [128, 36], F32)
            S.dma('sp', DMA(brt.t[:, 0:4], io['b_group'].partition_broadcast(128)), pwrites=[brt])
            S.dma('sp', DMA(brt.t[:, 4:36], io['b_expert'].partition_broadcast(128)), pwrites=[brt])
            gcross = sb('gcross', [128, D], F32); gmem = sb('gmem', [128, D], F32); gffn = sb('gffn', [128, D], F32)
            S.dma('sp', DMA(gcross.t[:, :], io['g_cross'].partition_broadcast(128)), writes=[gcross])
            S.dma('sp', DMA(gmem.t[:, :], io['g_mem'].partition_broadcast(128)), writes=[gmem])
            S.dma('sp', DMA(gffn.t[:, :], io['g_ffn'].partition_broadcast(128)), writes=[gffn])
            kcT = sb('kcT', [64, NSEQ, 4, 256], BF16)
            vca = sb('vca', [128, NSEQ, 2, 4, 128], BF16)
            S.op('pool', MSET(vca.t[:, :, :, :, :], 1.0), writes=[vca])
            xt = [sb(f'xt{i}', [128, D], F32) for i in range(2)]
            x1 = sb('x1', [128, D], F32)
            x2 = [sb(f'x2_{i}', [128, D], F32) for i in range(2)]
            junk = sb('junk', [128, D], BF16)
            ss = sb('ss', [128, 1], F32); rstd = sb('rstd', [128, 1], F32)
            hxb = sb('hxb', [128, D], BF16)
            hmf = sb('hmf', [128, D], F32)
            hmb = [sb(f'hmb{i}', [128, D], BF16) for i in range(2)]
            hxT = [sb(f'hxT{s}', [128, 8, 128], BF16) for s in range(4)]
            hmT = sb('hmT', [128, 8, 128], F32)
            yaT = sb('yaT', [128, 4, 512], BF16); ybT = sb('ybT', [128, 4, 512], BF16)
            gaT = sb('gaT', [128, 8, 512], BF16); gbT = sb('gbT', [128, 8, 512], BF16)
            mT = sb('mT', [128, 8, 512], BF16)
            t1 = sb('t1', [128, 512], F32); t2 = sb('t2', [128, 512], F32)
            qcs = sb('qcs', [64, 4, 512], BF16)
            ycs = sb('ycs', [64, 4, 512], BF16)
            pT = [sb(f'pT{i}', [128, 512], BF16) for i in range(2)]
            rec = sb('rec', [128, 512], F32)
            pA = ps('pA', [128, 512], F32); pB = ps('pB', [128, 512], F32)
            pt = [ps(f'pt{i}', [128, 512], F32) for i in range(2)]
            tp = ps('tp', [128, D], BF16)
            pq = ps('pq', [128, 512], F32)
            spc = ps('spc', [128, 512], F32)
            opc = ps('opc', [128, 512], F32)
            lg = sb('lg', [128, 36], F32)
            gmax = sb('gmax', [128, 1], F32); gmask = sb('gmask', [128, 4], F32); ge = sb('ge', [128, 4], F32)
            gs = sb('gs', [128, 1], F32); gw = sb('gw', [128, 1], F32)
            sel = sb('sel', [128, 8], F32); top8 = sb('top8', [128, 8], F32)
            m1 = sb('m1', [128, 8], F32); m2 = sb('m2', [128, 8], F32)
            dw = sb('dw', [128, 1], F32)
            oh1 = sb('oh1', [128, 64, 32], F32); oh2 = sb('oh2', [128, 64, 32], F32)
            ohs = sb('ohs', [128, 32], F32); cum = sb('cum', [128, 32], F32); rk = sb('rk', [128, 32], F32)
            S.op('pool', MSET(cum.t[:, :], 0.0), writes=[cum])
            pos = sb('pos', [128, 64, 2], F32); wts = sb('wts', [128, 64, 2], F32)
            j32 = sb('j32', [128, 32], F32)

            for seq in range(NSEQ):
                for mt in range(2):
                    xb = xt[mt]
                    S.dma('sp', DMA(xb.t[:, :], io['mem'][seq * 256 + mt * 128: seq * 256 + (mt + 1) * 128, :]), writes=[xb])
                    rms_tok(xb, gmem, hxb, junk, ss, rstd, eps_t)
                    S.op('pe', [TR(tp.t[:, c * 128:(c + 1) * 128], hxb.t[:, c * 128:(c + 1) * 128], identb.t[:, :]) for c in range(8)],
                         reads=[hxb, identb], writes=[tp])
                    S.op('act', CP(hxT[mt].t[:, :, :], tp.t[:, :].rearrange("p (c t) -> p c t", c=8)), reads=[tp], writes=[hxT[mt]])
                    S.op('pe', [MM(pt[0].t[:, 0:256], hxT[mt].t[:, c, :], wckv.t[:, c, 256:512], c == 0, c == 7) for c in range(8)],
                         reads=[hxT[mt], wckv], writes=[pt[0]])
                    S.op('act', CP(vca.t[:, seq, mt, :, 0:64], pt[0].t[:, 0:256].rearrange("p (h d) -> p h d", d=64)), reads=[pt[0]], pwrites=[vca])
                for hh in range(4):
                    for mt in range(2):
                        S.op('pe', [MM(pq.t[0:64, mt * 128:(mt + 1) * 128], wckv.t[:, c, hh * 64:(hh + 1) * 64], hxT[mt].t[:, c, :], c == 0, c == 7) for c in range(8)],
                             reads=[hxT[mt], wckv], pwrites=[pq])
                    S.op('act', CP(kcT.t[:, seq, hh, :], pq.t[0:64, 0:256]), reads=[pq], pwrites=[kcT])

            it = 0
            for seq in range(NSEQ):
                for st in range(SEQ // 512):
                    T0 = seq * SEQ + st * 512
                    S.dma('sp', DMA(yaT.t[:, :, :], YA[:, T0:T0 + 512].rearrange("(c p) t -> p c t", p=128)), writes=[yaT])
                    S.dma('sp', DMA(ybT.t[:, :, :], YB[:, T0:T0 + 512].rearrange("(c p) t -> p c t", p=128)), writes=[ybT])
                    S.dma('sp', DMA(gaT.t[:, :, :], GA[:, T0:T0 + 512].rearrange("(c p) t -> p c t", p=128)), writes=[gaT])
                    S.dma('sp', DMA(gbT.t[:, :, :], GB[:, T0:T0 + 512].rearrange("(c p) t -> p c t", p=128)), writes=[gbT])
                    for n in range(8):
                        S.op('pe', [MM(pA.t[:, :], wa.t[:, c, n * 128:(n + 1) * 128], yaT.t[:, c, :], c == 0, c == 3) for c in range(4)],
                             reads=[wa, yaT], writes=[pA])
                        S.op('pe', [MM(pB.t[:, :], wb.t[:, c, n * 128:(n + 1) * 128], ybT.t[:, c, :], c == 0, c == 3) for c in range(4)],
                             reads=[wb, ybT], writes=[pB])
                        S.op('dve', TT(t1.t[:, :], pA.t[:, :], gaT.t[:, n, :], ALU.mult), reads=[pA, gaT], writes=[t1])
                        S.op('dve', TT(t2.t[:, :], pB.t[:, :], gbT.t[:, n, :], ALU.mult), reads=[pB, gbT], writes=[t2])
                        S.op('pool', TT(mT.t[:, n, :], t1.t[:, :], t2.t[:, :], ALU.add), reads=[t1, t2], pwrites=[mT])
                    for sub in range(4):
                        r0 = T0 + sub * 128
                        xb = xt[it % 2]
                        S.dma('sp', DMA(xb.t[:, :], io['x'][r0:r0 + 128, :]), writes=[xb])
                        for half in range(2):
                            pp = pt[half]
                            S.op('pe', [MM(pp.t[:, :], mT.t[:, n, sub * 128:(sub + 1) * 128], wo.t[:, n, half * 512:(half + 1) * 512], n == 0, n == 7) for n in range(8)],
                                 reads=[mT, wo], writes=[pp])
                            S.op('dve', TT(x1.t[:, half * 512:(half + 1) * 512], pp.t[:, :], xb.t[:, half * 512:(half + 1) * 512], ALU.add),
                                 reads=[pp, xb], pwrites=[x1])
                        rms_tok(x1, gcross, hxb, junk, ss, rstd, eps_t)
                        S.op('pe', [TR(tp.t[:, c * 128:(c + 1) * 128], hxb.t[:, c * 128:(c + 1) * 128], identb.t[:, :]) for c in range(8)],
                             reads=[hxb, identb], writes=[tp])
                        S.op('act', CP(hxT[sub].t[:, :, :], tp.t[:, :].rearrange("p (c t) -> p c t", c=8)), reads=[tp], writes=[hxT[sub]])
                        for hh in range(4):
                            S.op('pe', [MM(pq.t[0:64, hh * 128:(hh + 1) * 128], wcq.t[:, c, hh * 64:(hh + 1) * 64], hxT[sub].t[:, c, :], c == 0, c == 7) for c in range(8)],
                                 reads=[hxT[sub], wcq], pwrites=[pq])
                        S.op('act', CP(qcs.t[:, :, 0:128], pq.t[0:64, :].rearrange("p (h t) -> p h t", h=4)), reads=[pq], writes=[qcs])
                        for hh in range(4):
                            for mt in range(2):
                                pb = pT[mt]
                                S.op('pe', MM(spc.t[:, mt * 128:(mt + 1) * 128], kcT.t[0:64, seq, hh, mt * 128:(mt + 1) * 128], qcs.t[0:64, hh, 0:128], True, True),
                                     reads=[kcT, qcs], writes=[spc] if mt == 0 else [], pwrites=[] if mt == 0 else [spc])
                            S.op('act', ACTF(pT[0].t[:, 0:256], spc.t[:, 0:256], AF.Exp, scale=0.125), reads=[spc], writes=[pT[0]])
                            S.op('pe', [MM(opc.t[:, hh * 128:(hh + 1) * 128], vca.t[:, seq, mt, hh, :], pT[0].t[:, mt * 128:(mt + 1) * 128], mt == 0, mt == 1) for mt in range(2)],
                                 reads=[vca, pT[0]], pwrites=[opc])
                        S.op('dve', RCP(rec.t[64:128, :], opc.t[64:128, :]), reads=[opc], writes=[rec])
                        S.op('dve', TT(ycs.t[0:64, :, 0:128], opc.t[0:64, :].rearrange("p (h t) -> p h t", h=4),
                                       rec.t[64:128, :].rearrange("p (h t) -> p h t", h=4), ALU.mult), reads=[opc, rec], writes=[ycs])
                        xo = x2[it % 2]
                        for half in range(2):
                            pp = pt[half]
                            S.op('pe', [MM(pp.t[:, :], ycs.t[0:64, hh, 0:128], wco.t[0:64, hh, half * 512:(half + 1) * 512], hh == 0, hh == 3) for hh in range(4)],
                                 reads=[ycs, wco], writes=[pp])
                            S.op('dve', TT(xo.t[:, half * 512:(half + 1) * 512], pp.t[:, :], x1.t[:, half * 512:(half + 1) * 512], ALU.add),
                                 reads=[pp, x1], pwrites=[xo])
                        S.dma('pool', DMA(X2[r0:r0 + 128, :], xo.t[:, :]), reads=[xo])
                        rms_tok(xo, gffn, hmf, junk, ss, rstd, eps_t)
                        hb_ = hmb[it % 2]
                        S.op('act', CP(hb_.t[:, :], hmf.t[:, :]), reads=[hmf], writes=[hb_])
                        S.dma('pool', DMA(HM[r0:r0 + 128, :], hb_.t[:, :]), reads=[hb_])
                        for half in range(2):
                            pp = pt[half]
                            S.op('pe', [TR(pp.t[:, cq * 128:(cq + 1) * 128], hmf.t[:, (half * 4 + cq) * 128:(half * 4 + cq + 1) * 128], identf.t[:, :]) for cq in range(4)],
                                 reads=[hmf, identf], writes=[pp])
                            S.op('act', CP(hmT.t[:, half * 4:(half + 1) * 4, :], pp.t[:, :].rearrange("p (c t) -> p c t", c=4)), reads=[pp], pwrites=[hmT])
                        S.op('pe', [MM(pq.t[:, 0:36], hmT.t[:, c, :], wr.t[:, c, :], c == 0, c == 7) for c in range(8)], reads=[hmT, wr], writes=[pq])
                        S.op('dve', TT(lg.t[:, :], pq.t[:, 0:36], brt.t[:, :], ALU.add), reads=[pq, brt], writes=[lg])
                        ti = it
                        S.op('dve', lambda e: e.tensor_reduce(out=gmax.t[:, 0:1], in_=lg.t[:, 0:4], axis=AX.X, op=ALU.max), reads=[lg], writes=[gmax])
                        S.op('dve', TS(gmask.t[:, :], lg.t[:, 0:4], gmax.t[:, 0:1], None, ALU.is_equal), reads=[lg, gmax], writes=[gmask])
                        S.op('dve', TS(ge.t[:, :], lg.t[:, 0:4], gmax.t[:, 0:1], None, ALU.subtract), reads=[lg, gmax], writes=[ge])
                        S.op('act', ACTF(ge.t[:, :], ge.t[:, :], AF.Exp), reads=[ge], writes=[ge])
                        S.op('dve', lambda e: e.tensor_reduce(out=gs.t[:, 0:1], in_=ge.t[:, :], axis=AX.X, op=ALU.add), reads=[ge], writes=[gs])
                        S.op('dve', RCP(gw.t[:, :], gs.t[:, :]), reads=[gs], writes=[gw])
                        S.op('dve', TS(sel.t[:, :], lg.t[:, 4:12], gmask.t[:, 0:1], None, ALU.mult), reads=[lg, gmask], writes=[sel])
                        for g in range(1, 4):
                            S.op('dve', STT(sel.t[:, :], lg.t[:, 4 + g * 8: 12 + g * 8], gmask.t[:, g:g + 1], sel.t[:, :], ALU.mult, ALU.add),
                                 reads=[lg, gmask, sel], writes=[sel])
                        S.op('dve', lambda e: e.max(out=top8.t[:, :], in_=sel.t[:, :]), reads=[sel], writes=[top8])
                        S.op('dve', TS(m1.t[:, :], sel.t[:, :], top8.t[:, 0:1], None, ALU.is_equal), reads=[sel, top8], writes=[m1])
                        S.op('dve', TS(m2.t[:, :], sel.t[:, :], top8.t[:, 1:2], None, ALU.is_equal), reads=[sel, top8], writes=[m2])
                        S.op('dve', TT(dw.t[:, :], top8.t[:, 1:2], top8.t[:, 0:1], ALU.subtract), reads=[top8], writes=[dw])
                        S.op('act', ACTF(dw.t[:, :], dw.t[:, :], AF.Exp), reads=[dw], writes=[dw])
                        S.op('dve', TS(dw.t[:, :], dw.t[:, :], 1.0, None, ALU.add), reads=[dw], writes=[dw])
                        S.op('dve', RCP(dw.t[:, :], dw.t[:, :]), reads=[dw], writes=[dw])
                        S.op('dve', TT(wts.t[:, ti, 0:1], dw.t[:, :], gw.t[:, :], ALU.mult), reads=[dw, gw], pwrites=[wts])
                        S.op('dve', TT(wts.t[:, ti, 1:2], gw.t[:, :], wts.t[:, ti, 0:1], ALU.subtract), reads=[gw, wts], pwrites=[wts])
                        for (mm_, oh) in ((m1, oh1), (m2, oh2)):
                            S.op('dve', TT(oh.t[:, ti, :].rearrange("p (g e) -> p g e", g=4),
                                           gmask.t[:, :].unsqueeze(2).to_broadcast([128, 4, 8]),
                                           mm_.t[:, :].unsqueeze(1).to_broadcast([128, 4, 8]), ALU.mult), reads=[gmask, mm_], pwrites=[oh])
                        S.op('dve', TT(ohs.t[:, :], oh1.t[:, ti, :], oh2.t[:, ti, :], ALU.add), reads=[oh1, oh2], writes=[ohs])
                        S.op('pe', [MM(spc.t[:, 0:32], trif.t[:, :], ohs.t[:, :], True, False), MM(spc.t[:, 0:32], onesf.t[:, :], cum.t[:, :], False, True)],
                             reads=[trif, ohs, onesf, cum], writes=[spc])
                        S.op('dve', CP(rk.t[:, :], spc.t[:, 0:32]), reads=[spc], writes=[rk])
                        S.op('pool', TT(cum.t[:, :], cum.t[:, :], ohs.t[:, :], ALU.add), reads=[cum, ohs], writes=[cum])
                        S.op('dve', TTR(j32.t[:, :], rk.t[:, :], oh1.t[:, ti, :], pos.t[:, ti, 0:1]), reads=[rk, oh1], writes=[j32], pwrites=[pos])
                        S.op('dve', TTR(j32.t[:, :], rk.t[:, :], oh2.t[:, ti, :], pos.t[:, ti, 1:2]), reads=[rk, oh2], writes=[j32], pwrites=[pos])
                        it += 1

            cnt = sb('cnt', [128, 32], F32); pad = sb('pad', [128, 32], F32); pend = sb('pend', [128, 32], F32); pstart = sb('pstart', [128, 32], F32)
            ki = sb('ki', [128, 32], I32); kf = sb('kf', [128, 32], F32); kc = sb('kc', [128, 32], F32)
            ones32 = sb('ones32', [128, 32], F32)
            S.op('pool', MSET(ones32.t[:, :], 1.0), writes=[ones32])
            S.op('pe', MM(spc.t[:, 0:32], onesf.t[:, :], cum.t[:, :], True, True), reads=[onesf, cum], writes=[spc])
            S.op('dve', CP(cnt.t[:, :], spc.t[:, 0:32]), reads=[spc], writes=[cnt])
            S.op('dve', TS(pad.t[:, :], cnt.t[:, :], 1.0 / MB, None, ALU.mult), reads=[cnt], writes=[pad])
            S.op('dve', CP(ki.t[:, :], pad.t[:, :]), reads=[pad], writes=[ki])
            S.op('dve', CP(kf.t[:, :], ki.t[:, :]), reads=[ki], writes=[kf])
            S.op('dve', TT(kc.t[:, :], kf.t[:, :], pad.t[:, :], ALU.is_lt), reads=[kf, pad], writes=[kc])
            S.op('dve', TT(kf.t[:, :], kf.t[:, :], kc.t[:, :], ALU.add), reads=[kf, kc], writes=[kf])
            S.op('dve', TS(pad.t[:, :], kf.t[:, :], float(MB), None, ALU.mult), reads=[kf], writes=[pad])
            S.op('dve', lambda e: e.tensor_tensor_scan(out=pend.t[:, :], data0=ones32.t[:, :], data1=pad.t[:, :], initial=0.0, op0=ALU.mult, op1=ALU.add),
                 reads=[ones32, pad], writes=[pend])
            S.op('dve', TT(pstart.t[:, :], pend.t[:, :], pad.t[:, :], ALU.subtract), reads=[pend, pad], writes=[pstart])
            dst = sb('dst', [128, 64, 2], F32)
            for ti in range(64):
                for k, oh in enumerate((oh1, oh2)):
                    S.op('dve', TTR(j32.t[:, :], pstart.t[:, :], oh.t[:, ti, :], dst.t[:, ti, k:k + 1]), reads=[pstart, oh], writes=[j32], pwrites=[dst])
            S.op('dve', TT(dst.t[:, :, :], dst.t[:, :, :], pos.t[:, :, :], ALU.add), reads=[dst, pos], writes=[dst])
            rt = sb('rt', [128, 64, 8], F32)
            S.op('pool', MSET(rt.t[:, :, :], 0.0), writes=[rt])
            S.op('dve', CP(rt.t[:, :, 0:2], dst.t[:, :, :]), reads=[dst], pwrites=[rt])
            S.op('dve', CP(rt.t[:, :, 2:4], wts.t[:, :, :]), reads=[wts], pwrites=[rt])
            bst = sb('bst', [128, NB], F32)
            S.dma('sp', DMA(bst.t[:, :], io['c_bstart']), writes=[bst])
            cmpb = sb('cmpb', [128, NB, 32], F32)
            S.op('dve', TT(cmpb.t[:, :, :], pend.t[:, :].unsqueeze(1).to_broadcast([128, NB, 32]),
                           bst.t[:, :].unsqueeze(2).to_broadcast([128, NB, 32]), ALU.is_le), reads=[pend, bst], writes=[cmpb])
            bef = sb('bef', [128, NB], F32)
            S.op('dve', lambda e: e.tensor_reduce(out=bef.t[:, :], in_=cmpb.t[:, :, :], axis=AX.X, op=ALU.add), reads=[cmpb], writes=[bef])
            pio = sb('pio', [128, 1], F32)
            S.dma('sp', DMA(pio.t[:, :], io['c_piota']), writes=[pio])
            S.op('dve', TS(bef.t[:, :], bef.t[:, :], 31.0, 128.0, ALU.min, ALU.mult), reads=[bef], writes=[bef])
            S.op('dve', TS(bef.t[:, :], bef.t[:, :], pio.t[:, 0:1], None, ALU.add), reads=[bef, pio], writes=[bef])
            S.dma('pool', DMA(RT.rearrange("(t p) k -> p t k", p=128), rt.t[:, :, :]), reads=[rt])
            BE = scr('BE', [128, NB], F32)
            S.dma('pool', DMA(BE[:, :], bef.t[:, :]), reads=[bef])
            S.end_phase()

        with contextlib.ExitStack() as es:
            sb, ps = mk(es)
            S.begin_phase('p4')
            rtf = sb('rtf', [128, 64, 8], F32)
            S.dma('sp', DMA(rtf.t[:, :, :], RT.rearrange("(t p) k -> p t k", p=128)), writes=[rtf])
            dsti = sb('dsti', [128, 64, 2], I32)
            S.op('dve', CP(dsti.t[:, :, :], rtf.t[:, :, 0:2]), reads=[rtf], writes=[dsti])
            hm = [sb(f'hm{i}', [128, D], BF16) for i in range(4)]
            for ti in range(64):
                hb_ = hm[ti % 4]
                S.dma('sp', DMA(hb_.t[:, :], HM[ti * 128:(ti + 1) * 128, :]), writes=[hb_])
                for k in range(2):
                    S.dma('pool', (lambda e, hb_=hb_, ti=ti, k=k: e.indirect_dma_start(
                        out=XS[:, :], out_offset=bass.IndirectOffsetOnAxis(ap=dsti.t[:, ti, k:k + 1], axis=0),
                        in_=hb_.t[:, :], in_offset=None)), reads=[hb_, dsti])
            S.end_phase()

        with contextlib.ExitStack() as es:
            sb, ps = mk(es)
            S.begin_phase('p5')
            identb = sb('identb', [128, 128], BF16)
            S.dma('sp', DMA(identb.t[:, :], io['c_ident_bf']), writes=[identb])
            bef = sb('bef', [128, NB], F32)
            S.dma('sp', DMA(bef.t[:, :], BE[:, :]), writes=[bef])
            bei = sb('bei', [128, NB], I32)
            S.op('dve', CP(bei.t[:, :], bef.t[:, :]), reads=[bef], writes=[bei])
            wst = [sb(f'wst{i}', [128, 4096], F32) for i in range(3)]
            wgb = [sb(f'wgb{i}', [128, 8, 512], BF16) for i in range(2)]
            wub = [sb(f'wub{i}', [128, 8, 512], BF16) for i in range(2)]
            wdb = [sb(f'wdb{i}', [128, 4, D], BF16) for i in range(2)]
            xs = [sb(f'xs{i}', [128, D], BF16) for i in range(4)]
            xsT = sb('xsT', [128, 8, 512], BF16)
            haT = sb('haT', [128, 4, 512], BF16)
            sg = sb('sg', [128, 512], F32)
            ysb = [sb(f'ysb{i}', [128, D], BF16) for i in range(2)]
            tp = [ps(f'tp{i}', [128, D], BF16) for i in range(2)]
            pg = [ps(f'pg{i}', [128, 512], F32) for i in range(2)]
            pu = [ps(f'pu{i}', [128, 512], F32) for i in range(2)]
            py = [ps(f'py{i}', [128, 512], F32) for i in range(2)]
            n_t = 0; n_g = 0; n_y = 0; n_ys = 0
            for b in range(NB):
                wg_, wu_, wd_ = wgb[b % 2], wub[b % 2], wdb[b % 2]
                for (src, stg, dstb, eng) in ((io['w_exp_gate'], wst[0], wg_, 'dve'), (io['w_exp_up'], wst[1], wu_, 'pool'), (io['w_exp_down'], wst[2], wd_, 'act')):
                    S.dma('pool', (lambda e, src=src, stg=stg, b=b: e.indirect_dma_start(
                        out=stg.t[:, :], out_offset=None, in_=src[:, :],
                        in_offset=bass.IndirectOffsetOnAxis(ap=bei.t[:, b:b + 1], axis=0))), reads=[bei], writes=[stg])
                    flat = dstb.t[:, :, :].rearrange("p c f -> p (c f)")
                    for hf in range(2):
                        S.op(eng, CP(flat[:, hf * 2048:(hf + 1) * 2048], stg.t[:, hf * 2048:(hf + 1) * 2048]), reads=[stg], writes=[dstb] if hf == 0 else [], pwrites=[] if hf == 0 else [dstb])
                for r in range(4):
                    S.dma('sp', DMA(xs[r].t[:, :], XS[b * MB + r * 128: b * MB + (r + 1) * 128, :]), writes=[xs[r]])
                for cp_ in range(4):
                    tpp = tp[n_t % 2]; n_t += 1
                    S.op('pe', [TR(tpp.t[:, ci * 512 + r * 128: ci * 512 + (r + 1) * 128], xs[r].t[:, bass.ds(cp_ * 2 + ci, 128, step=8)], identb.t[:, :])
                                for ci in range(2) for r in range(4)], reads=xs + [identb], writes=[tpp])
                    S.op('dve' if cp_ % 2 == 0 else 'act', CP(xsT.t[:, cp_ * 2:cp_ * 2 + 2, :], tpp.t[:, :].rearrange("p (c t) -> p c t", c=2)), reads=[tpp],
                         writes=[xsT] if cp_ == 0 else [], pwrites=[] if cp_ == 0 else [xsT])
                for j in range(4):
                    g_ = pg[n_g % 2]; u_ = pu[n_g % 2]; n_g += 1
                    S.op('pe', [MM(g_.t[:, :], wg_.t[:, c, bass.ds(j, 128, step=4)], xsT.t[:, c, :], c == 0, c == 7) for c in range(8)], reads=[wg_, xsT], writes=[g_])
                    S.op('pe', [MM(u_.t[:, :], wu_.t[:, c, bass.ds(j, 128, step=4)], xsT.t[:, c, :], c == 0, c == 7) for c in range(8)], reads=[wu_, xsT], writes=[u_])
                    S.op('act', ACTF(sg.t[:, :], g_.t[:, :], AF.Silu), reads=[g_], writes=[sg])
                    S.op('dve', TT(haT.t[:, j, :], u_.t[:, :], sg.t[:, :], ALU.mult), reads=[u_, sg], writes=[haT] if j == 0 else [], pwrites=[] if j == 0 else [haT])
                for r in range(4):
                    yb = ysb[n_ys % 2]; n_ys += 1
                    for half in range(2):
                        y_ = py[n_y % 2]; n_y += 1
                        S.op('pe', [MM(y_.t[:, :], haT.t[:, j, r * 128:(r + 1) * 128], wd_.t[:, j, half * 512:(half + 1) * 512], j == 0, j == 3) for j in range(4)],
                             reads=[haT, wd_], writes=[y_])
                        S.op('act' if half == 0 else 'dve', CP(yb.t[:, half * 512:(half + 1) * 512], y_.t[:, :]), reads=[y_], writes=[yb] if half == 0 else [], pwrites=[] if half == 0 else [yb])
                    S.dma('sp', DMA(YS[b * MB + r * 128: b * MB + (r + 1) * 128, :], yb.t[:, :]), reads=[yb])
            S.end_phase()

        with contextlib.ExitStack() as es:
            sb, ps = mk(es)
            S.begin_phase('p6')
            rtf = sb('rtf', [128, 64, 8], F32)
            S.dma('sp', DMA(rtf.t[:, :, :], RT.rearrange("(t p) k -> p t k", p=128)), writes=[rtf])
            dsti = sb('dsti', [128, 64, 2], I32)
            S.op('dve', CP(dsti.t[:, :, :], rtf.t[:, :, 0:2]), reads=[rtf], writes=[dsti])
            gfin = sb('gfin', [128, D], F32)
            S.dma('sp', DMA(gfin.t[:, :], io['g_final'].partition_broadcast(128)), writes=[gfin])
            eps_t = sb('eps_t', [128, 1], F32)
            S.op('dve', MSET(eps_t.t[:, :], 1e-6), writes=[eps_t])
            xin = [sb(f'xin{i}', [128, D], F32) for i in range(2)]
            y1 = [sb(f'y1_{i}', [128, D], BF16) for i in range(2)]
            y2 = [sb(f'y2_{i}', [128, D], BF16) for i in range(2)]
            xa = sb('xa', [128, D], F32)
            junk = sb('junk', [128, D], BF16)
            ss = sb('ss', [128, 1], F32); rstd = sb('rstd', [128, 1], F32)
            ob = [sb(f'ob{i}', [128, D], F32) for i in range(2)]
            for ti in range(64):
                xb = xin[ti % 2]; ya_ = y1[ti % 2]; yb_ = y2[ti % 2]; o_ = ob[ti % 2]
                S.dma('sp', DMA(xb.t[:, :], X2[ti * 128:(ti + 1) * 128, :]), writes=[xb])
                for k, yy in enumerate((ya_, yb_)):
                    S.dma('pool', (lambda e, yy=yy, ti=ti, k=k: e.indirect_dma_start(
                        out=yy.t[:, :], out_offset=None, in_=YS[:, :],
                        in_offset=bass.IndirectOffsetOnAxis(ap=dsti.t[:, ti, k:k + 1], axis=0))), reads=[dsti], writes=[yy])
                S.op('dve', STT(xa.t[:, :], ya_.t[:, :], rtf.t[:, ti, 2:3], xb.t[:, :], ALU.mult, ALU.add), reads=[ya_, rtf, xb], writes=[xa])
                S.op('dve', STT(xa.t[:, :], yb_.t[:, :], rtf.t[:, ti, 3:4], xa.t[:, :], ALU.mult, ALU.add), reads=[yb_, rtf, xa], writes=[xa])
                rms_tok(xa, gfin, o_, junk, ss, rstd, eps_t)
                S.dma('sp', DMA(out[ti * 128:(ti + 1) * 128, :], o_.t[:, :]), reads=[o_])
            S.end_phase()
        print("bass ops recorded:", S.nops)
    return nc


_CACHE = {}


def _consts():
    bf = ml_dtypes.bfloat16
    s = np.arange(128)[:, None]
    t = np.arange(128)[None, :]
    half = 32
    inv_freq = (10000.0 ** (-np.arange(half, dtype=np.float32) * 2.0 / 64)).astype(np.float32)
    return {
        'c_ident_bf': np.eye(128, dtype=np.float32).astype(bf),
        'c_ident_f': np.eye(128, dtype=np.float32),
        'c_tri': (s < t).astype(np.float32),
        'c_mask': np.where(s <= t, 0.0, -30000.0).astype(np.float32).astype(bf),
        'c_invf': np.concatenate([inv_freq, inv_freq]).reshape(64, 1).astype(np.float32),
        'c_bstart': np.broadcast_to((np.arange(NB, dtype=np.float32) * MB)[None, :], (128, NB)).copy(),
        'c_piota': np.arange(128, dtype=np.float32).reshape(128, 1),
    }


def kernel(**inputs):
    if 'nc' not in _CACHE:
        _CACHE['nc'] = build_program()
    nc = _CACHE['nc']
    a = {k: np.asarray(v) for k, v in inputs.items()}
    shared = {
        'g_mix': a['g_mix'][0], 'w_in': a['w_in'][0], 'b_fgate': a['b_fgate'][0],
        'w_branch_a': a['w_branch_a'][0], 'w_branch_b': a['w_branch_b'][0], 'w_out': a['w_out'][0],
        'lambda_q1': a['lambda_q1'][0], 'lambda_k1': a['lambda_k1'][0], 'lambda_q2': a['lambda_q2'][0], 'lambda_k2': a['lambda_k2'][0],
        'g_diff_sub': a['g_diff_sub'][0], 'g_cross': a['g_cross'][0], 'g_mem': a['g_mem'][0],
        'w_cq': a['w_cq'][0], 'w_ckv': a['w_ckv'][0], 'w_co': a['w_co'][0], 'g_ffn': a['g_ffn'][0],
        'w_group': a['w_group'][0], 'b_group': a['b_group'][0], 'w_expert': a['w_expert'][0], 'b_expert': a['b_expert'][0],
        'w_exp_gate': a['w_exp_gate'][0].reshape(NEXP * 128, 4096),
        'w_exp_up': a['w_exp_up'][0].reshape(NEXP * 128, 4096),
        'w_exp_down': a['w_exp_down'][0].reshape(NEXP * 128, 4096),
        'g_final': a['g_final'],
    }
    shared = {k: np.ascontiguousarray(v) for k, v in shared.items()}
    shared.update(_consts())
    in_maps = []
    for c in range(NCORES):
        m = dict(shared)
        m['x'] = np.ascontiguousarray(a['x'][c * NSEQ:(c + 1) * NSEQ].reshape(NT, D))
        m['mem'] = np.ascontiguousarray(a['mem'][c * NSEQ:(c + 1) * NSEQ].reshape(NSEQ * 256, D))
        m['positions'] = np.ascontiguousarray(a['positions'][c * NSEQ:(c + 1) * NSEQ].astype(np.int32))
        in_maps.append(m)
    res = run_bass_kernel_spmd(nc, in_maps, core_ids=list(range(NCORES)))
    outs = [np.asarray(r['out']).reshape(NSEQ, SEQ, D) for r in res.results]
    return np.concatenate(outs, axis=0).astype(np.float32)
```

```python
import contextlib
import math
import numpy as np
import ml_dtypes
import concourse.bass as bass
import concourse.mybir as mybir
from concourse.bass_utils import run_bass_kernel_spmd

F32 = mybir.dt.float32
BF16 = mybir.dt.bfloat16
I32 = mybir.dt.int32
ALU = mybir.AluOpType
AF = mybir.ActivationFunctionType
AX = mybir.AxisListType

NCORES = 8
SEQ = 4096
D = 1024
NSEQ = 2
NT = NSEQ * SEQ
INW = 5128
NEXP = 32
MB = 512
NB = NT * 2 // MB + NEXP
NROWS = NB * MB
C_FQ, C_FK, C_FV, C_FL, C_DQ, C_DK, C_DV, C_GA, C_GB = 0, 512, 1024, 1536, 1544, 2056, 2568, 3080, 4104
LAMBDA_INIT = 0.8 - 0.6 * math.exp(0.0)
NDS = 8
DEBUG_OUT = False
MAX_PHASE = 99


class Buf:
    def __init__(self, t):
        self.t = t
        self.w = {}
        self.pw = {}
        self.r = {}


class Sched:
    ENG = ['pe', 'dve', 'act', 'pool', 'sp']

    def __init__(self, nc, es):
        self.nc = nc
        self.es = es
        self.dma_pool = {q: [es.enter_context(nc.semaphore(f"dq_{q}_{i}")) for i in range(NDS)] for q in ('sp', 'pool', 'act')}
        self.dma_uses = {}
        for q in self.dma_pool:
            for s in self.dma_pool[q]:
                self.dma_uses[id(s)] = 0
        self.semobj = {}
        for q in self.dma_pool:
            for s in self.dma_pool[q]:
                self.semobj[id(s)] = s
        self.dma_rr = {'sp': 0, 'pool': 0, 'act': 0}
        self.nops = 0
        self.phase_idx = 0

    def begin_phase(self, name):
        self.phase_idx += 1
        self.esem = {}
        for e in self.ENG:
            s = self.es.enter_context(self.nc.semaphore(f"{name}_{e}"))
            self.esem[e] = s
            self.semobj[id(s)] = s
        self.cnt = {e: 0 for e in self.ENG}
        self.ops = {e: [] for e in self.ENG}
        self.seen = {e: {} for e in self.ENG}

    def _waits(self, eng, toks):
        out = []
        for (s, v) in toks:
            if v <= 0:
                continue
            if self.seen[eng].get(s, 0) >= v:
                continue
            self.seen[eng][s] = v
            out.append((s, v))
        return out

    def _deps(self, reads, writes, pwrites):
        toks = []
        for b in reads:
            toks.extend(b.w.items())
            toks.extend(b.pw.items())
        for b in writes:
            toks.extend(b.w.items())
            toks.extend(b.pw.items())
            toks.extend(b.r.items())
        for b in pwrites:
            toks.extend(b.w.items())
            toks.extend(b.r.items())
        return toks

    def _commit(self, sid, val, reads, writes, pwrites):
        for b in reads:
            b.r[sid] = val
        for b in writes:
            b.w = {sid: val}
            b.pw = {}
            b.r = {}
        for b in pwrites:
            b.pw[sid] = val

    def op(self, eng, fns, reads=(), writes=(), pwrites=()):
        own = id(self.esem[eng])
        toks = self._deps(reads, writes, pwrites)
        if eng == 'pe':
            toks = [t for t in toks if t[0] != own]
        waits = self._waits(eng, toks)
        self.cnt[eng] += 1
        if callable(fns):
            fns = [fns]
        self.ops[eng].append((waits, fns, own, 1))
        self.nops += len(fns) + len(waits)
        self._commit(own, self.cnt[eng], reads, writes, pwrites)

    def dma(self, q, fn, reads=(), writes=(), pwrites=()):
        pool = self.dma_pool[q]
        i = self.dma_rr[q]
        self.dma_rr[q] = (i + 1) % len(pool)
        s = id(pool[i])
        toks = self._deps(reads, writes, pwrites) + [(s, 16 * self.dma_uses[s])]
        waits = self._waits(q, toks)
        self.dma_uses[s] += 1
        self.ops[q].append((waits, [fn], s, 16))
        self.nops += 1 + len(waits)
        self._commit(s, 16 * self.dma_uses[s], reads, writes, pwrites)

    def end_phase(self):
        final = [(id(self.esem[e]), self.cnt[e]) for e in self.ENG]
        final += [(s, 16 * u) for s, u in self.dma_uses.items()]
        for e in self.ENG:
            waits = self._waits(e, final)
            self.ops[e].append((waits, [], None, 0))
        if self.phase_idx > MAX_PHASE:
            return
        semobj = self.semobj
        ops = self.ops

        def replay(h, lst):
            for waits, fns, sem, inc in lst:
                for (s, v) in waits:
                    h.wait_ge(semobj[s], v)
                for k, fn in enumerate(fns):
                    ins = fn(h)
                    if k == len(fns) - 1 and sem is not None:
                        ins.then_inc(semobj[sem], inc)

        with self.nc.Block() as block:
            @block.tensor
            def _(h):
                replay(h, ops['pe'])

            @block.vector
            def _(h):
                replay(h, ops['dve'])

            @block.scalar
            def _(h):
                replay(h, ops['act'])

            @block.gpsimd
            def _(h):
                replay(h, ops['pool'])

            @block.sync
            def _(h):
                replay(h, ops['sp'])


def MM(out, lhsT, rhs, start=True, stop=True):
    return lambda e: e.matmul(out, lhsT=lhsT, rhs=rhs, start=start, stop=stop)


def TR(out, in_, ident):
    return lambda e: e.transpose(out, in_, ident)


def ACTF(out, in_, func, **kw):
    return lambda e: e.activation(out=out, in_=in_, func=func, **kw)


def TT(out, in0, in1, op):
    return lambda e: e.tensor_tensor(out=out, in0=in0, in1=in1, op=op)


def TS(out, in0, s1, s2, op0, op1=None):
    if op1 is None:
        return lambda e: e.tensor_scalar(out=out, in0=in0, scalar1=s1, scalar2=None, op0=op0)
    return lambda e: e.tensor_scalar(out=out, in0=in0, scalar1=s1, scalar2=s2, op0=op0, op1=op1)


def STT(out, in0, scalar, in1, op0, op1):
    return lambda e: e.scalar_tensor_tensor(out=out, in0=in0, scalar=scalar, in1=in1, op0=op0, op1=op1)


def CP(out, in_):
    return lambda e: (e.tensor_copy(out=out, in_=in_) if hasattr(e, 'tensor_copy') else e.activation(out=out, in_=in_, func=AF.Copy))


def RCP(out, in_):
    return lambda e: e.reciprocal(out=out, in_=in_)


def MSET(ap, c):
    return lambda e: e.memset(ap, c)


def DMA(out, in_):
    return lambda e: e.dma_start(out=out, in_=in_)


def TTR(out, in0, in1, accum):
    return lambda e: e.scalar_tensor_tensor(out=out, in0=in0, scalar=1.0, in1=in1, op0=ALU.mult, op1=ALU.mult, accum_out=accum)


def build_program():
    nc = bass.Bass("TRN2", target_bir_lowering=False)
    io = {}

    def din(name, shape, dt=F32):
        io[name] = nc.dram_tensor(name, shape, dt, kind="ExternalInput").ap()

    din('x', [NT, D]); din('mem', [NSEQ * 256, D]); din('positions', [NSEQ, SEQ], I32)
    din('g_mix', [D]); din('w_in', [D, INW]); din('b_fgate', [8])
    din('w_branch_a', [512, D]); din('w_branch_b', [512, D]); din('w_out', [D, D])
    for n in ('lambda_q1', 'lambda_k1', 'lambda_q2', 'lambda_k2'):
        din(n, [64])
    din('g_diff_sub', [128]); din('g_cross', [D]); din('g_mem', [D])
    din('w_cq', [D, 256]); din('w_ckv', [D, 512]); din('w_co', [256, D]); din('g_ffn', [D])
    din('w_group', [D, 4]); din('b_group', [4]); din('w_expert', [D, 32]); din('b_expert', [32])
    din('w_exp_gate', [NEXP * 128, 4096]); din('w_exp_up', [NEXP * 128, 4096]); din('w_exp_down', [NEXP * 128, 4096])
    din('g_final', [D])
    din('c_ident_bf', [128, 128], BF16); din('c_ident_f', [128, 128]); din('c_tri', [128, 128]); din('c_mask', [128, 128], BF16)
    din('c_invf', [64, 1]); din('c_bstart', [128, NB]); din('c_piota', [128, 1])
    out = nc.dram_tensor('out', [NT, D], F32, kind="ExternalOutput").ap()

    def scr(name, shape, dt):
        return nc.dram_tensor(name, shape, dt, kind=("ExternalOutput" if DEBUG_OUT else "Internal")).ap()

    FQ = scr('FQ', [8, 70, NT], BF16); FK = scr('FK', [8, 70, NT], BF16)
    FV = scr('FV', [NT, 512], BF16); DV = scr('DV', [NT, 512], BF16)
    DQ = scr('DQ', [8, 64, NT], BF16); DK = scr('DK', [8, 64, NT], BF16)
    GA = scr('GA', [D, NT], BF16); GB = scr('GB', [D, NT], BF16)
    YA = scr('YA', [512, NT], BF16); YB = scr('YB', [512, NT], BF16)
    HM = scr('HM', [NT, D], BF16); X2 = scr('X2', [NT, D], F32)
    XS = scr('XS', [NROWS, D], BF16); YS = scr('YS', [NROWS, D], BF16)
    RT = scr('RT', [NT, 8], F32)

    with contextlib.ExitStack() as top:
        S = Sched(nc, top)

        pfx_ctr = [0]

        def mk(es):
            pfx_ctr[0] += 1
            pfx = f"ph{pfx_ctr[0]}_"

            def sb(n, shp, dt):
                return Buf(es.enter_context(nc.sbuf_tensor(pfx + n, shp, dt)))

            def ps(n, shp, dt):
                return Buf(es.enter_context(nc.psum_tensor(pfx + n, shp, dt)))
            return sb, ps

        def rms_tok(xb, gb, hb, junk, ss, rstd, eps_t):
            S.op('dve', TTR(junk.t[:, :], xb.t[:, :], xb.t[:, :], ss.t[:, 0:1]), reads=[xb], writes=[junk, ss])
            S.op('act', ACTF(rstd.t[:, 0:1], ss.t[:, 0:1], AF.Ln, scale=1.0 / D, bias=eps_t.t[:, 0:1]), reads=[ss, eps_t], writes=[rstd])
            S.op('act', ACTF(rstd.t[:, 0:1], rstd.t[:, 0:1], AF.Exp, scale=-0.5), reads=[rstd], writes=[rstd])
            S.op('dve', STT(hb.t[:, :], xb.t[:, :], rstd.t[:, 0:1], gb.t[:, :], ALU.mult, ALU.mult), reads=[xb, rstd, gb], writes=[hb])

        with contextlib.ExitStack() as es:
            sb, ps = mk(es)
            S.begin_phase('p1')
            w = [sb(f'w_in{c}', [128, INW], BF16) for c in range(8)]
            for c in range(8):
                S.dma('pool', DMA(w[c].t[:, :], io['w_in'][c * 128:(c + 1) * 128, :]), writes=[w[c]])
            gmix = sb('gmix', [128, D], F32)
            S.dma('sp', DMA(gmix.t[:, :], io['g_mix'].partition_broadcast(128)), writes=[gmix])
            bfg = sb('bfg', [128, 8], F32)
            S.dma('sp', DMA(bfg.t[:, :], io['b_fgate'].partition_broadcast(128)), writes=[bfg])
            identb = sb('identb', [128, 128], BF16)
            S.dma('sp', DMA(identb.t[:, :], io['c_ident_bf']), writes=[identb])
            identf = sb('identf', [128, 128], F32)
            S.dma('sp', DMA(identf.t[:, :], io['c_ident_f']), writes=[identf])
            invf = sb('invf', [64, 1], F32)
            S.dma('sp', DMA(invf.t[:, :], io['c_invf']), writes=[invf])
            eps_t = sb('eps_t', [128, 1], F32)
            S.op('dve', MSET(eps_t.t[:, :], 1e-6), writes=[eps_t])
            one_t = sb('one_t', [128, 1], F32)
            S.op('dve', MSET(one_t.t[:, :], 1.0), writes=[one_t])
            posi = sb('posi', [64, 512], I32)
            ang = sb('ang', [64, 512], F32)
            tk = sb('tk', [64, 512], I32)
            tf = sb('tf', [64, 512], F32)
            cosT = sb('cosT', [64, 512], F32)
            sinT = sb('sinT', [64, 512], F32)
            logfT = sb('logfT', [8, SEQ], F32)
            cc = sb('cc', [8, 1024], F32)
            carry = sb('carry', [8, 1], F32)
            onesf8 = sb('onesf8', [8, 1024], F32)
            S.op('pool', MSET(onesf8.t[:, :], 1.0), writes=[onesf8])
            onesb8 = sb('onesb8', [8, 1024], BF16)
            S.op('pool', MSET(onesb8.t[:, :], 1.0), writes=[onesb8])
            cparts = [sb(f'cp{i}', [8, 1024], BF16) for i in range(3)]
            nparts = [sb(f'np{i}', [8, 1024], BF16) for i in range(3)]
            cr = sb('cr', [8, 1024], F32)
            c8 = sb('c8', [8, 1024], F32)
            xt = [sb(f'xt{i}', [128, D], F32) for i in range(2)]
            junk = sb('junk', [128, D], BF16)
            ss = sb('ss', [128, 1], F32)
            rstd = sb('rstd', [128, 1], F32)
            hb = [sb(f'hb{i}', [128, D], BF16) for i in range(2)]
            hT = [sb(f'hT{i}', [128, 8, 512], BF16) for i in range(2)]
            tp = [ps(f'tp{i}', [128, D], BF16) for i in range(2)]
            pt = [ps(f'pt{i}', [128, 512], F32) for i in range(2)]
            pf = [ps(f'pf{i}', [128, 512], F32) for i in range(2)]
            psm = ps('psm', [128, 512], F32)
            vsb = [sb(f'vsb{i}', [128, 512], BF16) for i in range(2)]
            lf = sb('lf', [128, 8], F32)
            lf2 = sb('lf2', [128, 8], F32)
            fqs = [sb(f'fqs{i}', [64, 8, 512], BF16) for i in range(2)]
            gsb = [sb('gsb0', [128, 8, 512], BF16)] * 2
            ra = sb('ra', [64, 512], F32)
            rb = sb('rb', [64, 512], F32)

            def sincos(dst, shift):
                S.op('dve', TS(tf.t[:, :], ang.t[:, :], 1.0 / (2 * math.pi), 0.5 + shift, ALU.mult, ALU.add), reads=[ang], writes=[tf])
                S.op('dve', CP(tk.t[:, :], tf.t[:, :]), reads=[tf], writes=[tk])
                S.op('dve', CP(dst.t[:, :], tk.t[:, :]), reads=[tk], writes=[dst])
                S.op('dve', TT(tf.t[:, :], tf.t[:, :], dst.t[:, :], ALU.subtract), reads=[tf, dst], writes=[tf])
                S.op('dve', TS(dst.t[:, :], tf.t[:, :], 0.0, None, ALU.is_lt), reads=[tf], writes=[dst])
                S.op('dve', TT(tf.t[:, :], tf.t[:, :], dst.t[:, :], ALU.add), reads=[tf, dst], writes=[tf])
                S.op('dve', TS(tf.t[:, :], tf.t[:, :], -0.5, 2 * math.pi, ALU.add, ALU.mult), reads=[tf], writes=[tf])
                S.op('dve', TS(tf.t[:, :], tf.t[:, :], -3.14159, 3.14159, ALU.max, ALU.min), reads=[tf], writes=[tf])
                S.op('act', ACTF(dst.t[:, :], tf.t[:, :], AF.Sin), reads=[tf], writes=[dst])

            cosT2 = [cosT, sb('cosTb', [64, 512], F32)]
            sinT2 = [sinT, sb('sinTb', [64, 512], F32)]
            NST = SEQ // 512
            sts = [(seq, st) for seq in range(NSEQ) for st in range(NST)]

            def a_parts(gi):
                seq, st = sts[gi]
                T0 = seq * SEQ + st * 512
                hp = hT[gi % 2]
                cT = cosT2[gi % 2]; sT = sinT2[gi % 2]
                parts = []

                def p_sincos():
                    S.dma('sp', DMA(posi.t[:, :], io['positions'][seq, st * 512:(st + 1) * 512].partition_broadcast(64)), writes=[posi])
                    S.op('dve', CP(ang.t[:, :], posi.t[:, :]), reads=[posi], writes=[ang])
                    S.op('dve', TS(ang.t[:, :], ang.t[:, :], invf.t[:, 0:1], None, ALU.mult), reads=[ang, invf], writes=[ang])
                    sincos(sT, 0.0)
                    sincos(cT, 0.25)
                parts.append(p_sincos)
                for sub in range(4):
                    it = gi * 4 + sub
                    xb = xt[it % 2]; hbb = hb[it % 2]; tpp = tp[it % 2]
                    r0 = T0 + sub * 128

                    def p_main(sub=sub, xb=xb, hbb=hbb, tpp=tpp, r0=r0):
                        S.dma('sp', DMA(xb.t[:, :], io['x'][r0:r0 + 128, :]), writes=[xb])
                        rms_tok(xb, gmix, hbb, junk, ss, rstd, eps_t)
                        S.op('pe', [TR(tpp.t[:, c * 128:(c + 1) * 128], hbb.t[:, c * 128:(c + 1) * 128], identb.t[:, :]) for c in range(8)],
                             reads=[hbb, identb], writes=[tpp])
                        S.op('act', CP(hp.t[:, :, sub * 128:(sub + 1) * 128], tpp.t[:, :].rearrange("p (c t) -> p c t", c=8)), reads=[tpp], pwrites=[hp])
                        for k, (col, dst) in enumerate(((C_FV, FV), (C_DV, DV))):
                            pp = pt[k]; vb = vsb[k]
                            S.op('pe', [MM(pp.t[:, :], hp.t[:, c, sub * 128:(sub + 1) * 128], w[c].t[:, col:col + 512], c == 0, c == 7) for c in range(8)],
                                 reads=[hp] + w, writes=[pp])
                            S.op('act', CP(vb.t[:, :], pp.t[:, :]), reads=[pp], writes=[vb])
                            S.dma('pool', DMA(dst[r0:r0 + 128, :], vb.t[:, :]), reads=[vb])
                        S.op('pe', [MM(psm.t[:, 0:8], hp.t[:, c, sub * 128:(sub + 1) * 128], w[c].t[:, C_FL:C_FL + 8], c == 0, c == 7) for c in range(8)],
                             reads=[hp] + w, writes=[psm])
                        S.op('dve', TT(lf.t[:, :], psm.t[:, 0:8], bfg.t[:, :], ALU.add), reads=[psm, bfg], writes=[lf])
                        S.op('act', ACTF(lf.t[:, :], lf.t[:, :], AF.Exp, scale=-1.0), reads=[lf], writes=[lf])
                        S.op('act', ACTF(lf.t[:, :], lf.t[:, :], AF.Ln, bias=one_t.t[:, 0:1]), reads=[lf, one_t], writes=[lf])
                        S.op('dve', TS(lf2.t[:, :], lf.t[:, :], -1.0, None, ALU.mult), reads=[lf], writes=[lf2])

                    def p_tail(sub=sub):
                        S.op('pe', TR(psm.t[0:8, 128:256], lf2.t[:, :], identf.t[:, :]), reads=[lf2, identf], writes=[psm])
                        S.op('dve', CP(logfT.t[:, st * 512 + sub * 128: st * 512 + (sub + 1) * 128], psm.t[0:8, 128:256]), reads=[psm], pwrites=[logfT])
                    parts.append(p_main)
                    parts.append(p_tail)
                return parts

            def b_parts(gi):
                seq, st = sts[gi]
                T0 = seq * SEQ + st * 512
                hall = hT[gi % 2]
                cT = cosT2[gi % 2]; sT = sinT2[gi % 2]
                parts = []
                fi = [0]

                def mm_group(pp, rows, col, width):
                    S.op('pe', [MM(pp.t[0:rows, :], w[c].t[:, col: col + width], hall.t[:, c, :], c == 0, c == 7) for c in range(8)],
                         reads=[hall] + w, writes=[pp])

                for k, (col, dst) in enumerate(((C_FQ, FQ), (C_FK, FK))):
                    fb = fqs[k]
                    for hh in range(8):
                        def g(hh=hh, fb=fb, col=col, dst=dst):
                            pp = pf[fi[0] % 2]; fi[0] += 1
                            mm_group(pp, 64, col + hh * 64, 64)
                            S.op('act', CP(fb.t[:, hh, :], pp.t[0:64, :]), reads=[pp], pwrites=[fb])
                            if hh == 7:
                                S.dma('pool', DMA(dst[:, 0:64, T0:T0 + 512].rearrange("h r t -> r h t"), fb.t[:, :, :]), reads=[fb])
                        parts.append(g)
                for k, (col, dst) in enumerate(((C_DQ, DQ), (C_DK, DK))):
                    fb = fqs[k]
                    for j in range(8):
                        def g(j=j, fb=fb, col=col, dst=dst):
                            pp = pf[fi[0] % 2]; fi[0] += 1
                            mm_group(pp, 64, col + j * 64, 64)
                            S.op('dve', TT(ra.t[0:32, :], pp.t[0:32, :], cT.t[0:32, :], ALU.mult), reads=[pp, cT], pwrites=[ra])
                            S.op('dve', TT(rb.t[0:32, :], pp.t[32:64, :], sT.t[0:32, :], ALU.mult), reads=[pp, sT], pwrites=[rb])
                            S.op('dve', TT(ra.t[32:64, :], pp.t[32:64, :], cT.t[32:64, :], ALU.mult), reads=[pp, cT], pwrites=[ra])
                            S.op('dve', TT(rb.t[32:64, :], pp.t[0:32, :], sT.t[32:64, :], ALU.mult), reads=[pp, sT], pwrites=[rb])
                            S.op('pool', TT(fb.t[0:32, j, :], ra.t[0:32, :], rb.t[0:32, :], ALU.subtract), reads=[ra, rb], pwrites=[fb])
                            S.op('pool', TT(fb.t[32:64, j, :], ra.t[32:64, :], rb.t[32:64, :], ALU.add), reads=[ra, rb], pwrites=[fb])
                            if j == 7:
                                S.dma('pool', DMA(dst[:, :, T0:T0 + 512].rearrange("h r t -> r h t"), fb.t[:, :, :]), reads=[fb])
                        parts.append(g)
                for k, (col, dst) in enumerate(((C_GA, GA), (C_GB, GB))):
                    gb_ = gsb[k]
                    for n in range(8):
                        def g(n=n, gb_=gb_, col=col, dst=dst):
                            pp = pf[fi[0] % 2]; fi[0] += 1
                            mm_group(pp, 128, col + n * 128, 128)
                            S.op('act', ACTF(gb_.t[:, n, :], pp.t[:, :], AF.Sigmoid), reads=[pp], pwrites=[gb_])
                            if n == 7:
                                S.dma('pool', DMA(dst[:, T0:T0 + 512].rearrange("(n p) t -> p n t", p=128), gb_.t[:, :, :]), reads=[gb_])
                        parts.append(g)
                return parts

            def scan_seq(seq):
                for ch in range(4):
                    lsl = slice(ch * 1024, (ch + 1) * 1024)
                    init = 0.0 if ch == 0 else carry.t[:, 0:1]
                    S.op('dve', (lambda e, lsl=lsl, init=init: e.tensor_tensor_scan(out=cc.t[:, :], data0=onesf8.t[:, :], data1=logfT.t[:, lsl], initial=init, op0=ALU.mult, op1=ALU.add)),
                         reads=[onesf8, logfT, carry], writes=[cc])
                    S.op('dve', CP(carry.t[:, 0:1], cc.t[:, 1023:1024]), reads=[cc], writes=[carry])
                    S.op('dve', TS(c8.t[:, :], cc.t[:, :], 8.0, None, ALU.mult), reads=[cc], writes=[c8])
                    src = c8
                    for i in range(3):
                        S.op('dve', CP(cparts[i].t[:, :], src.t[:, :]), reads=[src], writes=[cparts[i]])
                        S.op('dve', TS(nparts[i].t[:, :], cparts[i].t[:, :], -1.0, None, ALU.mult), reads=[cparts[i]], writes=[nparts[i]])
                        if i < 2:
                            S.op('dve', TT(cr.t[:, :], src.t[:, :], cparts[i].t[:, :], ALU.subtract), reads=[src, cparts[i]], writes=[cr])
                            src = cr
                    sl = slice(seq * SEQ + ch * 1024, seq * SEQ + (ch + 1) * 1024)
                    for i in range(3):
                        S.dma('pool', DMA(FQ[:, 64 + i, sl], cparts[i].t[:, :]), reads=[cparts[i]])
                        S.dma('pool', DMA(FQ[:, 67 + i, sl], onesb8.t[:, :]), reads=[onesb8])
                        S.dma('pool', DMA(FK[:, 64 + i, sl], onesb8.t[:, :]), reads=[onesb8])
                        S.dma('pool', DMA(FK[:, 67 + i, sl], nparts[i].t[:, :]), reads=[nparts[i]])

            for p in a_parts(0):
                p()
            for gi in range(len(sts)):
                bl = b_parts(gi)
                seq, st = sts[gi]
                al = a_parts(gi + 1) if gi + 1 < len(sts) else []
                if st == NST - 1:
                    bl[0](); bl[1]()
                    scan_seq(seq)
                    bl = bl[2:]
                step = max(1, len(bl) // max(1, len(al))) if al else len(bl)
                ai = 0
                for bi, g in enumerate(bl):
                    g()
                    if al and (bi + 1) % step == 0 and ai < len(al):
                        al[ai](); ai += 1
                while ai < len(al):
                    al[ai](); ai += 1
            S.end_phase()

        with contextlib.ExitStack() as es:
            sb, ps = mk(es)
            S.begin_phase('p2a')
            identb = sb('identb', [128, 128], BF16)
            S.dma('sp', DMA(identb.t[:, :], io['c_ident_bf']), writes=[identb])
            maskb = sb('maskb', [128, 128], BF16)
            S.dma('sp', DMA(maskb.t[:, :], io['c_mask']), writes=[maskb])
            vaug = sb('vaug', [128, 32, 8, 128], BF16)
            S.op('pool', MSET(vaug.t[:, :, :, :], 1.0), writes=[vaug])
            qT = [sb(f'qT{i}', [70, SEQ], BF16) for i in range(2)]
            kT = [sb(f'kT{i}', [70, SEQ], BF16) for i in range(2)]
            sp_ = [ps(f'sp{i}', [128, 512], F32) for i in range(3)]
            op_ = [ps(f'op{i}', [128, 512], F32) for i in range(2)]
            pT = [sb(f'pT{i}', [128, 512], BF16) for i in range(4)]
            rec = sb('rec', [128, 512], F32)
            yab = [sb(f'yab{i}', [64, 512], BF16) for i in range(2)]
            LOOK = 2
            items = []
            n_o = 0
            heads = [(seq, h) for seq in range(NSEQ) for h in range(8)]
            for hi, (seq, h) in enumerate(heads):
                for j in range(8):
                    last = 4 * j + 3
                    for i in range(last + 1):
                        items.append(dict(hi=hi, seq=seq, h=h, j=j, i=i, last=last, ob=n_o % 2, first=(j == 0 and i == 0)))
                    n_o += 1

            def load_v(seq):
                tb = seq * SEQ
                for t in range(32):
                    S.dma('sp', DMA(vaug.t[:, t, :, 0:64], FV[tb + t * 128: tb + (t + 1) * 128, :].rearrange("s (h d) -> s h d", d=64)),
                          pwrites=[vaug])

            def load_head(hi):
                seq, h = heads[hi]
                tb = seq * SEQ
                q = qT[hi % 2]; k_ = kT[hi % 2]
                S.dma('sp', DMA(q.t[:, :], FQ[h, :, tb:tb + SEQ]), writes=[q])
                S.dma('sp', DMA(k_.t[:, :], FK[h, :, tb:tb + SEQ]), writes=[k_])

            def emit_s(n):
                it = items[n]
                if it['first']:
                    if it['hi'] == 0:
                        load_v(0)
                        load_head(0)
                    if it['hi'] + 1 < len(heads):
                        load_head(it['hi'] + 1)
                q = qT[it['hi'] % 2]; k_ = kT[it['hi'] % 2]
                i, j = it['i'], it['j']
                off = max(0, i - 4 * j) * 128
                diag = i >= 4 * j
                spb = sp_[n % len(sp_)]
                fns = [MM(spb.t[:, off:512], k_.t[0:70, i * 128:(i + 1) * 128], q.t[0:70, j * 512 + off:(j + 1) * 512], True, not diag)]
                if diag:
                    fns.append(MM(spb.t[:, off:off + 128], identb.t[:, :], maskb.t[:, :], False, True))
                S.op('pe', fns, reads=[k_, q, identb, maskb], writes=[spb])

            def emit_rest(n):
                it = items[n]
                i, j, h, seq = it['i'], it['j'], it['h'], it['seq']
                tb = seq * SEQ
                off = max(0, i - 4 * j) * 128
                spb = sp_[n % len(sp_)]; pb = pT[n % len(pT)]; ob = op_[it['ob']]
                S.op('act', ACTF(pb.t[:, off:512], spb.t[:, off:512], AF.Exp, scale=0.125), reads=[spb], writes=[pb])
                S.op('pe', MM(ob.t[:, off:512], vaug.t[:, i, h, :], pb.t[:, off:512], i == 0, i == it['last']),
                     reads=[vaug, pb], writes=[ob] if i == 0 else [], pwrites=[] if i == 0 else [ob])
                if i == it['last']:
                    yb = yab[it['ob']]
                    S.op('dve', RCP(rec.t[64:128, :], ob.t[64:128, :]), reads=[ob], writes=[rec])
                    S.op('dve', TT(yb.t[0:64, :], ob.t[0:64, :], rec.t[64:128, :], ALU.mult), reads=[ob, rec], writes=[yb])
                    S.dma('pool', DMA(YA[h * 64:(h + 1) * 64, tb + j * 512: tb + (j + 1) * 512], yb.t[:, :]), reads=[yb])
                    if j == 7 and h == 7 and it['hi'] + 1 < len(heads):
                        load_v(seq + 1)

            for n in range(min(LOOK, len(items))):
                emit_s(n)
            for n in range(len(items)):
                if n + LOOK < len(items):
                    nxt = items[n + LOOK]
                    emit_s(n + LOOK)
                emit_rest(n)
            S.end_phase()

        with contextlib.ExitStack() as es:
            sb, ps = mk(es)
            S.begin_phase('p2b')
            identb = sb('identb', [128, 128], BF16)
            S.dma('sp', DMA(identb.t[:, :], io['c_ident_bf']), writes=[identb])
            maskb = sb('maskb', [128, 128], BF16)
            S.dma('sp', DMA(maskb.t[:, :], io['c_mask']), writes=[maskb])
            onesb = sb('onesb', [128, 128], BF16)
            S.op('pool', MSET(onesb.t[:, :], 1.0), writes=[onesb])
            lqa = sb('lqa', [128, 64], F32); lka = sb('lka', [128, 64], F32); lj = sb('lj', [128, 64], F32)
            l1 = sb('l1', [128, 1], F32); l2 = sb('l2', [128, 1], F32); nlam = sb('nlam', [128, 1], F32)
            for (qa, ka, dst) in (('lambda_q1', 'lambda_k1', l1), ('lambda_q2', 'lambda_k2', l2)):
                S.dma('sp', DMA(lqa.t[:, :], io[qa].partition_broadcast(128)), writes=[lqa])
                S.dma('sp', DMA(lka.t[:, :], io[ka].partition_broadcast(128)), writes=[lka])
                S.op('dve', TTR(lj.t[:, :], lqa.t[:, :], lka.t[:, :], dst.t[:, 0:1]), reads=[lqa, lka], writes=[lj, dst])
                S.op('act', ACTF(dst.t[:, 0:1], dst.t[:, 0:1], AF.Exp), reads=[dst], writes=[dst])
            S.op('dve', TT(nlam.t[:, :], l2.t[:, :], l1.t[:, :], ALU.subtract), reads=[l1, l2], writes=[nlam])
            S.op('dve', TS(nlam.t[:, :], nlam.t[:, :], -LAMBDA_INIT, None, ALU.add), reads=[nlam], writes=[nlam])
            gsub = sb('gsub', [128, 1], F32)
            S.dma('sp', DMA(gsub.t[:, :], io['g_diff_sub'].rearrange("(p o) -> p o", o=1)), writes=[gsub])
            S.op('dve', TS(gsub.t[:, :], gsub.t[:, :], 1.0 - LAMBDA_INIT, None, ALU.mult), reads=[gsub], writes=[gsub])
            eps5 = sb('eps5', [128, 1], F32)
            S.op('dve', MSET(eps5.t[:, :], 1e-5), writes=[eps5])
            dvt = sb('dvt', [128, 32, 512], BF16)
            accP = [sb(f'accP{i}', [128, 512], F32) for i in range(2)]
            onesf = sb('onesf', [128, 128], F32)
            S.op('pool', MSET(onesf.t[:, :], 1.0), writes=[onesf])
            qk = [[sb(f'qk{i}_{m}', [64, SEQ], BF16) for m in range(4)] for i in range(2)]
            sp_ = [ps(f'sp{i}', [128, 512], F32) for i in range(3)]
            OD = [ps(f'od{i}', [128, 512], F32) for i in range(4)]
            pss = ps('pss', [128, 512], F32)
            pT = [sb(f'pT{i}', [128, 512], BF16) for i in range(4)]
            r1 = sb('r1', [128, 512], F32); a1 = sb('a1', [128, 512], F32); a2 = sb('a2', [128, 512], F32)
            sq = sb('sq', [128, 512], BF16); rs = sb('rs', [128, 512], F32)
            ybb = [sb(f'ybb{i}', [128, 512], BF16) for i in range(2)]
            LOOK = 2
            items = []
            heads = [(seq, hd) for seq in range(NSEQ) for hd in range(4)]
            for hi, (seq, hd) in enumerate(heads):
                for j in range(8):
                    last = 4 * j + 3
                    for i in range(last + 1):
                        for comp in range(2):
                            items.append(dict(hi=hi, seq=seq, hd=hd, j=j, i=i, comp=comp, last=last, first=(j == 0 and i == 0 and comp == 0)))
            n_y = [0]

            def load_v(seq):
                tb = seq * SEQ
                for t4 in range(4):
                    S.dma('sp', DMA(dvt.t[:, t4 * 8:(t4 + 1) * 8, :], DV[tb + t4 * 1024: tb + (t4 + 1) * 1024, :].rearrange("(t s) d -> s t d", s=128)),
                          pwrites=[dvt])

            def load_head(hi):
                seq, hd = heads[hi]
                tb = seq * SEQ
                q1, q2, k1, k2 = qk[hi % 2]
                S.dma('sp', DMA(q1.t[:, :], DQ[hd * 2, :, tb:tb + SEQ]), writes=[q1])
                S.dma('sp', DMA(q2.t[:, :], DQ[hd * 2 + 1, :, tb:tb + SEQ]), writes=[q2])
                S.dma('sp', DMA(k1.t[:, :], DK[hd * 2, :, tb:tb + SEQ]), writes=[k1])
                S.dma('sp', DMA(k2.t[:, :], DK[hd * 2 + 1, :, tb:tb + SEQ]), writes=[k2])

            def emit_s(n):
                it = items[n]
                if it['first']:
                    if it['hi'] == 0:
                        load_v(0)
                        load_head(0)
                    if it['hi'] + 1 < len(heads):
                        load_head(it['hi'] + 1)
                q1, q2, k1, k2 = qk[it['hi'] % 2]
                qq, kk = (q1, k1) if it['comp'] == 0 else (q2, k2)
                i, j = it['i'], it['j']
                off = max(0, i - 4 * j) * 128
                diag = i >= 4 * j
                spb = sp_[n % len(sp_)]
                fns = [MM(spb.t[:, off:512], kk.t[0:64, i * 128:(i + 1) * 128], qq.t[0:64, j * 512 + off:(j + 1) * 512], True, not diag)]
                if diag:
                    fns.append(MM(spb.t[:, off:off + 128], identb.t[:, :], maskb.t[:, :], False, True))
                S.op('pe', fns, reads=[kk, qq, identb, maskb], writes=[spb])

            def emit_rest(n):
                it = items[n]
                i, j, hd, seq, comp = it['i'], it['j'], it['hd'], it['seq'], it['comp']
                tb = seq * SEQ
                off = max(0, i - 4 * j) * 128
                spb = sp_[n % len(sp_)]; pb = pT[n % len(pT)]
                S.op('act', ACTF(pb.t[:, off:512], spb.t[:, off:512], AF.Exp, scale=0.125), reads=[spb], writes=[pb])
                ob = OD[comp * 2]; db = OD[comp * 2 + 1]
                acc = accP[comp]
                eng = 'dve' if comp == 0 else 'pool'
                if i == 0:
                    S.op(eng, CP(acc.t[:, :], pb.t[:, :]), reads=[pb], writes=[acc])
                else:
                    S.op(eng, TT(acc.t[:, off:512], acc.t[:, off:512], pb.t[:, off:512], ALU.add), reads=[pb, acc], writes=[acc])
                S.op('pe', MM(ob.t[:, off:512], dvt.t[:, i, hd * 128:(hd + 1) * 128], pb.t[:, off:512], i == 0, i == it['last']),
                     reads=[dvt, pb], writes=[ob] if i == 0 else [], pwrites=[] if i == 0 else [ob])
                if i == it['last']:
                    S.op('pe', MM(db.t[:, :], onesf.t[:, :], acc.t[:, :], True, True), reads=[onesf, acc], writes=[db])
                if i == it['last'] and comp == 1:
                    S.op('dve', RCP(r1.t[:, :], OD[1].t[:, :]), reads=[OD[1]], writes=[r1])
                    S.op('dve', TT(a1.t[:, :], OD[0].t[:, :], r1.t[:, :], ALU.mult), reads=[OD[0], r1], writes=[a1])
                    S.op('dve', RCP(r1.t[:, :], OD[3].t[:, :]), reads=[OD[3]], writes=[r1])
                    S.op('dve', TT(a2.t[:, :], OD[2].t[:, :], r1.t[:, :], ALU.mult), reads=[OD[2], r1], writes=[a2])
                    S.op('dve', STT(a1.t[:, :], a2.t[:, :], nlam.t[:, 0:1], a1.t[:, :], ALU.mult, ALU.add), reads=[a2, nlam, a1], writes=[a1])
                    S.op('act', ACTF(sq.t[:, :], a1.t[:, :], AF.Square), reads=[a1], writes=[sq])
                    S.op('pe', MM(pss.t[:, :], onesb.t[:, :], sq.t[:, :], True, True), reads=[onesb, sq], writes=[pss])
                    S.op('act', ACTF(rs.t[:, :], pss.t[:, :], AF.Ln, scale=1.0 / 128, bias=eps5.t[:, 0:1]), reads=[pss, eps5], writes=[rs])
                    S.op('act', ACTF(rs.t[:, :], rs.t[:, :], AF.Exp, scale=-0.5), reads=[rs], writes=[rs])
                    yb = ybb[n_y[0] % 2]; n_y[0] += 1
                    S.op('dve', STT(yb.t[:, :], a1.t[:, :], gsub.t[:, 0:1], rs.t[:, :], ALU.mult, ALU.mult), reads=[a1, gsub, rs], writes=[yb])
                    S.dma('pool', DMA(YB[hd * 128:(hd + 1) * 128, tb + j * 512: tb + (j + 1) * 512], yb.t[:, :]), reads=[yb])
                    if j == 7 and hd == 3 and it['hi'] + 1 < len(heads):
                        load_v(seq + 1)

            for n in range(min(LOOK, len(items))):
                emit_s(n)
            for n in range(len(items)):
                if n + LOOK < len(items):
                    emit_s(n + LOOK)
                emit_rest(n)
            S.end_phase()

        with contextlib.ExitStack() as es:
            sb, ps = mk(es)
            S.begin_phase('p3')
            identb = sb('identb', [128, 128], BF16)
            S.dma('sp', DMA(identb.t[:, :], io['c_ident_bf']), writes=[identb])
            identf = sb('identf', [128, 128], F32)
            S.dma('sp', DMA(identf.t[:, :], io['c_ident_f']), writes=[identf])
            trif = sb('trif', [128, 128], F32)
            S.dma('sp', DMA(trif.t[:, :], io['c_tri']), writes=[trif])
            onesf = sb('onesf', [128, 128], F32)
            S.op('pool', MSET(onesf.t[:, :], 1.0), writes=[onesf])
            eps_t = sb('eps_t', [128, 1], F32)
            S.op('dve', MSET(eps_t.t[:, :], 1e-6), writes=[eps_t])
            wa = sb('wa', [128, 4, D], BF16); wb = sb('wb', [128, 4, D], BF16); wo = sb('wo', [128, 8, D], BF16)
            wcq = sb('wcq', [128, 8, 256], BF16); wckv = sb('wckv', [128, 8, 512], BF16); wco = sb('wco', [64, 4, D], BF16)
            S.dma('pool', DMA(wa.t[:, :, :], io['w_branch_a'].rearrange("(c p) n -> p c n", p=128)), writes=[wa])
            S.dma('pool', DMA(wb.t[:, :, :], io['w_branch_b'].rearrange("(c p) n -> p c n", p=128)), writes=[wb])
            S.dma('pool', DMA(wo.t[:, :, :], io['w_out'].rearrange("(c p) n -> p c n", p=128)), writes=[wo])
            S.dma('pool', DMA(wcq.t[:, :, :], io['w_cq'].rearrange("(c p) n -> p c n", p=128)), writes=[wcq])
            S.dma('pool', DMA(wckv.t[:, :, :], io['w_ckv'].rearrange("(c p) n -> p c n", p=128)), writes=[wckv])
            S.dma('pool', DMA(wco.t[:, :, :], io['w_co'].rearrange("(h d) n -> d h n", d=64)), writes=[wco])
            wr = sb('wr', [128, 8, 36], F32)
            S.dma('sp', DMA(wr.t[:, :, 0:4], io['w_group'].rearrange("(c p) n -> p c n", p=128)), pwrites=[wr])
            S.dma('sp', DMA(wr.t[:, :, 4:36], io['w_expert'].rearrange("(c p) n -> p c n", p=128)), pwrites=[wr])
            brt = sb('brt', [128, 36], F32)
            S.dma('sp', DMA(brt.t[:, 0:4], io['b_group'].partition_broadcast(128)), pwrites=[brt])
            S.dma('sp', DMA(brt.t[:, 4:36], io['b_expert'].partition_broadcast(128)), pwrites=[brt])
            gcross = sb('gcross', [128, D], F32); gmem = sb('gmem', [128, D], F32); gffn = sb('gffn', [128, D], F32)
            S.dma('sp', DMA(gcross.t[:, :], io['g_cross'].partition_broadcast(128)), writes=[gcross])
            S.dma('sp', DMA(gmem.t[:, :], io['g_mem'].partition_broadcast(128)), writes=[gmem])
            S.dma('sp', DMA(gffn.t[:, :], io['g_ffn'].partition_broadcast(128)), writes=[gffn])
            kcT = sb('kcT', [64, NSEQ, 4, 256], BF16)
            vca = sb('vca', [128, NSEQ, 2, 4, 128], BF16)
            S.op('pool', MSET(vca.t[:, :, :, :, :], 1.0), writes=[vca])
            xt = [sb(f'xt{i}', [128, D], F32) for i in range(2)]
            x1 = sb('x1', [128, D], F32)
            x2 = [sb(f'x2_{i}', [128, D], F32) for i in range(2)]
            junk = sb('junk', [128, D], BF16)
            ss = sb('ss', [128, 1], F32); rstd = sb('rstd', [128, 1], F32)
            hxb = sb('hxb', [128, D], BF16)
            hmf = sb('hmf', [128, D], F32)
            hmb = [sb(f'hmb{i}', [128, D], BF16) for i in range(2)]
            hxT = [sb(f'hxT{s}', [128, 8, 128], BF16) for s in range(4)]
            hmT = sb('hmT', [128, 8, 128], F32)
            yaT = sb('yaT', [128, 4, 512], BF16); ybT = sb('ybT', [128, 4, 512], BF16)
            gaT = sb('gaT', [128, 8, 512], BF16); gbT = sb('gbT', [128, 8, 512], BF16)
            mT = sb('mT', [128, 8, 512], BF16)
            t1 = sb('t1', [128, 512], F32); t2 = sb('t2', [128, 512], F32)
            qcs = sb('qcs', [64, 4, 512], BF16)
            ycs = sb('ycs', [64, 4, 512], BF16)
            pT = [sb(f'pT{i}', [128, 512], BF16) for i in range(2)]
            rec = sb('rec', [128, 512], F32)
            pA = ps('pA', [128, 512], F32); pB = ps('pB', [128, 512], F32)
            pt = [ps(f'pt{i}', [128, 512], F32) for i in range(2)]
            tp = ps('tp', [128, D], BF16)
            pq = ps('pq', [128, 512], F32)
            spc = ps('spc', [128, 512], F32)
            opc = ps('opc', [128, 512], F32)
            lg = sb('lg', [128, 36], F32)
            gmax = sb('gmax', [128, 1], F32); gmask = sb('gmask', [128, 4], F32); ge = sb('ge', [128, 4], F32)
            gs = sb('gs', [128, 1], F32); gw = sb('gw', [128, 1], F32)
            sel = sb('sel', [128, 8], F32); top8 = sb('top8', [128, 8], F32)
            m1 = sb('m1', [128, 8], F32); m2 = sb('m2', [128, 8], F32)
            dw = sb('dw', [128, 1], F32)
            oh1 = sb('oh1', [128, 64, 32], F32); oh2 = sb('oh2', [128, 64, 32], F32)
            ohs = sb('ohs', [128, 32], F32); cum = sb('cum', [128, 32], F32); rk = sb('rk', [128, 32], F32)
            S.op('pool', MSET(cum.t[:, :], 0.0), writes=[cum])
            pos = sb('pos', [128, 64, 2], F32); wts = sb('wts', [128, 64, 2], F32)
            j32 = sb('j32', [128, 32], F32)

            for seq in range(NSEQ):
                for mt in range(2):
                    xb = xt[mt]
                    S.dma('sp', DMA(xb.t[:, :], io['mem'][seq * 256 + mt * 128: seq * 256 + (mt + 1) * 128, :]), writes=[xb])
                    rms_tok(xb, gmem, hxb, junk, ss, rstd, eps_t)
                    S.op('pe', [TR(tp.t[:, c * 128:(c + 1) * 128], hxb.t[:, c * 128:(c + 1) * 128], identb.t[:, :]) for c in range(8)],
                         reads=[hxb, identb], writes=[tp])
                    S.op('act', CP(hxT[mt].t[:, :, :], tp.t[:, :].rearrange("p (c t) -> p c t", c=8)), reads=[tp], writes=[hxT[mt]])
                    S.op('pe', [MM(pt[0].t[:, 0:256], hxT[mt].t[:, c, :], wckv.t[:, c, 256:512], c == 0, c == 7) for c in range(8)],
                         reads=[hxT[mt], wckv], writes=[pt[0]])
                    S.op('act', CP(vca.t[:, seq, mt, :, 0:64], pt[0].t[:, 0:256].rearrange("p (h d) -> p h d", d=64)), reads=[pt[0]], pwrites=[vca])
                for hh in range(4):
                    for mt in range(2):
                        S.op('pe', [MM(pq.t[0:64, mt * 128:(mt + 1) * 128], wckv.t[:, c, hh * 64:(hh + 1) * 64], hxT[mt].t[:, c, :], c == 0, c == 7) for c in range(8)],
                             reads=[hxT[mt], wckv], pwrites=[pq])
                    S.op('act', CP(kcT.t[:, seq, hh, :], pq.t[0:64, 0:256]), reads=[pq], pwrites=[kcT])

            it = 0
            for seq in range(NSEQ):
                for st in range(SEQ // 512):
                    T0 = seq * SEQ + st * 512
                    S.dma('sp', DMA(yaT.t[:, :, :], YA[:, T0:T0 + 512].rearrange("(c p) t -> p c t", p=128)), writes=[yaT])
                    S.dma('sp', DMA(ybT.t[:, :, :], YB[:, T0:T0 + 512].rearrange("(c p) t -> p c t", p=128)), writes=[ybT])
                    S.dma('sp', DMA(gaT.t[:, :, :], GA[:, T0:T0 + 512].rearrange("(c p) t -> p c t", p=128)), writes=[gaT])
                    S.dma('sp', DMA(gbT.t[:, :, :], GB[:, T0:T0 + 512].rearrange("(c p) t -> p c t", p=128)), writes=[gbT])
                    for n in range(8):
                        S.op('pe', [MM(pA.t[:, :], wa.t[:, c, n * 128:(n + 1) * 128], yaT.t[:, c, :], c == 0, c == 3) for c in range(4)],
                             reads=[wa, yaT], writes=[pA])
                        S.op('pe', [MM(pB.t[:, :], wb.t[:, c, n * 128:(n + 1) * 128], ybT.t[:, c, :], c == 0, c == 3) for c in range(4)],
                             reads=[wb, ybT], writes=[pB])
                        S.op('dve', TT(t1.t[:, :], pA.t[:, :], gaT.t[:, n, :], ALU.mult), reads=[pA, gaT], writes=[t1])
                        S.op('dve', TT(t2.t[:, :], pB.t[:, :], gbT.t[:, n, :], ALU.mult), reads=[pB, gbT], writes=[t2])
                        S.op('pool', TT(mT.t[:, n, :], t1.t[:, :], t2.t[:, :], ALU.add), reads=[t1, t2], pwrites=[mT])
                    for sub in range(4):
                        r0 = T0 + sub * 128
                        xb = xt[it % 2]
                        S.dma('sp', DMA(xb.t[:, :], io['x'][r0:r0 + 128, :]), writes=[xb])
                        for half in range(2):
                            pp = pt[half]
                            S.op('pe', [MM(pp.t[:, :], mT.t[:, n, sub * 128:(sub + 1) * 128], wo.t[:, n, half * 512:(half + 1) * 512], n == 0, n == 7) for n in range(8)],
                                 reads=[mT, wo], writes=[pp])
                            S.op('dve', TT(x1.t[:, half * 512:(half + 1) * 512], pp.t[:, :], xb.t[:, half * 512:(half + 1) * 512], ALU.add),
                                 reads=[pp, xb], pwrites=[x1])
                        rms_tok(x1, gcross, hxb, junk, ss, rstd, eps_t)
                        S.op('pe', [TR(tp.t[:, c * 128:(c + 1) * 128], hxb.t[:, c * 128:(c + 1) * 128], identb.t[:, :]) for c in range(8)],
                             reads=[hxb, identb], writes=[tp])
                        S.op('act', CP(hxT[sub].t[:, :, :], tp.t[:, :].rearrange("p (c t) -> p c t", c=8)), reads=[tp], writes=[hxT[sub]])
                        for hh in range(4):
                            S.op('pe', [MM(pq.t[0:64, hh * 128:(hh + 1) * 128], wcq.t[:, c, hh * 64:(hh + 1) * 64], hxT[sub].t[:, c, :], c == 0, c == 7) for c in range(8)],
                                 reads=[hxT[sub], wcq], pwrites=[pq])
                        S.op('act', CP(qcs.t[:, :, 0:128], pq.t[0:64, :].rearrange("p (h t) -> p h t", h=4)), reads=[pq], writes=[qcs])
                        for hh in range(4):
                            for mt in range(2):
                                pb = pT[mt]
                                S.op('pe', MM(spc.t[:, mt * 128:(mt + 1) * 128], kcT.t[0:64, seq, hh, mt * 128:(mt + 1) * 128], qcs.t[0:64, hh, 0:128], True, True),
                                     reads=[kcT, qcs], writes=[spc] if mt == 0 else [], pwrites=[] if mt == 0 else [spc])
                            S.op('act', ACTF(pT[0].t[:, 0:256], spc.t[:, 0:256], AF.Exp, scale=0.125), reads=[spc], writes=[pT[0]])
                            S.op('pe', [MM(opc.t[:, hh * 128:(hh + 1) * 128], vca.t[:, seq, mt, hh, :], pT[0].t[:, mt * 128:(mt + 1) * 128], mt == 0, mt == 1) for mt in range(2)],
                                 reads=[vca, pT[0]], pwrites=[opc])
                        S.op('dve', RCP(rec.t[64:128, :], opc.t[64:128, :]), reads=[opc], writes=[rec])
                        S.op('dve', TT(ycs.t[0:64, :, 0:128], opc.t[0:64, :].rearrange("p (h t) -> p h t", h=4),
                                       rec.t[64:128, :].rearrange("p (h t) -> p h t", h=4), ALU.mult), reads=[opc, rec], writes=[ycs])
                        xo = x2[it % 2]
                        for half in range(2):
                            pp = pt[half]
                            S.op('pe', [MM(pp.t[:, :], ycs.t[0:64, hh, 0:128], wco.t[0:64, hh, half * 512:(half + 1) * 512], hh == 0, hh == 3) for hh in range(4)],
                                 reads=[ycs, wco], writes=[pp])
                            S.op('dve', TT(xo.t[:, half * 512:(half + 1) * 512], pp.t[:, :], x1.t[:, half * 512:(half + 1) * 512], ALU.add),
                                 reads=[pp, x1], pwrites=[xo])
                        S.dma('pool', DMA(X2[r0:r0 + 128, :], xo.t[:, :]), reads=[xo])
                        rms_tok(xo, gffn, hmf, junk, ss, rstd, eps_t)
                        hb_ = hmb[it % 2]
                        S.op('act', CP(hb_.t[:, :], hmf.t[:, :]), reads=[hmf], writes=[hb_])
                        S.dma('pool', DMA(HM[r0:r0 + 128, :], hb_.t[:, :]), reads=[hb_])
                        for half in range(2):
                            pp = pt[half]
                            S.op('pe', [TR(pp.t[:, cq * 128:(cq + 1) * 128], hmf.t[:, (half * 4 + cq) * 128:(half * 4 + cq + 1) * 128], identf.t[:, :]) for cq in range(4)],
                                 reads=[hmf, identf], writes=[pp])
                            S.op('act', CP(hmT.t[:, half * 4:(half + 1) * 4, :], pp.t[:, :].rearrange("p (c t) -> p c t", c=4)), reads=[pp], pwrites=[hmT])
                        S.op('pe', [MM(pq.t[:, 0:36], hmT.t[:, c, :], wr.t[:, c, :], c == 0, c == 7) for c in range(8)], reads=[hmT, wr], writes=[pq])
                        S.op('dve', TT(lg.t[:, :], pq.t[:, 0:36], brt.t[:, :], ALU.add), reads=[pq, brt], writes=[lg])
                        ti = it
                        S.op('dve', lambda e: e.tensor_reduce(out=gmax.t[:, 0:1], in_=lg.t[:, 0:4], axis=AX.X, op=ALU.max), reads=[lg], writes=[gmax])
                        S.op('dve', TS(gmask.t[:, :], lg.t[:, 0:4], gmax.t[:, 0:1], None, ALU.is_equal), reads=[lg, gmax], writes=[gmask])
                        S.op('dve', TS(ge.t[:, :], lg.t[:, 0:4], gmax.t[:, 0:1], None, ALU.subtract), reads=[lg, gmax], writes=[ge])
                        S.op('act', ACTF(ge.t[:, :], ge.t[:, :], AF.Exp), reads=[ge], writes=[ge])
                        S.op('dve', lambda e: e.tensor_reduce(out=gs.t[:, 0:1], in_=ge.t[:, :], axis=AX.X, op=ALU.add), reads=[ge], writes=[gs])
                        S.op('dve', RCP(gw.t[:, :], gs.t[:, :]), reads=[gs], writes=[gw])
                        S.op('dve', TS(sel.t[:, :], lg.t[:, 4:12], gmask.t[:, 0:1], None, ALU.mult), reads=[lg, gmask], writes=[sel])
                        for g in range(1, 4):
                            S.op('dve', STT(sel.t[:, :], lg.t[:, 4 + g * 8: 12 + g * 8], gmask.t[:, g:g + 1], sel.t[:, :], ALU.mult, ALU.add),
                                 reads=[lg, gmask, sel], writes=[sel])
                        S.op('dve', lambda e: e.max(out=top8.t[:, :], in_=sel.t[:, :]), reads=[sel], writes=[top8])
                        S.op('dve', TS(m1.t[:, :], sel.t[:, :], top8.t[:, 0:1], None, ALU.is_equal), reads=[sel, top8], writes=[m1])
                        S.op('dve', TS(m2.t[:, :], sel.t[:, :], top8.t[:, 1:2], None, ALU.is_equal), reads=[sel, top8], writes=[m2])
                        S.op('dve', TT(dw.t[:, :], top8.t[:, 1:2], top8.t[:, 0:1], ALU.subtract), reads=[top8], writes=[dw])
                        S.op('act', ACTF(dw.t[:, :], dw.t[:, :], AF.Exp), reads=[dw], writes=[dw])
                        S.op('dve', TS(dw.t[:, :], dw.t[:, :], 1.0, None, ALU.add), reads=[dw], writes=[dw])
                        S.op('dve', RCP(dw.t[:, :], dw.t[:, :]), reads=[dw], writes=[dw])
                        S.op('dve', TT(wts.t[:, ti, 0:1], dw.t[:, :], gw.t[:, :], ALU.mult), reads=[dw, gw], pwrites=[wts])
                        S.op('dve', TT(wts.t[:, ti, 1:2], gw.t[:, :], wts.t[:, ti, 0:1], ALU.subtract), reads=[gw, wts], pwrites=[wts])
                        for (mm_, oh) in ((m1, oh1), (m2, oh2)):
                            S.op('dve', TT(oh.t[:, ti, :].rearrange("p (g e) -> p g e", g=4),
                                           gmask.t[:, :].unsqueeze(2).to_broadcast([128, 4, 8]),
                                           mm_.t[:, :].unsqueeze(1).to_broadcast([128, 4, 8]), ALU.mult), reads=[gmask, mm_], pwrites=[oh])
                        S.op('dve', TT(ohs.t[:, :], oh1.t[:, ti, :], oh2.t[:, ti, :], ALU.add), reads=[oh1, oh2], writes=[ohs])
                        S.op('pe', [MM(spc.t[:, 0:32], trif.t[:, :], ohs.t[:, :], True, False), MM(spc.t[:, 0:32], onesf.t[:, :], cum.t[:, :], False, True)],
                             reads=[trif, ohs, onesf, cum], writes=[spc])
                        S.op('dve', CP(rk.t[:, :], spc.t[:, 0:32]), reads=[spc], writes=[rk])
                        S.op('pool', TT(cum.t[:, :], cum.t[:, :], ohs.t[:, :], ALU.add), reads=[cum, ohs], writes=[cum])
                        S.op('dve', TTR(j32.t[:, :], rk.t[:, :], oh1.t[:, ti, :], pos.t[:, ti, 0:1]), reads=[rk, oh1], writes=[j32], pwrites=[pos])
                        S.op('dve', TTR(j32.t[:, :], rk.t[:, :], oh2.t[:, ti, :], pos.t[:, ti, 1:2]), reads=[rk, oh2], writes=[j32], pwrites=[pos])
                        it += 1

            cnt = sb('cnt', [128, 32], F32); pad = sb('pad', [128, 32], F32); pend = sb('pend', [128, 32], F32); pstart = sb('pstart', [128, 32], F32)
            ki = sb('ki', [128, 32], I32); kf = sb('kf', [128, 32], F32); kc = sb('kc', [128, 32], F32)
            ones32 = sb('ones32', [128, 32], F32)
            S.op('pool', MSET(ones32.t[:, :], 1.0), writes=[ones32])
            S.op('pe', MM(spc.t[:, 0:32], onesf.t[:, :], cum.t[:, :], True, True), reads=[onesf, cum], writes=[spc])
            S.op('dve', CP(cnt.t[:, :], spc.t[:, 0:32]), reads=[spc], writes=[cnt])
            S.op('dve', TS(pad.t[:, :], cnt.t[:, :], 1.0 / MB, None, ALU.mult), reads=[cnt], writes=[pad])
            S.op('dve', CP(ki.t[:, :], pad.t[:, :]), reads=[pad], writes=[ki])
            S.op('dve', CP(kf.t[:, :], ki.t[:, :]), reads=[ki], writes=[kf])
            S.op('dve', TT(kc.t[:, :], kf.t[:, :], pad.t[:, :], ALU.is_lt), reads=[kf, pad], writes=[kc])
            S.op('dve', TT(kf.t[:, :], kf.t[:, :], kc.t[:, :], ALU.add), reads=[kf, kc], writes=[kf])
            S.op('dve', TS(pad.t[:, :], kf.t[:, :], float(MB), None, ALU.mult), reads=[kf], writes=[pad])
            S.op('dve', lambda e: e.tensor_tensor_scan(out=pend.t[:, :], data0=ones32.t[:, :], data1=pad.t[:, :], initial=0.0, op0=ALU.mult, op1=ALU.add),
                 reads=[ones32, pad], writes=[pend])
            S.op('dve', TT(pstart.t[:, :], pend.t[:, :], pad.t[:, :], ALU.subtract), reads=[pend, pad], writes=[pstart])
            dst = sb('dst', [128, 64, 2], F32)
            for ti in range(64):
                for k, oh in enumerate((oh1, oh2)):
                    S.op('dve', TTR(j32.t[:, :], pstart.t[:, :], oh.t[:, ti, :], dst.t[:, ti, k:k + 1]), reads=[pstart, oh], writes=[j32], pwrites=[dst])
            S.op('dve', TT(dst.t[:, :, :], dst.t[:, :, :], pos.t[:, :, :], ALU.add), reads=[dst, pos], writes=[dst])
            rt = sb('rt', [128, 64, 8], F32)
            S.op('pool', MSET(rt.t[:, :, :], 0.0), writes=[rt])
            S.op('dve', CP(rt.t[:, :, 0:2], dst.t[:, :, :]), reads=[dst], pwrites=[rt])
            S.op('dve', CP(rt.t[:, :, 2:4], wts.t[:, :, :]), reads=[wts], pwrites=[rt])
            bst = sb('bst', [128, NB], F32)
            S.dma('sp', DMA(bst.t[:, :], io['c_bstart']), writes=[bst])
            cmpb = sb('cmpb', [128, NB, 32], F32)
            S.op('dve', TT(cmpb.t[:, :, :], pend.t[:, :].unsqueeze(1).to_broadcast([128, NB, 32]),
                           bst.t[:, :].unsqueeze(2).to_broadcast([128, NB, 32]), ALU.is_le), reads=[pend, bst], writes=[cmpb])
            bef = sb('bef', [128, NB], F32)
            S.op('dve', lambda e: e.tensor_reduce(out=bef.t[:, :], in_=cmpb.t[:, :, :], axis=AX.X, op=ALU.add), reads=[cmpb], writes=[bef])
            pio = sb('pio', [128, 1], F32)
            S.dma('sp', DMA(pio.t[:, :], io['c_piota']), writes=[pio])
            S.op('dve', TS(bef.t[:, :], bef.t[:, :], 31.0, 128.0, ALU.min, ALU.mult), reads=[bef], writes=[bef])
            S.op('dve', TS(bef.t[:, :], bef.t[:, :], pio.t[:, 0:1], None, ALU.add), reads=[bef, pio], writes=[bef])
            S.dma('pool', DMA(RT.rearrange("(t p) k -> p t k", p=128), rt.t[:, :, :]), reads=[rt])
            BE = scr('BE', [128, NB], F32)
            S.dma('pool', DMA(BE[:, :], bef.t[:, :]), reads=[bef])
            S.end_phase()

        with contextlib.ExitStack() as es:
            sb, ps = mk(es)
            S.begin_phase('p4')
            rtf = sb('rtf', [128, 64, 8], F32)
            S.dma('sp', DMA(rtf.t[:, :, :], RT.rearrange("(t p) k -> p t k", p=128)), writes=[rtf])
            dsti = sb('dsti', [128, 64, 2], I32)
            S.op('dve', CP(dsti.t[:, :, :], rtf.t[:, :, 0:2]), reads=[rtf], writes=[dsti])
            hm = [sb(f'hm{i}', [128, D], BF16) for i in range(4)]
            for ti in range(64):
                hb_ = hm[ti % 4]
                S.dma('sp', DMA(hb_.t[:, :], HM[ti * 128:(ti + 1) * 128, :]), writes=[hb_])
                for k in range(2):
                    S.dma('pool', (lambda e, hb_=hb_, ti=ti, k=k: e.indirect_dma_start(
                        out=XS[:, :], out_offset=bass.IndirectOffsetOnAxis(ap=dsti.t[:, ti, k:k + 1], axis=0),
                        in_=hb_.t[:, :], in_offset=None)), reads=[hb_, dsti])
            S.end_phase()

        with contextlib.ExitStack() as es:
            sb, ps = mk(es)
            S.begin_phase('p5')
            identb = sb('identb', [128, 128], BF16)
            S.dma('sp', DMA(identb.t[:, :], io['c_ident_bf']), writes=[identb])
            bef = sb('bef', [128, NB], F32)
            S.dma('sp', DMA(bef.t[:, :], BE[:, :]), writes=[bef])
            bei = sb('bei', [128, NB], I32)
            S.op('dve', CP(bei.t[:, :], bef.t[:, :]), reads=[bef], writes=[bei])
            wst2 = [[sb(f'wst{q}_{i}', [128, 4096], F32) for i in range(3)] for q in range(2)]
            wgb = [sb(f'wgb{i}', [128, 8, 512], BF16) for i in range(2)]
            wub = [sb(f'wub{i}', [128, 8, 512], BF16) for i in range(2)]
            wdb = [sb(f'wdb{i}', [128, 4, D], BF16) for i in range(2)]
            xs = [sb(f'xs{i}', [128, D], BF16) for i in range(4)]
            xsT = sb('xsT', [128, 8, 512], BF16)
            haT = sb('haT', [128, 4, 512], BF16)
            sg = sb('sg', [128, 512], F32)
            ysb = [sb(f'ysb{i}', [128, D], BF16) for i in range(2)]
            tp = [ps(f'tp{i}', [128, D], BF16) for i in range(2)]
            pg = [ps(f'pg{i}', [128, 512], F32) for i in range(2)]
            pu = [ps(f'pu{i}', [128, 512], F32) for i in range(2)]
            py = [ps(f'py{i}', [128, 512], F32) for i in range(2)]
            n_t = 0; n_g = 0; n_y = 0; n_ys = 0
            def gathers(b):
                for (src, stg) in zip((io['w_exp_gate'], io['w_exp_up'], io['w_exp_down']), wst2[b % 2]):
                    S.dma('pool', (lambda e, src=src, stg=stg, b=b: e.indirect_dma_start(
                        out=stg.t[:, :], out_offset=None, in_=src[:, :],
                        in_offset=bass.IndirectOffsetOnAxis(ap=bei.t[:, b:b + 1], axis=0))), reads=[bei], writes=[stg])

            gathers(0)
            for b in range(NB):
                wg_, wu_, wd_ = wgb[b % 2], wub[b % 2], wdb[b % 2]
                wst = wst2[b % 2]
                if b + 1 < NB:
                    gathers(b + 1)
                for (stg, dstb, eng) in ((wst[0], wg_, 'dve'), (wst[1], wu_, 'pool'), (wst[2], wd_, 'act')):
                    flat = dstb.t[:, :, :].rearrange("p c f -> p (c f)")
                    for hf in range(2):
                        S.op(eng, CP(flat[:, hf * 2048:(hf + 1) * 2048], stg.t[:, hf * 2048:(hf + 1) * 2048]), reads=[stg], writes=[dstb] if hf == 0 else [], pwrites=[] if hf == 0 else [dstb])
                for r in range(4):
                    S.dma('sp', DMA(xs[r].t[:, :], XS[b * MB + r * 128: b * MB + (r + 1) * 128, :]), writes=[xs[r]])
                for cp_ in range(4):
                    tpp = tp[n_t % 2]; n_t += 1
                    S.op('pe', [TR(tpp.t[:, ci * 512 + r * 128: ci * 512 + (r + 1) * 128], xs[r].t[:, bass.ds(cp_ * 2 + ci, 128, step=8)], identb.t[:, :])
                                for ci in range(2) for r in range(4)], reads=xs + [identb], writes=[tpp])
                    S.op('dve' if cp_ % 2 == 0 else 'act', CP(xsT.t[:, cp_ * 2:cp_ * 2 + 2, :], tpp.t[:, :].rearrange("p (c t) -> p c t", c=2)), reads=[tpp],
                         writes=[xsT] if cp_ == 0 else [], pwrites=[] if cp_ == 0 else [xsT])
                for j in range(4):
                    g_ = pg[n_g % 2]; u_ = pu[n_g % 2]; n_g += 1
                    S.op('pe', [MM(g_.t[:, :], wg_.t[:, c, bass.ds(j, 128, step=4)], xsT.t[:, c, :], c == 0, c == 7) for c in range(8)], reads=[wg_, xsT], writes=[g_])
                    S.op('pe', [MM(u_.t[:, :], wu_.t[:, c, bass.ds(j, 128, step=4)], xsT.t[:, c, :], c == 0, c == 7) for c in range(8)], reads=[wu_, xsT], writes=[u_])
                    S.op('act', ACTF(sg.t[:, :], g_.t[:, :], AF.Silu), reads=[g_], writes=[sg])
                    S.op('dve', TT(haT.t[:, j, :], u_.t[:, :], sg.t[:, :], ALU.mult), reads=[u_, sg], writes=[haT] if j == 0 else [], pwrites=[] if j == 0 else [haT])
                for r in range(4):
                    yb = ysb[n_ys % 2]; n_ys += 1
                    for half in range(2):
                        y_ = py[n_y % 2]; n_y += 1
                        S.op('pe', [MM(y_.t[:, :], haT.t[:, j, r * 128:(r + 1) * 128], wd_.t[:, j, half * 512:(half + 1) * 512], j == 0, j == 3) for j in range(4)],
                             reads=[haT, wd_], writes=[y_])
                        S.op('act' if half == 0 else 'dve', CP(yb.t[:, half * 512:(half + 1) * 512], y_.t[:, :]), reads=[y_], writes=[yb] if half == 0 else [], pwrites=[] if half == 0 else [yb])
                    S.dma('act', DMA(YS[b * MB + r * 128: b * MB + (r + 1) * 128, :], yb.t[:, :]), reads=[yb])
            S.end_phase()

        with contextlib.ExitStack() as es:
            sb, ps = mk(es)
            S.begin_phase('p6')
            rtf = sb('rtf', [128, 64, 8], F32)
            S.dma('sp', DMA(rtf.t[:, :, :], RT.rearrange("(t p) k -> p t k", p=128)), writes=[rtf])
            dsti = sb('dsti', [128, 64, 2], I32)
            S.op('dve', CP(dsti.t[:, :, :], rtf.t[:, :, 0:2]), reads=[rtf], writes=[dsti])
            gfin = sb('gfin', [128, D], F32)
            S.dma('sp', DMA(gfin.t[:, :], io['g_final'].partition_broadcast(128)), writes=[gfin])
            eps_t = sb('eps_t', [128, 1], F32)
            S.op('dve', MSET(eps_t.t[:, :], 1e-6), writes=[eps_t])
            xin = [sb(f'xin{i}', [128, D], F32) for i in range(2)]
            y1 = [sb(f'y1_{i}', [128, D], BF16) for i in range(2)]
            y2 = [sb(f'y2_{i}', [128, D], BF16) for i in range(2)]
            xa = sb('xa', [128, D], F32)
            junk = sb('junk', [128, D], BF16)
            ss = sb('ss', [128, 1], F32); rstd = sb('rstd', [128, 1], F32)
            ob = [sb(f'ob{i}', [128, D], F32) for i in range(2)]
            for ti in range(64):
                xb = xin[ti % 2]; ya_ = y1[ti % 2]; yb_ = y2[ti % 2]; o_ = ob[ti % 2]
                S.dma('sp', DMA(xb.t[:, :], X2[ti * 128:(ti + 1) * 128, :]), writes=[xb])
                for k, yy in enumerate((ya_, yb_)):
                    S.dma('pool', (lambda e, yy=yy, ti=ti, k=k: e.indirect_dma_start(
                        out=yy.t[:, :], out_offset=None, in_=YS[:, :],
                        in_offset=bass.IndirectOffsetOnAxis(ap=dsti.t[:, ti, k:k + 1], axis=0))), reads=[dsti], writes=[yy])
                S.op('dve', STT(xa.t[:, :], ya_.t[:, :], rtf.t[:, ti, 2:3], xb.t[:, :], ALU.mult, ALU.add), reads=[ya_, rtf, xb], writes=[xa])
                S.op('dve', STT(xa.t[:, :], yb_.t[:, :], rtf.t[:, ti, 3:4], xa.t[:, :], ALU.mult, ALU.add), reads=[yb_, rtf, xa], writes=[xa])
                rms_tok(xa, gfin, o_, junk, ss, rstd, eps_t)
                S.dma('act', DMA(out[ti * 128:(ti + 1) * 128, :], o_.t[:, :]), reads=[o_])
            S.end_phase()
        print("bass ops recorded:", S.nops)
    return nc


_CACHE = {}


def _consts():
    bf = ml_dtypes.bfloat16
    s = np.arange(128)[:, None]
    t = np.arange(128)[None, :]
    half = 32
    inv_freq = (10000.0 ** (-np.arange(half, dtype=np.float32) * 2.0 / 64)).astype(np.float32)
    return {
        'c_ident_bf': np.eye(128, dtype=np.float32).astype(bf),
        'c_ident_f': np.eye(128, dtype=np.float32),
        'c_tri': (s < t).astype(np.float32),
        'c_mask': np.where(s <= t, 0.0, -30000.0).astype(np.float32).astype(bf),
        'c_invf': np.concatenate([inv_freq, inv_freq]).reshape(64, 1).astype(np.float32),
        'c_bstart': np.broadcast_to((np.arange(NB, dtype=np.float32) * MB)[None, :], (128, NB)).copy(),
        'c_piota': np.arange(128, dtype=np.float32).reshape(128, 1),
    }


def kernel(**inputs):
    if 'nc' not in _CACHE:
        _CACHE['nc'] = build_program()
    nc = _CACHE['nc']
    a = {k: np.asarray(v) for k, v in inputs.items()}
    shared = {
        'g_mix': a['g_mix'][0], 'w_in': a['w_in'][0], 'b_fgate': a['b_fgate'][0],
        'w_branch_a': a['w_branch_a'][0], 'w_branch_b': a['w_branch_b'][0], 'w_out': a['w_out'][0],
        'lambda_q1': a['lambda_q1'][0], 'lambda_k1': a['lambda_k1'][0], 'lambda_q2': a['lambda_q2'][0], 'lambda_k2': a['lambda_k2'][0],
        'g_diff_sub': a['g_diff_sub'][0], 'g_cross': a['g_cross'][0], 'g_mem': a['g_mem'][0],
        'w_cq': a['w_cq'][0], 'w_ckv': a['w_ckv'][0], 'w_co': a['w_co'][0], 'g_ffn': a['g_ffn'][0],
        'w_group': a['w_group'][0], 'b_group': a['b_group'][0], 'w_expert': a['w_expert'][0], 'b_expert': a['b_expert'][0],
        'w_exp_gate': a['w_exp_gate'][0].reshape(NEXP * 128, 4096),
        'w_exp_up': a['w_exp_up'][0].reshape(NEXP * 128, 4096),
        'w_exp_down': a['w_exp_down'][0].reshape(NEXP * 128, 4096),
        'g_final': a['g_final'],
    }
    shared = {k: np.ascontiguousarray(v) for k, v in shared.items()}
    shared.update(_consts())
    in_maps = []
    for c in range(NCORES):
        m = dict(shared)
        m['x'] = np.ascontiguousarray(a['x'][c * NSEQ:(c + 1) * NSEQ].reshape(NT, D))
        m['mem'] = np.ascontiguousarray(a['mem'][c * NSEQ:(c + 1) * NSEQ].reshape(NSEQ * 256, D))
        m['positions'] = np.ascontiguousarray(a['positions'][c * NSEQ:(c + 1) * NSEQ].astype(np.int32))
        in_maps.append(m)
    res = run_bass_kernel_spmd(nc, in_maps, core_ids=list(range(NCORES)))
    outs = [np.asarray(r['out']).reshape(NSEQ, SEQ, D) for r in res.results]
    return np.concatenate(outs, axis=0).astype(np.float32)
```

```python
import contextlib
import math
import numpy as np
import ml_dtypes
import concourse.bass as bass
import concourse.mybir as mybir
from concourse.bass_utils import run_bass_kernel_spmd

F32 = mybir.dt.float32
BF16 = mybir.dt.bfloat16
I32 = mybir.dt.int32
ALU = mybir.AluOpType
AF = mybir.ActivationFunctionType
AX = mybir.AxisListType

NCORES = 8
SEQ = 4096
D = 1024
NSEQ = 2
NT = NSEQ * SEQ
INW = 5128
NEXP = 32
MB = 512
NB = NT * 2 // MB + NEXP
NROWS = NB * MB
C_FQ, C_FK, C_FV, C_FL, C_DQ, C_DK, C_DV, C_GA, C_GB = 0, 512, 1024, 1536, 1544, 2056, 2568, 3080, 4104
LAMBDA_INIT = 0.8 - 0.6 * math.exp(0.0)
NDS = 8
DEBUG_OUT = False
MAX_PHASE = 99


class Buf:
    def __init__(self, t):
        self.t = t
        self.w = {}
        self.pw = {}
        self.r = {}


class Sched:
    ENG = ['pe', 'dve', 'act', 'pool', 'sp']

    def __init__(self, nc, es):
        self.nc = nc
        self.es = es
        self.dma_pool = {q: [es.enter_context(nc.semaphore(f"dq_{q}_{i}")) for i in range(NDS)] for q in ('sp', 'pool', 'act')}
        self.dma_uses = {}
        for q in self.dma_pool:
            for s in self.dma_pool[q]:
                self.dma_uses[id(s)] = 0
        self.semobj = {}
        for q in self.dma_pool:
            for s in self.dma_pool[q]:
                self.semobj[id(s)] = s
        self.dma_rr = {'sp': 0, 'pool': 0, 'act': 0}
        self.nops = 0
        self.phase_idx = 0

    def begin_phase(self, name):
        self.phase_idx += 1
        self.esem = {}
        for e in self.ENG:
            s = self.es.enter_context(self.nc.semaphore(f"{name}_{e}"))
            self.esem[e] = s
            self.semobj[id(s)] = s
        self.cnt = {e: 0 for e in self.ENG}
        self.ops = {e: [] for e in self.ENG}
        self.seen = {e: {} for e in self.ENG}

    def _waits(self, eng, toks):
        out = []
        for (s, v) in toks:
            if v <= 0:
                continue
            if self.seen[eng].get(s, 0) >= v:
                continue
            self.seen[eng][s] = v
            out.append((s, v))
        return out

    def _deps(self, reads, writes, pwrites):
        toks = []
        for b in reads:
            toks.extend(b.w.items())
            toks.extend(b.pw.items())
        for b in writes:
            toks.extend(b.w.items())
            toks.extend(b.pw.items())
            toks.extend(b.r.items())
        for b in pwrites:
            toks.extend(b.w.items())
            toks.extend(b.r.items())
        return toks

    def _commit(self, sid, val, reads, writes, pwrites):
        for b in reads:
            b.r[sid] = val
        for b in writes:
            b.w = {sid: val}
            b.pw = {}
            b.r = {}
        for b in pwrites:
            b.pw[sid] = val

    def op(self, eng, fns, reads=(), writes=(), pwrites=()):
        own = id(self.esem[eng])
        toks = self._deps(reads, writes, pwrites)
        if eng == 'pe':
            toks = [t for t in toks if t[0] != own]
        waits = self._waits(eng, toks)
        self.cnt[eng] += 1
        if callable(fns):
            fns = [fns]
        self.ops[eng].append((waits, fns, own, 1))
        self.nops += len(fns) + len(waits)
        self._commit(own, self.cnt[eng], reads, writes, pwrites)

    def dma(self, q, fn, reads=(), writes=(), pwrites=()):
        pool = self.dma_pool[q]
        i = self.dma_rr[q]
        self.dma_rr[q] = (i + 1) % len(pool)
        s = id(pool[i])
        toks = self._deps(reads, writes, pwrites) + [(s, 16 * self.dma_uses[s])]
        waits = self._waits(q, toks)
        self.dma_uses[s] += 1
        self.ops[q].append((waits, [fn], s, 16))
        self.nops += 1 + len(waits)
        self._commit(s, 16 * self.dma_uses[s], reads, writes, pwrites)

    def end_phase(self):
        final = [(id(self.esem[e]), self.cnt[e]) for e in self.ENG]
        final += [(s, 16 * u) for s, u in self.dma_uses.items()]
        for e in self.ENG:
            waits = self._waits(e, final)
            self.ops[e].append((waits, [], None, 0))
        if self.phase_idx > MAX_PHASE:
            return
        semobj = self.semobj
        ops = self.ops

        def replay(h, lst):
            for waits, fns, sem, inc in lst:
                for (s, v) in waits:
                    h.wait_ge(semobj[s], v)
                for k, fn in enumerate(fns):
                    ins = fn(h)
                    if k == len(fns) - 1 and sem is not None:
                        ins.then_inc(semobj[sem], inc)

        with self.nc.Block() as block:
            @block.tensor
            def _(h):
                replay(h, ops['pe'])

            @block.vector
            def _(h):
                replay(h, ops['dve'])

            @block.scalar
            def _(h):
                replay(h, ops['act'])

            @block.gpsimd
            def _(h):
                replay(h, ops['pool'])

            @block.sync
            def _(h):
                replay(h, ops['sp'])


def MM(out, lhsT, rhs, start=True, stop=True):
    return lambda e: e.matmul(out, lhsT=lhsT, rhs=rhs, start=start, stop=stop)


def TR(out, in_, ident):
    return lambda e: e.transpose(out, in_, ident)


def ACTF(out, in_, func, **kw):
    return lambda e: e.activation(out=out, in_=in_, func=func, **kw)


def TT(out, in0, in1, op):
    return lambda e: e.tensor_tensor(out=out, in0=in0, in1=in1, op=op)


def TS(out, in0, s1, s2, op0, op1=None):
    if op1 is None:
        return lambda e: e.tensor_scalar(out=out, in0=in0, scalar1=s1, scalar2=None, op0=op0)
    return lambda e: e.tensor_scalar(out=out, in0=in0, scalar1=s1, scalar2=s2, op0=op0, op1=op1)


def STT(out, in0, scalar, in1, op0, op1):
    return lambda e: e.scalar_tensor_tensor(out=out, in0=in0, scalar=scalar, in1=in1, op0=op0, op1=op1)


def CP(out, in_):
    return lambda e: (e.tensor_copy(out=out, in_=in_) if hasattr(e, 'tensor_copy') else e.activation(out=out, in_=in_, func=AF.Copy))


def RCP(out, in_):
    return lambda e: e.reciprocal(out=out, in_=in_)


def MSET(ap, c):
    return lambda e: e.memset(ap, c)


def DMA(out, in_):
    return lambda e: e.dma_start(out=out, in_=in_)


def TTR(out, in0, in1, accum):
    return lambda e: e.scalar_tensor_tensor(out=out, in0=in0, scalar=1.0, in1=in1, op0=ALU.mult, op1=ALU.mult, accum_out=accum)


def build_program():
    nc = bass.Bass("TRN2", target_bir_lowering=False)
    io = {}

    def din(name, shape, dt=F32):
        io[name] = nc.dram_tensor(name, shape, dt, kind="ExternalInput").ap()

    din('x', [NT, D]); din('mem', [NSEQ * 256, D]); din('positions', [NSEQ, SEQ], I32)
    din('g_mix', [D]); din('w_in', [D, INW]); din('b_fgate', [8])
    din('w_branch_a', [512, D]); din('w_branch_b', [512, D]); din('w_out', [D, D])
    for n in ('lambda_q1', 'lambda_k1', 'lambda_q2', 'lambda_k2'):
        din(n, [64])
    din('g_diff_sub', [128]); din('g_cross', [D]); din('g_mem', [D])
    din('w_cq', [D, 256]); din('w_ckv', [D, 512]); din('w_co', [256, D]); din('g_ffn', [D])
    din('w_group', [D, 4]); din('b_group', [4]); din('w_expert', [D, 32]); din('b_expert', [32])
    din('w_exp_gate', [NEXP * 128, 4096]); din('w_exp_up', [NEXP * 128, 4096]); din('w_exp_down', [NEXP * 128, 4096])
    din('g_final', [D])
    din('c_ident_bf', [128, 128], BF16); din('c_ident_f', [128, 128]); din('c_tri', [128, 128]); din('c_mask', [128, 128], BF16)
    din('c_invf', [64, 1]); din('c_bstart', [128, NB]); din('c_piota', [128, 1])
    out = nc.dram_tensor('out', [NT, D], F32, kind="ExternalOutput").ap()

    def scr(name, shape, dt):
        return nc.dram_tensor(name, shape, dt, kind=("ExternalOutput" if DEBUG_OUT else "Internal")).ap()

    FQ = scr('FQ', [8, 70, NT], BF16); FK = scr('FK', [8, 70, NT], BF16)
    FV = scr('FV', [NT, 512], BF16); DV = scr('DV', [NT, 512], BF16)
    DQ = scr('DQ', [8, 64, NT], BF16); DK = scr('DK', [8, 64, NT], BF16)
    GA = scr('GA', [D, NT], BF16); GB = scr('GB', [D, NT], BF16)
    YA = scr('YA', [512, NT], BF16); YB = scr('YB', [512, NT], BF16)
    HM = scr('HM', [NT, D], BF16); X2 = scr('X2', [NT, D], F32)
    XS = scr('XS', [NROWS, D], BF16); YS = scr('YS', [NROWS, D], BF16)
    RT = scr('RT', [NT, 8], F32)

    with contextlib.ExitStack() as top:
        S = Sched(nc, top)

        pfx_ctr = [0]

        def mk(es):
            pfx_ctr[0] += 1
            pfx = f"ph{pfx_ctr[0]}_"

            def sb(n, shp, dt):
                return Buf(es.enter_context(nc.sbuf_tensor(pfx + n, shp, dt)))

            def ps(n, shp, dt):
                return Buf(es.enter_context(nc.psum_tensor(pfx + n, shp, dt)))
            return sb, ps

        def rms_tok(xb, gb, hb, junk, ss, rstd, eps_t):
            S.op('dve', TTR(junk.t[:, :], xb.t[:, :], xb.t[:, :], ss.t[:, 0:1]), reads=[xb], writes=[junk, ss])
            S.op('act', ACTF(rstd.t[:, 0:1], ss.t[:, 0:1], AF.Ln, scale=1.0 / D, bias=eps_t.t[:, 0:1]), reads=[ss, eps_t], writes=[rstd])
            S.op('act', ACTF(rstd.t[:, 0:1], rstd.t[:, 0:1], AF.Exp, scale=-0.5), reads=[rstd], writes=[rstd])
            S.op('dve', STT(hb.t[:, :], xb.t[:, :], rstd.t[:, 0:1], gb.t[:, :], ALU.mult, ALU.mult), reads=[xb, rstd, gb], writes=[hb])

        with contextlib.ExitStack() as es:
            sb, ps = mk(es)
            S.begin_phase('p1')
            w = [sb(f'w_in{c}', [128, INW], BF16) for c in range(8)]
            for c in range(8):
                S.dma('pool', DMA(w[c].t[:, :], io['w_in'][c * 128:(c + 1) * 128, :]), writes=[w[c]])
            gmix = sb('gmix', [128, D], F32)
            S.dma('sp', DMA(gmix.t[:, :], io['g_mix'].partition_broadcast(128)), writes=[gmix])
            bfg = sb('bfg', [128, 8], F32)
            S.dma('sp', DMA(bfg.t[:, :], io['b_fgate'].partition_broadcast(128)), writes=[bfg])
            identb = sb('identb', [128, 128], BF16)
            S.dma('sp', DMA(identb.t[:, :], io['c_ident_bf']), writes=[identb])
            identf = sb('identf', [128, 128], F32)
            S.dma('sp', DMA(identf.t[:, :], io['c_ident_f']), writes=[identf])
            invf = sb('invf', [64, 1], F32)
            S.dma('sp', DMA(invf.t[:, :], io['c_invf']), writes=[invf])
            eps_t = sb('eps_t', [128, 1], F32)
            S.op('dve', MSET(eps_t.t[:, :], 1e-6), writes=[eps_t])
            one_t = sb('one_t', [128, 1], F32)
            S.op('dve', MSET(one_t.t[:, :], 1.0), writes=[one_t])
            posi = sb('posi', [64, 512], I32)
            ang = sb('ang', [64, 512], F32)
            tk = sb('tk', [64, 512], I32)
            tf = sb('tf', [64, 512], F32)
            cosT = sb('cosT', [64, 512], F32)
            sinT = sb('sinT', [64, 512], F32)
            logfT = sb('logfT', [8, SEQ], F32)
            cc = sb('cc', [8, 1024], F32)
            carry = sb('carry', [8, 1], F32)
            onesf8 = sb('onesf8', [8, 1024], F32)
            S.op('pool', MSET(onesf8.t[:, :], 1.0), writes=[onesf8])
            onesb8 = sb('onesb8', [8, 1024], BF16)
            S.op('pool', MSET(onesb8.t[:, :], 1.0), writes=[onesb8])
            cparts = [sb(f'cp{i}', [8, 1024], BF16) for i in range(3)]
            nparts = [sb(f'np{i}', [8, 1024], BF16) for i in range(3)]
            cr = sb('cr', [8, 1024], F32)
            c8 = sb('c8', [8, 1024], F32)
            xt = [sb(f'xt{i}', [128, D], F32) for i in range(2)]
            junk = sb('junk', [128, D], BF16)
            ss = sb('ss', [128, 1], F32)
            rstd = sb('rstd', [128, 1], F32)
            hb = [sb(f'hb{i}', [128, D], BF16) for i in range(2)]
            hT = [sb(f'hT{i}', [128, 8, 512], BF16) for i in range(2)]
            tp = [ps(f'tp{i}', [128, D], BF16) for i in range(2)]
            pt = [ps(f'pt{i}', [128, 512], F32) for i in range(2)]
            pf = [ps(f'pf{i}', [128, 512], F32) for i in range(2)]
            psm = ps('psm', [128, 512], F32)
            vsb = [sb(f'vsb{i}', [128, 512], BF16) for i in range(2)]
            lf = sb('lf', [128, 8], F32)
            lf2 = sb('lf2', [128, 8], F32)
            fqs = [sb(f'fqs{i}', [64, 8, 512], BF16) for i in range(2)]
            gsb = [sb('gsb0', [128, 8, 512], BF16)] * 2
            ra = sb('ra', [64, 512], F32)
            rb = sb('rb', [64, 512], F32)

            def sincos(dst, shift):
                S.op('dve', TS(tf.t[:, :], ang.t[:, :], 1.0 / (2 * math.pi), 0.5 + shift, ALU.mult, ALU.add), reads=[ang], writes=[tf])
                S.op('dve', CP(tk.t[:, :], tf.t[:, :]), reads=[tf], writes=[tk])
                S.op('dve', CP(dst.t[:, :], tk.t[:, :]), reads=[tk], writes=[dst])
                S.op('dve', TT(tf.t[:, :], tf.t[:, :], dst.t[:, :], ALU.subtract), reads=[tf, dst], writes=[tf])
                S.op('dve', TS(dst.t[:, :], tf.t[:, :], 0.0, None, ALU.is_lt), reads=[tf], writes=[dst])
                S.op('dve', TT(tf.t[:, :], tf.t[:, :], dst.t[:, :], ALU.add), reads=[tf, dst], writes=[tf])
                S.op('dve', TS(tf.t[:, :], tf.t[:, :], -0.5, 2 * math.pi, ALU.add, ALU.mult), reads=[tf], writes=[tf])
                S.op('dve', TS(tf.t[:, :], tf.t[:, :], -3.14159, 3.14159, ALU.max, ALU.min), reads=[tf], writes=[tf])
                S.op('act', ACTF(dst.t[:, :], tf.t[:, :], AF.Sin), reads=[tf], writes=[dst])

            cosT2 = [cosT, sb('cosTb', [64, 512], F32)]
            sinT2 = [sinT, sb('sinTb', [64, 512], F32)]
            NST = SEQ // 512
            sts = [(seq, st) for seq in range(NSEQ) for st in range(NST)]

            def a_parts(gi):
                seq, st = sts[gi]
                T0 = seq * SEQ + st * 512
                hp = hT[gi % 2]
                cT = cosT2[gi % 2]; sT = sinT2[gi % 2]
                parts = []

                def p_sincos():
                    S.dma('sp', DMA(posi.t[:, :], io['positions'][seq, st * 512:(st + 1) * 512].partition_broadcast(64)), writes=[posi])
                    S.op('dve', CP(ang.t[:, :], posi.t[:, :]), reads=[posi], writes=[ang])
                    S.op('dve', TS(ang.t[:, :], ang.t[:, :], invf.t[:, 0:1], None, ALU.mult), reads=[ang, invf], writes=[ang])
                    sincos(sT, 0.0)
                    sincos(cT, 0.25)
                    S.op('dve', TS(sT.t[0:32, :], sT.t[0:32, :], -1.0, None, ALU.mult), reads=[sT], writes=[sT])
                parts.append(p_sincos)
                for sub in range(4):
                    it = gi * 4 + sub
                    xb = xt[it % 2]; hbb = hb[it % 2]; tpp = tp[it % 2]
                    r0 = T0 + sub * 128

                    def p_main(sub=sub, xb=xb, hbb=hbb, tpp=tpp, r0=r0):
                        S.dma('sp', DMA(xb.t[:, :], io['x'][r0:r0 + 128, :]), writes=[xb])
                        rms_tok(xb, gmix, hbb, junk, ss, rstd, eps_t)
                        S.op('pe', [TR(tpp.t[:, c * 128:(c + 1) * 128], hbb.t[:, c * 128:(c + 1) * 128], identb.t[:, :]) for c in range(8)],
                             reads=[hbb, identb], writes=[tpp])
                        S.op('act', CP(hp.t[:, :, sub * 128:(sub + 1) * 128], tpp.t[:, :].rearrange("p (c t) -> p c t", c=8)), reads=[tpp], pwrites=[hp])
                        for k, (col, dst) in enumerate(((C_FV, FV), (C_DV, DV))):
                            pp = pt[k]; vb = vsb[k]
                            S.op('pe', [MM(pp.t[:, :], hp.t[:, c, sub * 128:(sub + 1) * 128], w[c].t[:, col:col + 512], c == 0, c == 7) for c in range(8)],
                                 reads=[hp] + w, writes=[pp])
                            S.op('act', CP(vb.t[:, :], pp.t[:, :]), reads=[pp], writes=[vb])
                            S.dma('pool', DMA(dst[r0:r0 + 128, :], vb.t[:, :]), reads=[vb])
                        S.op('pe', [MM(psm.t[:, 0:8], hp.t[:, c, sub * 128:(sub + 1) * 128], w[c].t[:, C_FL:C_FL + 8], c == 0, c == 7) for c in range(8)],
                             reads=[hp] + w, writes=[psm])
                        S.op('dve', TT(lf.t[:, :], psm.t[:, 0:8], bfg.t[:, :], ALU.add), reads=[psm, bfg], writes=[lf])
                        S.op('act', ACTF(lf.t[:, :], lf.t[:, :], AF.Exp, scale=-1.0), reads=[lf], writes=[lf])
                        S.op('act', ACTF(lf.t[:, :], lf.t[:, :], AF.Ln, bias=one_t.t[:, 0:1]), reads=[lf, one_t], writes=[lf])
                        S.op('dve', TS(lf2.t[:, :], lf.t[:, :], -1.0, None, ALU.mult), reads=[lf], writes=[lf2])

                    def p_tail(sub=sub):
                        S.op('pe', TR(psm.t[0:8, 128:256], lf2.t[:, :], identf.t[:, :]), reads=[lf2, identf], writes=[psm])
                        S.op('dve', CP(logfT.t[:, st * 512 + sub * 128: st * 512 + (sub + 1) * 128], psm.t[0:8, 128:256]), reads=[psm], pwrites=[logfT])
                    parts.append(p_main)
                    parts.append(p_tail)
                return parts

            def b_parts(gi):
                seq, st = sts[gi]
                T0 = seq * SEQ + st * 512
                hall = hT[gi % 2]
                cT = cosT2[gi % 2]; sT = sinT2[gi % 2]
                parts = []
                fi = [0]

                def mm_group(pp, rows, col, width):
                    S.op('pe', [MM(pp.t[0:rows, :], w[c].t[:, col: col + width], hall.t[:, c, :], c == 0, c == 7) for c in range(8)],
                         reads=[hall] + w, writes=[pp])

                for k, (col, dst) in enumerate(((C_FQ, FQ), (C_FK, FK))):
                    fb = fqs[k]
                    for hh in range(8):
                        def g(hh=hh, fb=fb, col=col, dst=dst):
                            pp = pf[fi[0] % 2]; fi[0] += 1
                            mm_group(pp, 64, col + hh * 64, 64)
                            S.op('act', CP(fb.t[:, hh, :], pp.t[0:64, :]), reads=[pp], pwrites=[fb])
                            if hh == 7:
                                S.dma('pool', DMA(dst[:, 0:64, T0:T0 + 512].rearrange("h r t -> r h t"), fb.t[:, :, :]), reads=[fb])
                        parts.append(g)
                for k, (col, dst) in enumerate(((C_DQ, DQ), (C_DK, DK))):
                    fb = fqs[k]
                    for j in range(8):
                        def g(j=j, fb=fb, col=col, dst=dst):
                            pp = pf[fi[0] % 2]; fi[0] += 1
                            mm_group(pp, 64, col + j * 64, 64)
                            S.op('dve', TT(ra.t[0:64, :], pp.t[0:64, :], cT.t[0:64, :], ALU.mult), reads=[pp, cT], writes=[ra])
                            S.op('dve', TT(rb.t[0:32, :], pp.t[32:64, :], sT.t[0:32, :], ALU.mult), reads=[pp, sT], pwrites=[rb])
                            S.op('dve', TT(rb.t[32:64, :], pp.t[0:32, :], sT.t[32:64, :], ALU.mult), reads=[pp, sT], pwrites=[rb])
                            S.op('pool', TT(fb.t[0:64, j, :], ra.t[0:64, :], rb.t[0:64, :], ALU.add), reads=[ra, rb], pwrites=[fb])
                            if j == 7:
                                S.dma('pool', DMA(dst[:, :, T0:T0 + 512].rearrange("h r t -> r h t"), fb.t[:, :, :]), reads=[fb])
                        parts.append(g)
                for k, (col, dst) in enumerate(((C_GA, GA), (C_GB, GB))):
                    gb_ = gsb[k]
                    for n in range(8):
                        def g(n=n, gb_=gb_, col=col, dst=dst):
                            pp = pf[fi[0] % 2]; fi[0] += 1
                            mm_group(pp, 128, col + n * 128, 128)
                            S.op('act', ACTF(gb_.t[:, n, :], pp.t[:, :], AF.Sigmoid), reads=[pp], pwrites=[gb_])
                            if n == 7:
                                S.dma('pool', DMA(dst[:, T0:T0 + 512].rearrange("(n p) t -> p n t", p=128), gb_.t[:, :, :]), reads=[gb_])
                        parts.append(g)
                return parts

            def scan_seq(seq):
                for ch in range(4):
                    lsl = slice(ch * 1024, (ch + 1) * 1024)
                    init = 0.0 if ch == 0 else carry.t[:, 0:1]
                    S.op('dve', (lambda e, lsl=lsl, init=init: e.tensor_tensor_scan(out=cc.t[:, :], data0=onesf8.t[:, :], data1=logfT.t[:, lsl], initial=init, op0=ALU.mult, op1=ALU.add)),
                         reads=[onesf8, logfT, carry], writes=[cc])
                    S.op('dve', CP(carry.t[:, 0:1], cc.t[:, 1023:1024]), reads=[cc], writes=[carry])
                    S.op('dve', TS(c8.t[:, :], cc.t[:, :], 8.0, None, ALU.mult), reads=[cc], writes=[c8])
                    src = c8
                    for i in range(3):
                        S.op('dve', CP(cparts[i].t[:, :], src.t[:, :]), reads=[src], writes=[cparts[i]])
                        S.op('dve', TS(nparts[i].t[:, :], cparts[i].t[:, :], -1.0, None, ALU.mult), reads=[cparts[i]], writes=[nparts[i]])
                        if i < 2:
                            S.op('dve', TT(cr.t[:, :], src.t[:, :], cparts[i].t[:, :], ALU.subtract), reads=[src, cparts[i]], writes=[cr])
                            src = cr
                    sl = slice(seq * SEQ + ch * 1024, seq * SEQ + (ch + 1) * 1024)
                    for i in range(3):
                        S.dma('pool', DMA(FQ[:, 64 + i, sl], cparts[i].t[:, :]), reads=[cparts[i]])
                        S.dma('pool', DMA(FQ[:, 67 + i, sl], onesb8.t[:, :]), reads=[onesb8])
                        S.dma('pool', DMA(FK[:, 64 + i, sl], onesb8.t[:, :]), reads=[onesb8])
                        S.dma('pool', DMA(FK[:, 67 + i, sl], nparts[i].t[:, :]), reads=[nparts[i]])

            for p in a_parts(0):
                p()
            for gi in range(len(sts)):
                bl = b_parts(gi)
                seq, st = sts[gi]
                al = a_parts(gi + 1) if gi + 1 < len(sts) else []
                if st == NST - 1:
                    bl[0](); bl[1]()
                    scan_seq(seq)
                    bl = bl[2:]
                step = max(1, len(bl) // max(1, len(al))) if al else len(bl)
                ai = 0
                for bi, g in enumerate(bl):
                    g()
                    if al and (bi + 1) % step == 0 and ai < len(al):
                        al[ai](); ai += 1
                while ai < len(al):
                    al[ai](); ai += 1
            S.end_phase()

        with contextlib.ExitStack() as es:
            sb, ps = mk(es)
            S.begin_phase('p2a')
            identb = sb('identb', [128, 128], BF16)
            S.dma('sp', DMA(identb.t[:, :], io['c_ident_bf']), writes=[identb])
            maskb = sb('maskb', [128, 128], BF16)
            S.dma('sp', DMA(maskb.t[:, :], io['c_mask']), writes=[maskb])
            vaug = sb('vaug', [128, 32, 8, 128], BF16)
            S.op('pool', MSET(vaug.t[:, :, :, :], 1.0), writes=[vaug])
            qT = [sb(f'qT{i}', [70, SEQ], BF16) for i in range(2)]
            kT = [sb(f'kT{i}', [70, SEQ], BF16) for i in range(2)]
            sp_ = [ps(f'sp{i}', [128, 512], F32) for i in range(3)]
            op_ = [ps(f'op{i}', [128, 512], F32) for i in range(2)]
            pT = [sb(f'pT{i}', [128, 512], BF16) for i in range(4)]
            rec = sb('rec', [128, 512], F32)
            yab = [sb(f'yab{i}', [64, 512], BF16) for i in range(2)]
            LOOK = 2
            items = []
            n_o = 0
            heads = [(seq, h) for seq in range(NSEQ) for h in range(8)]
            for hi, (seq, h) in enumerate(heads):
                for j in range(8):
                    last = 4 * j + 3
                    for i in range(last + 1):
                        items.append(dict(hi=hi, seq=seq, h=h, j=j, i=i, last=last, ob=n_o % 2, first=(j == 0 and i == 0)))
                    n_o += 1

            def load_v(seq):
                tb = seq * SEQ
                for t in range(32):
                    S.dma('sp', DMA(vaug.t[:, t, :, 0:64], FV[tb + t * 128: tb + (t + 1) * 128, :].rearrange("s (h d) -> s h d", d=64)),
                          pwrites=[vaug])

            def load_head(hi):
                seq, h = heads[hi]
                tb = seq * SEQ
                q = qT[hi % 2]; k_ = kT[hi % 2]
                S.dma('sp', DMA(q.t[:, :], FQ[h, :, tb:tb + SEQ]), writes=[q])
                S.dma('sp', DMA(k_.t[:, :], FK[h, :, tb:tb + SEQ]), writes=[k_])

            def emit_s(n):
                it = items[n]
                if it['first']:
                    if it['hi'] == 0:
                        load_v(0)
                        load_head(0)
                    if it['hi'] + 1 < len(heads):
                        load_head(it['hi'] + 1)
                q = qT[it['hi'] % 2]; k_ = kT[it['hi'] % 2]
                i, j = it['i'], it['j']
                off = max(0, i - 4 * j) * 128
                diag = i >= 4 * j
                spb = sp_[n % len(sp_)]
                fns = [MM(spb.t[:, off:512], k_.t[0:70, i * 128:(i + 1) * 128], q.t[0:70, j * 512 + off:(j + 1) * 512], True, not diag)]
                if diag:
                    fns.append(MM(spb.t[:, off:off + 128], identb.t[:, :], maskb.t[:, :], False, True))
                S.op('pe', fns, reads=[k_, q, identb, maskb], writes=[spb])

            def emit_rest(n):
                it = items[n]
                i, j, h, seq = it['i'], it['j'], it['h'], it['seq']
                tb = seq * SEQ
                off = max(0, i - 4 * j) * 128
                spb = sp_[n % len(sp_)]; pb = pT[n % len(pT)]; ob = op_[it['ob']]
                S.op('act', ACTF(pb.t[:, off:512], spb.t[:, off:512], AF.Exp, scale=0.125), reads=[spb], writes=[pb])
                S.op('pe', MM(ob.t[:, off:512], vaug.t[:, i, h, :], pb.t[:, off:512], i == 0, i == it['last']),
                     reads=[vaug, pb], writes=[ob] if i == 0 else [], pwrites=[] if i == 0 else [ob])
                if i == it['last']:
                    yb = yab[it['ob']]
                    S.op('dve', RCP(rec.t[64:128, :], ob.t[64:128, :]), reads=[ob], writes=[rec])
                    S.op('dve', TT(yb.t[0:64, :], ob.t[0:64, :], rec.t[64:128, :], ALU.mult), reads=[ob, rec], writes=[yb])
                    S.dma('pool', DMA(YA[h * 64:(h + 1) * 64, tb + j * 512: tb + (j + 1) * 512], yb.t[:, :]), reads=[yb])
                    if j == 7 and h == 7 and it['hi'] + 1 < len(heads):
                        load_v(seq + 1)

            for n in range(min(LOOK, len(items))):
                emit_s(n)
            for n in range(len(items)):
                if n + LOOK < len(items):
                    nxt = items[n + LOOK]
                    emit_s(n + LOOK)
                emit_rest(n)
            S.end_phase()

        with contextlib.ExitStack() as es:
            sb, ps = mk(es)
            S.begin_phase('p2b')
            identb = sb('identb', [128, 128], BF16)
            S.dma('sp', DMA(identb.t[:, :], io['c_ident_bf']), writes=[identb])
            maskb = sb('maskb', [128, 128], BF16)
            S.dma('sp', DMA(maskb.t[:, :], io['c_mask']), writes=[maskb])
            onesb = sb('onesb', [128, 128], BF16)
            S.op('pool', MSET(onesb.t[:, :], 1.0), writes=[onesb])
            lqa = sb('lqa', [128, 64], F32); lka = sb('lka', [128, 64], F32); lj = sb('lj', [128, 64], F32)
            l1 = sb('l1', [128, 1], F32); l2 = sb('l2', [128, 1], F32); nlam = sb('nlam', [128, 1], F32)
            for (qa, ka, dst) in (('lambda_q1', 'lambda_k1', l1), ('lambda_q2', 'lambda_k2', l2)):
                S.dma('sp', DMA(lqa.t[:, :], io[qa].partition_broadcast(128)), writes=[lqa])
                S.dma('sp', DMA(lka.t[:, :], io[ka].partition_broadcast(128)), writes=[lka])
                S.op('dve', TTR(lj.t[:, :], lqa.t[:, :], lka.t[:, :], dst.t[:, 0:1]), reads=[lqa, lka], writes=[lj, dst])
                S.op('act', ACTF(dst.t[:, 0:1], dst.t[:, 0:1], AF.Exp), reads=[dst], writes=[dst])
            S.op('dve', TT(nlam.t[:, :], l2.t[:, :], l1.t[:, :], ALU.subtract), reads=[l1, l2], writes=[nlam])
            S.op('dve', TS(nlam.t[:, :], nlam.t[:, :], -LAMBDA_INIT, None, ALU.add), reads=[nlam], writes=[nlam])
            gsub = sb('gsub', [128, 1], F32)
            S.dma('sp', DMA(gsub.t[:, :], io['g_diff_sub'].rearrange("(p o) -> p o", o=1)), writes=[gsub])
            S.op('dve', TS(gsub.t[:, :], gsub.t[:, :], 1.0 - LAMBDA_INIT, None, ALU.mult), reads=[gsub], writes=[gsub])
            eps5 = sb('eps5', [128, 1], F32)
            S.op('dve', MSET(eps5.t[:, :], 1e-5), writes=[eps5])
            dvt = sb('dvt', [128, 32, 512], BF16)
            accP = [sb(f'accP{i}', [128, 512], F32) for i in range(2)]
            onesf = sb('onesf', [128, 128], F32)
            S.op('pool', MSET(onesf.t[:, :], 1.0), writes=[onesf])
            qk = [[sb(f'qk{i}_{m}', [64, SEQ], BF16) for m in range(4)] for i in range(2)]
            sp_ = [ps(f'sp{i}', [128, 512], F32) for i in range(3)]
            OD = [ps(f'od{i}', [128, 512], F32) for i in range(4)]
            pss = ps('pss', [128, 512], F32)
            pT = [sb(f'pT{i}', [128, 512], BF16) for i in range(4)]
            r1 = sb('r1', [128, 512], F32); a1 = sb('a1', [128, 512], F32); a2 = sb('a2', [128, 512], F32)
            sq = sb('sq', [128, 512], BF16); rs = sb('rs', [128, 512], F32)
            ybb = [sb(f'ybb{i}', [128, 512], BF16) for i in range(2)]
            LOOK = 2
            items = []
            heads = [(seq, hd) for seq in range(NSEQ) for hd in range(4)]
            for hi, (seq, hd) in enumerate(heads):
                for j in range(8):
                    last = 4 * j + 3
                    for i in range(last + 1):
                        for comp in range(2):
                            items.append(dict(hi=hi, seq=seq, hd=hd, j=j, i=i, comp=comp, last=last, first=(j == 0 and i == 0 and comp == 0)))
            n_y = [0]

            def load_v(seq):
                tb = seq * SEQ
                for t4 in range(4):
                    S.dma('sp', DMA(dvt.t[:, t4 * 8:(t4 + 1) * 8, :], DV[tb + t4 * 1024: tb + (t4 + 1) * 1024, :].rearrange("(t s) d -> s t d", s=128)),
                          pwrites=[dvt])

            def load_head(hi):
                seq, hd = heads[hi]
                tb = seq * SEQ
                q1, q2, k1, k2 = qk[hi % 2]
                S.dma('sp', DMA(q1.t[:, :], DQ[hd * 2, :, tb:tb + SEQ]), writes=[q1])
                S.dma('sp', DMA(q2.t[:, :], DQ[hd * 2 + 1, :, tb:tb + SEQ]), writes=[q2])
                S.dma('sp', DMA(k1.t[:, :], DK[hd * 2, :, tb:tb + SEQ]), writes=[k1])
                S.dma('sp', DMA(k2.t[:, :], DK[hd * 2 + 1, :, tb:tb + SEQ]), writes=[k2])

            def emit_s(n):
                it = items[n]
                if it['first']:
                    if it['hi'] == 0:
                        load_v(0)
                        load_head(0)
                    if it['hi'] + 1 < len(heads):
                        load_head(it['hi'] + 1)
                q1, q2, k1, k2 = qk[it['hi'] % 2]
                qq, kk = (q1, k1) if it['comp'] == 0 else (q2, k2)
                i, j = it['i'], it['j']
                off = max(0, i - 4 * j) * 128
                diag = i >= 4 * j
                spb = sp_[n % len(sp_)]
                fns = [MM(spb.t[:, off:512], kk.t[0:64, i * 128:(i + 1) * 128], qq.t[0:64, j * 512 + off:(j + 1) * 512], True, not diag)]
                if diag:
                    fns.append(MM(spb.t[:, off:off + 128], identb.t[:, :], maskb.t[:, :], False, True))
                S.op('pe', fns, reads=[kk, qq, identb, maskb], writes=[spb])

            def emit_rest(n):
                it = items[n]
                i, j, hd, seq, comp = it['i'], it['j'], it['hd'], it['seq'], it['comp']
                tb = seq * SEQ
                off = max(0, i - 4 * j) * 128
                spb = sp_[n % len(sp_)]; pb = pT[n % len(pT)]
                S.op('act', ACTF(pb.t[:, off:512], spb.t[:, off:512], AF.Exp, scale=0.125), reads=[spb], writes=[pb])
                ob = OD[comp * 2]; db = OD[comp * 2 + 1]
                acc = accP[comp]
                eng = 'dve' if comp == 0 else 'pool'
                if i == 0:
                    S.op(eng, CP(acc.t[:, :], pb.t[:, :]), reads=[pb], writes=[acc])
                else:
                    S.op(eng, TT(acc.t[:, off:512], acc.t[:, off:512], pb.t[:, off:512], ALU.add), reads=[pb, acc], writes=[acc])
                S.op('pe', MM(ob.t[:, off:512], dvt.t[:, i, hd * 128:(hd + 1) * 128], pb.t[:, off:512], i == 0, i == it['last']),
                     reads=[dvt, pb], writes=[ob] if i == 0 else [], pwrites=[] if i == 0 else [ob])
                if i == it['last']:
                    S.op('pe', MM(db.t[:, :], onesf.t[:, :], acc.t[:, :], True, True), reads=[onesf, acc], writes=[db])
                if i == it['last'] and comp == 1:
                    S.op('dve', RCP(r1.t[:, :], OD[1].t[:, :]), reads=[OD[1]], writes=[r1])
                    S.op('dve', TT(a1.t[:, :], OD[0].t[:, :], r1.t[:, :], ALU.mult), reads=[OD[0], r1], writes=[a1])
                    S.op('dve', RCP(r1.t[:, :], OD[3].t[:, :]), reads=[OD[3]], writes=[r1])
                    S.op('dve', TT(a2.t[:, :], OD[2].t[:, :], r1.t[:, :], ALU.mult), reads=[OD[2], r1], writes=[a2])
                    S.op('dve', STT(a1.t[:, :], a2.t[:, :], nlam.t[:, 0:1], a1.t[:, :], ALU.mult, ALU.add), reads=[a2, nlam, a1], writes=[a1])
                    S.op('act', ACTF(sq.t[:, :], a1.t[:, :], AF.Square), reads=[a1], writes=[sq])
                    S.op('pe', MM(pss.t[:, :], onesb.t[:, :], sq.t[:, :], True, True), reads=[onesb, sq], writes=[pss])
                    S.op('act', ACTF(rs.t[:, :], pss.t[:, :], AF.Ln, scale=1.0 / 128, bias=eps5.t[:, 0:1]), reads=[pss, eps5], writes=[rs])
                    S.op('act', ACTF(rs.t[:, :], rs.t[:, :], AF.Exp, scale=-0.5), reads=[rs], writes=[rs])
                    yb = ybb[n_y[0] % 2]; n_y[0] += 1
                    S.op('dve', STT(yb.t[:, :], a1.t[:, :], gsub.t[:, 0:1], rs.t[:, :], ALU.mult, ALU.mult), reads=[a1, gsub, rs], writes=[yb])
                    S.dma('pool', DMA(YB[hd * 128:(hd + 1) * 128, tb + j * 512: tb + (j + 1) * 512], yb.t[:, :]), reads=[yb])
                    if j == 7 and hd == 3 and it['hi'] + 1 < len(heads):
                        load_v(seq + 1)

            for n in range(min(LOOK, len(items))):
                emit_s(n)
            for n in range(len(items)):
                if n + LOOK < len(items):
                    emit_s(n + LOOK)
                emit_rest(n)
            S.end_phase()

        with contextlib.ExitStack() as es:
            sb, ps = mk(es)
            S.begin_phase('p3')
            identb = sb('identb', [128, 128], BF16)
            S.dma('sp', DMA(identb.t[:, :], io['c_ident_bf']), writes=[identb])
            identf = sb('identf', [128, 128], F32)
            S.dma('sp', DMA(identf.t[:, :], io['c_ident_f']), writes=[identf])
            trif = sb('trif', [128, 128], F32)
            S.dma('sp', DMA(trif.t[:, :], io['c_tri']), writes=[trif])
            onesf = sb('onesf', [128, 128], F32)
            S.op('pool', MSET(onesf.t[:, :], 1.0), writes=[onesf])
            eps_t = sb('eps_t', [128, 1], F32)
            S.op('dve', MSET(eps_t.t[:, :], 1e-6), writes=[eps_t])
            wa = sb('wa', [128, 4, D], BF16); wb = sb('wb', [128, 4, D], BF16); wo = sb('wo', [128, 8, D], BF16)
            wcq = sb('wcq', [128, 8, 256], BF16); wckv = sb('wckv', [128, 8, 512], BF16); wco = sb('wco', [64, 4, D], BF16)
            S.dma('pool', DMA(wa.t[:, :, :], io['w_branch_a'].rearrange("(c p) n -> p c n", p=128)), writes=[wa])
            S.dma('pool', DMA(wb.t[:, :, :], io['w_branch_b'].rearrange("(c p) n -> p c n", p=128)), writes=[wb])
            S.dma('pool', DMA(wo.t[:, :, :], io['w_out'].rearrange("(c p) n -> p c n", p=128)), writes=[wo])
            S.dma('pool', DMA(wcq.t[:, :, :], io['w_cq'].rearrange("(c p) n -> p c n", p=128)), writes=[wcq])
            S.dma('pool', DMA(wckv.t[:, :, :], io['w_ckv'].rearrange("(c p) n -> p c n", p=128)), writes=[wckv])
            S.dma('pool', DMA(wco.t[:, :, :], io['w_co'].rearrange("(h d) n -> d h n", d=64)), writes=[wco])
            wr = sb('wr', [128, 8, 36], F32)
            S.dma('sp', DMA(wr.t[:, :, 0:4], io['w_group'].rearrange("(c p) n -> p c n", p=128)), pwrites=[wr])
            S.dma('sp', DMA(wr.t[:, :, 4:36], io['w_expert'].rearrange("(c p) n -> p c n", p=128)), pwrites=[wr])
            brt = sb('brt', [128, 36], F32)
            S.dma('sp', DMA(brt.t[:, 0:4], io['b_group'].partition_broadcast(128)), pwrites=[brt])
            S.dma('sp', DMA(brt.t[:, 4:36], io['b_expert'].partition_broadcast(128)), pwrites=[brt])
            gcross = sb('gcross', [128, D], F32); gmem = sb('gmem', [128, D], F32); gffn = sb('gffn', [128, D], F32)
            S.dma('sp', DMA(gcross.t[:, :], io['g_cross'].partition_broadcast(128)), writes=[gcross])
            S.dma('sp', DMA(gmem.t[:, :], io['g_mem'].partition_broadcast(128)), writes=[gmem])
            S.dma('sp', DMA(gffn.t[:, :], io['g_ffn'].partition_broadcast(128)), writes=[gffn])
            kcT = sb('kcT', [64, NSEQ, 4, 256], BF16)
            vca = sb('vca', [128, NSEQ, 2, 4, 128], BF16)
            S.op('pool', MSET(vca.t[:, :, :, :, :], 1.0), writes=[vca])
            xt = [sb(f'xt{i}', [128, D], F32) for i in range(2)]
            x1 = sb('x1', [128, D], F32)
            x2 = [sb(f'x2_{i}', [128, D], F32) for i in range(2)]
            junk = sb('junk', [128, D], BF16)
            ss = sb('ss', [128, 1], F32); rstd = sb('rstd', [128, 1], F32)
            hxb = sb('hxb', [128, D], BF16)
            hmf = sb('hmf', [128, D], F32)
            hmb = [sb(f'hmb{i}', [128, D], BF16) for i in range(2)]
            hxT = [sb(f'hxT{s}', [128, 8, 128], BF16) for s in range(4)]
            hmT = sb('hmT', [128, 8, 128], F32)
            yaT = sb('yaT', [128, 4, 512], BF16); ybT = sb('ybT', [128, 4, 512], BF16)
            gaT = sb('gaT', [128, 8, 512], BF16); gbT = sb('gbT', [128, 8, 512], BF16)
            mT = sb('mT', [128, 8, 512], BF16)
            t1 = sb('t1', [128, 512], F32); t2 = sb('t2', [128, 512], F32)
            qcs = sb('qcs', [64, 4, 512], BF16)
            ycs = sb('ycs', [64, 4, 512], BF16)
            pT = [sb(f'pT{i}', [128, 512], BF16) for i in range(2)]
            rec = sb('rec', [128, 512], F32)
            pA = ps('pA', [128, 512], F32); pB = ps('pB', [128, 512], F32)
            pt = [ps(f'pt{i}', [128, 512], F32) for i in range(2)]
            tp = ps('tp', [128, D], BF16)
            pq = ps('pq', [128, 512], F32)
            spc = ps('spc', [128, 512], F32)
            opc = ps('opc', [128, 512], F32)
            lg = sb('lg', [128, 36], F32)
            gmax = sb('gmax', [128, 1], F32); gmask = sb('gmask', [128, 4], F32); ge = sb('ge', [128, 4], F32)
            gs = sb('gs', [128, 1], F32); gw = sb('gw', [128, 1], F32)
            sel = sb('sel', [128, 8], F32); top8 = sb('top8', [128, 8], F32)
            m1 = sb('m1', [128, 8], F32); m2 = sb('m2', [128, 8], F32)
            dw = sb('dw', [128, 1], F32)
            oh1 = sb('oh1', [128, 64, 32], F32); oh2 = sb('oh2', [128, 64, 32], F32)
            ohs = sb('ohs', [128, 32], F32); cum = sb('cum', [128, 32], F32); rk = sb('rk', [128, 32], F32)
            S.op('pool', MSET(cum.t[:, :], 0.0), writes=[cum])
            pos = sb('pos', [128, 64, 2], F32); wts = sb('wts', [128, 64, 2], F32)
            j32 = sb('j32', [128, 32], F32)

            for seq in range(NSEQ):
                for mt in range(2):
                    xb = xt[mt]
                    S.dma('sp', DMA(xb.t[:, :], io['mem'][seq * 256 + mt * 128: seq * 256 + (mt + 1) * 128, :]), writes=[xb])
                    rms_tok(xb, gmem, hxb, junk, ss, rstd, eps_t)
                    S.op('pe', [TR(tp.t[:, c * 128:(c + 1) * 128], hxb.t[:, c * 128:(c + 1) * 128], identb.t[:, :]) for c in range(8)],
                         reads=[hxb, identb], writes=[tp])
                    S.op('act', CP(hxT[mt].t[:, :, :], tp.t[:, :].rearrange("p (c t) -> p c t", c=8)), reads=[tp], writes=[hxT[mt]])
                    S.op('pe', [MM(pt[0].t[:, 0:256], hxT[mt].t[:, c, :], wckv.t[:, c, 256:512], c == 0, c == 7) for c in range(8)],
                         reads=[hxT[mt], wckv], writes=[pt[0]])
                    S.op('act', CP(vca.t[:, seq, mt, :, 0:64], pt[0].t[:, 0:256].rearrange("p (h d) -> p h d", d=64)), reads=[pt[0]], pwrites=[vca])
                for hh in range(4):
                    for mt in range(2):
                        S.op('pe', [MM(pq.t[0:64, mt * 128:(mt + 1) * 128], wckv.t[:, c, hh * 64:(hh + 1) * 64], hxT[mt].t[:, c, :], c == 0, c == 7) for c in range(8)],
                             reads=[hxT[mt], wckv], pwrites=[pq])
                    S.op('act', CP(kcT.t[:, seq, hh, :], pq.t[0:64, 0:256]), reads=[pq], pwrites=[kcT])

            it = 0
            for seq in range(NSEQ):
                for st in range(SEQ // 512):
                    T0 = seq * SEQ + st * 512
                    S.dma('sp', DMA(yaT.t[:, :, :], YA[:, T0:T0 + 512].rearrange("(c p) t -> p c t", p=128)), writes=[yaT])
                    S.dma('sp', DMA(ybT.t[:, :, :], YB[:, T0:T0 + 512].rearrange("(c p) t -> p c t", p=128)), writes=[ybT])
                    S.dma('sp', DMA(gaT.t[:, :, :], GA[:, T0:T0 + 512].rearrange("(c p) t -> p c t", p=128)), writes=[gaT])
                    S.dma('sp', DMA(gbT.t[:, :, :], GB[:, T0:T0 + 512].rearrange("(c p) t -> p c t", p=128)), writes=[gbT])
                    for n in range(8):
                        S.op('pe', [MM(pA.t[:, :], wa.t[:, c, n * 128:(n + 1) * 128], yaT.t[:, c, :], c == 0, c == 3) for c in range(4)],
                             reads=[wa, yaT], writes=[pA])
                        S.op('pe', [MM(pB.t[:, :], wb.t[:, c, n * 128:(n + 1) * 128], ybT.t[:, c, :], c == 0, c == 3) for c in range(4)],
                             reads=[wb, ybT], writes=[pB])
                        S.op('dve', TT(t1.t[:, :], pA.t[:, :], gaT.t[:, n, :], ALU.mult), reads=[pA, gaT], writes=[t1])
                        S.op('dve', TT(t2.t[:, :], pB.t[:, :], gbT.t[:, n, :], ALU.mult), reads=[pB, gbT], writes=[t2])
                        S.op('pool', TT(mT.t[:, n, :], t1.t[:, :], t2.t[:, :], ALU.add), reads=[t1, t2], pwrites=[mT])
                    for sub in range(4):
                        r0 = T0 + sub * 128
                        xb = xt[it % 2]
                        S.dma('sp', DMA(xb.t[:, :], io['x'][r0:r0 + 128, :]), writes=[xb])
                        for half in range(2):
                            pp = pt[half]
                            S.op('pe', [MM(pp.t[:, :], mT.t[:, n, sub * 128:(sub + 1) * 128], wo.t[:, n, half * 512:(half + 1) * 512], n == 0, n == 7) for n in range(8)],
                                 reads=[mT, wo], writes=[pp])
                            S.op('dve', TT(x1.t[:, half * 512:(half + 1) * 512], pp.t[:, :], xb.t[:, half * 512:(half + 1) * 512], ALU.add),
                                 reads=[pp, xb], pwrites=[x1])
                        rms_tok(x1, gcross, hxb, junk, ss, rstd, eps_t)
                        S.op('pe', [TR(tp.t[:, c * 128:(c + 1) * 128], hxb.t[:, c * 128:(c + 1) * 128], identb.t[:, :]) for c in range(8)],
                             reads=[hxb, identb], writes=[tp])
                        S.op('act', CP(hxT[sub].t[:, :, :], tp.t[:, :].rearrange("p (c t) -> p c t", c=8)), reads=[tp], writes=[hxT[sub]])
                        for hh in range(4):
                            S.op('pe', [MM(pq.t[0:64, hh * 128:(hh + 1) * 128], wcq.t[:, c, hh * 64:(hh + 1) * 64], hxT[sub].t[:, c, :], c == 0, c == 7) for c in range(8)],
                                 reads=[hxT[sub], wcq], pwrites=[pq])
                        S.op('act', CP(qcs.t[:, :, 0:128], pq.t[0:64, :].rearrange("p (h t) -> p h t", h=4)), reads=[pq], writes=[qcs])
                        for hh in range(4):
                            for mt in range(2):
                                pb = pT[mt]
                                S.op('pe', MM(spc.t[:, mt * 128:(mt + 1) * 128], kcT.t[0:64, seq, hh, mt * 128:(mt + 1) * 128], qcs.t[0:64, hh, 0:128], True, True),
                                     reads=[kcT, qcs], writes=[spc] if mt == 0 else [], pwrites=[] if mt == 0 else [spc])
                            S.op('act', ACTF(pT[0].t[:, 0:256], spc.t[:, 0:256], AF.Exp, scale=0.125), reads=[spc], writes=[pT[0]])
                            S.op('pe', [MM(opc.t[:, hh * 128:(hh + 1) * 128], vca.t[:, seq, mt, hh, :], pT[0].t[:, mt * 128:(mt + 1) * 128], mt == 0, mt == 1) for mt in range(2)],
                                 reads=[vca, pT[0]], pwrites=[opc])
                        S.op('dve', RCP(rec.t[64:128, :], opc.t[64:128, :]), reads=[opc], writes=[rec])
                        S.op('dve', TT(ycs.t[0:64, :, 0:128], opc.t[0:64, :].rearrange("p (h t) -> p h t", h=4),
                                       rec.t[64:128, :].rearrange("p (h t) -> p h t", h=4), ALU.mult), reads=[opc, rec], writes=[ycs])
                        xo = x2[it % 2]
                        for half in range(2):
                            pp = pt[half]
                            S.op('pe', [MM(pp.t[:, :], ycs.t[0:64, hh, 0:128], wco.t[0:64, hh, half * 512:(half + 1) * 512], hh == 0, hh == 3) for hh in range(4)],
                                 reads=[ycs, wco], writes=[pp])
                            S.op('dve', TT(xo.t[:, half * 512:(half + 1) * 512], pp.t[:, :], x1.t[:, half * 512:(half + 1) * 512], ALU.add),
                                 reads=[pp, x1], pwrites=[xo])
                        S.dma('pool', DMA(X2[r0:r0 + 128, :], xo.t[:, :]), reads=[xo])
                        rms_tok(xo, gffn, hmf, junk, ss, rstd, eps_t)
                        hb_ = hmb[it % 2]
                        S.op('act', CP(hb_.t[:, :], hmf.t[:, :]), reads=[hmf], writes=[hb_])
                        S.dma('pool', DMA(HM[r0:r0 + 128, :], hb_.t[:, :]), reads=[hb_])
                        for half in range(2):
                            pp = pt[half]
                            S.op('pe', [TR(pp.t[:, cq * 128:(cq + 1) * 128], hmf.t[:, (half * 4 + cq) * 128:(half * 4 + cq + 1) * 128], identf.t[:, :]) for cq in range(4)],
                                 reads=[hmf, identf], writes=[pp])
                            S.op('act', CP(hmT.t[:, half * 4:(half + 1) * 4, :], pp.t[:, :].rearrange("p (c t) -> p c t", c=4)), reads=[pp], pwrites=[hmT])
                        S.op('pe', [MM(pq.t[:, 0:36], hmT.t[:, c, :], wr.t[:, c, :], c == 0, c == 7) for c in range(8)], reads=[hmT, wr], writes=[pq])
                        S.op('dve', TT(lg.t[:, :], pq.t[:, 0:36], brt.t[:, :], ALU.add), reads=[pq, brt], writes=[lg])
                        ti = it
                        S.op('dve', lambda e: e.tensor_reduce(out=gmax.t[:, 0:1], in_=lg.t[:, 0:4], axis=AX.X, op=ALU.max), reads=[lg], writes=[gmax])
                        S.op('dve', TS(gmask.t[:, :], lg.t[:, 0:4], gmax.t[:, 0:1], None, ALU.is_equal), reads=[lg, gmax], writes=[gmask])
                        S.op('dve', TS(ge.t[:, :], lg.t[:, 0:4], gmax.t[:, 0:1], None, ALU.subtract), reads=[lg, gmax], writes=[ge])
                        S.op('act', ACTF(ge.t[:, :], ge.t[:, :], AF.Exp), reads=[ge], writes=[ge])
                        S.op('dve', lambda e: e.tensor_reduce(out=gs.t[:, 0:1], in_=ge.t[:, :], axis=AX.X, op=ALU.add), reads=[ge], writes=[gs])
                        S.op('dve', RCP(gw.t[:, :], gs.t[:, :]), reads=[gs], writes=[gw])
                        S.op('dve', TS(sel.t[:, :], lg.t[:, 4:12], gmask.t[:, 0:1], None, ALU.mult), reads=[lg, gmask], writes=[sel])
                        for g in range(1, 4):
                            S.op('dve', STT(sel.t[:, :], lg.t[:, 4 + g * 8: 12 + g * 8], gmask.t[:, g:g + 1], sel.t[:, :], ALU.mult, ALU.add),
                                 reads=[lg, gmask, sel], writes=[sel])
                        S.op('dve', lambda e: e.max(out=top8.t[:, :], in_=sel.t[:, :]), reads=[sel], writes=[top8])
                        S.op('dve', TS(m1.t[:, :], sel.t[:, :], top8.t[:, 0:1], None, ALU.is_equal), reads=[sel, top8], writes=[m1])
                        S.op('dve', TS(m2.t[:, :], sel.t[:, :], top8.t[:, 1:2], None, ALU.is_equal), reads=[sel, top8], writes=[m2])
                        S.op('dve', TT(dw.t[:, :], top8.t[:, 1:2], top8.t[:, 0:1], ALU.subtract), reads=[top8], writes=[dw])
                        S.op('act', ACTF(dw.t[:, :], dw.t[:, :], AF.Exp), reads=[dw], writes=[dw])
                        S.op('dve', TS(dw.t[:, :], dw.t[:, :], 1.0, None, ALU.add), reads=[dw], writes=[dw])
                        S.op('dve', RCP(dw.t[:, :], dw.t[:, :]), reads=[dw], writes=[dw])
                        S.op('dve', TT(wts.t[:, ti, 0:1], dw.t[:, :], gw.t[:, :], ALU.mult), reads=[dw, gw], pwrites=[wts])
                        S.op('dve', TT(wts.t[:, ti, 1:2], gw.t[:, :], wts.t[:, ti, 0:1], ALU.subtract), reads=[gw, wts], pwrites=[wts])
                        for (mm_, oh) in ((m1, oh1), (m2, oh2)):
                            S.op('dve', TT(oh.t[:, ti, :].rearrange("p (g e) -> p g e", g=4),
                                           gmask.t[:, :].unsqueeze(2).to_broadcast([128, 4, 8]),
                                           mm_.t[:, :].unsqueeze(1).to_broadcast([128, 4, 8]), ALU.mult), reads=[gmask, mm_], pwrites=[oh])
                        S.op('dve', TT(ohs.t[:, :], oh1.t[:, ti, :], oh2.t[:, ti, :], ALU.add), reads=[oh1, oh2], writes=[ohs])
                        S.op('pe', [MM(spc.t[:, 0:32], trif.t[:, :], ohs.t[:, :], True, False), MM(spc.t[:, 0:32], onesf.t[:, :], cum.t[:, :], False, True)],
                             reads=[trif, ohs, onesf, cum], writes=[spc])
                        S.op('dve', CP(rk.t[:, :], spc.t[:, 0:32]), reads=[spc], writes=[rk])
                        S.op('pool', TT(cum.t[:, :], cum.t[:, :], ohs.t[:, :], ALU.add), reads=[cum, ohs], writes=[cum])
                        S.op('dve', TTR(j32.t[:, :], rk.t[:, :], oh1.t[:, ti, :], pos.t[:, ti, 0:1]), reads=[rk, oh1], writes=[j32], pwrites=[pos])
                        S.op('dve', TTR(j32.t[:, :], rk.t[:, :], oh2.t[:, ti, :], pos.t[:, ti, 1:2]), reads=[rk, oh2], writes=[j32], pwrites=[pos])
                        it += 1

            cnt = sb('cnt', [128, 32], F32); pad = sb('pad', [128, 32], F32); pend = sb('pend', [128, 32], F32); pstart = sb('pstart', [128, 32], F32)
            ki = sb('ki', [128, 32], I32); kf = sb('kf', [128, 32], F32); kc = sb('kc', [128, 32], F32)
            ones32 = sb('ones32', [128, 32], F32)
            S.op('pool', MSET(ones32.t[:, :], 1.0), writes=[ones32])
            S.op('pe', MM(spc.t[:, 0:32], onesf.t[:, :], cum.t[:, :], True, True), reads=[onesf, cum], writes=[spc])
            S.op('dve', CP(cnt.t[:, :], spc.t[:, 0:32]), reads=[spc], writes=[cnt])
            S.op('dve', TS(pad.t[:, :], cnt.t[:, :], 1.0 / MB, None, ALU.mult), reads=[cnt], writes=[pad])
            S.op('dve', CP(ki.t[:, :], pad.t[:, :]), reads=[pad], writes=[ki])
            S.op('dve', CP(kf.t[:, :], ki.t[:, :]), reads=[ki], writes=[kf])
            S.op('dve', TT(kc.t[:, :], kf.t[:, :], pad.t[:, :], ALU.is_lt), reads=[kf, pad], writes=[kc])
            S.op('dve', TT(kf.t[:, :], kf.t[:, :], kc.t[:, :], ALU.add), reads=[kf, kc], writes=[kf])
            S.op('dve', TS(pad.t[:, :], kf.t[:, :], float(MB), None, ALU.mult), reads=[kf], writes=[pad])
            S.op('dve', lambda e: e.tensor_tensor_scan(out=pend.t[:, :], data0=ones32.t[:, :], data1=pad.t[:, :], initial=0.0, op0=ALU.mult, op1=ALU.add),
                 reads=[ones32, pad], writes=[pend])
            S.op('dve', TT(pstart.t[:, :], pend.t[:, :], pad.t[:, :], ALU.subtract), reads=[pend, pad], writes=[pstart])
            dst = sb('dst', [128, 64, 2], F32)
            for ti in range(64):
                for k, oh in enumerate((oh1, oh2)):
                    S.op('dve', TTR(j32.t[:, :], pstart.t[:, :], oh.t[:, ti, :], dst.t[:, ti, k:k + 1]), reads=[pstart, oh], writes=[j32], pwrites=[dst])
            S.op('dve', TT(dst.t[:, :, :], dst.t[:, :, :], pos.t[:, :, :], ALU.add), reads=[dst, pos], writes=[dst])
            rt = sb('rt', [128, 64, 8], F32)
            S.op('pool', MSET(rt.t[:, :, :], 0.0), writes=[rt])
            S.op('dve', CP(rt.t[:, :, 0:2], dst.t[:, :, :]), reads=[dst], pwrites=[rt])
            S.op('dve', CP(rt.t[:, :, 2:4], wts.t[:, :, :]), reads=[wts], pwrites=[rt])
            bst = sb('bst', [128, NB], F32)
            S.dma('sp', DMA(bst.t[:, :], io['c_bstart']), writes=[bst])
            cmpb = sb('cmpb', [128, NB, 32], F32)
            S.op('dve', TT(cmpb.t[:, :, :], pend.t[:, :].unsqueeze(1).to_broadcast([128, NB, 32]),
                           bst.t[:, :].unsqueeze(2).to_broadcast([128, NB, 32]), ALU.is_le), reads=[pend, bst], writes=[cmpb])
            bef = sb('bef', [128, NB], F32)
            S.op('dve', lambda e: e.tensor_reduce(out=bef.t[:, :], in_=cmpb.t[:, :, :], axis=AX.X, op=ALU.add), reads=[cmpb], writes=[bef])
            pio = sb('pio', [128, 1], F32)
            S.dma('sp', DMA(pio.t[:, :], io['c_piota']), writes=[pio])
            S.op('dve', TS(bef.t[:, :], bef.t[:, :], 31.0, 128.0, ALU.min, ALU.mult), reads=[bef], writes=[bef])
            S.op('dve', TS(bef.t[:, :], bef.t[:, :], pio.t[:, 0:1], None, ALU.add), reads=[bef, pio], writes=[bef])
            S.dma('pool', DMA(RT.rearrange("(t p) k -> p t k", p=128), rt.t[:, :, :]), reads=[rt])
            BE = scr('BE', [128, NB], F32)
            S.dma('pool', DMA(BE[:, :], bef.t[:, :]), reads=[bef])
            S.end_phase()

        with contextlib.ExitStack() as es:
            sb, ps = mk(es)
            S.begin_phase('p4')
            rtf = sb('rtf', [128, 64, 8], F32)
            S.dma('sp', DMA(rtf.t[:, :, :], RT.rearrange("(t p) k -> p t k", p=128)), writes=[rtf])
            dsti = sb('dsti', [128, 64, 2], I32)
            S.op('dve', CP(dsti.t[:, :, :], rtf.t[:, :, 0:2]), reads=[rtf], writes=[dsti])
            hm = [sb(f'hm{i}', [128, D], BF16) for i in range(4)]
            for ti in range(64):
                hb_ = hm[ti % 4]
                S.dma('sp', DMA(hb_.t[:, :], HM[ti * 128:(ti + 1) * 128, :]), writes=[hb_])
                for k in range(2):
                    S.dma('pool', (lambda e, hb_=hb_, ti=ti, k=k: e.indirect_dma_start(
                        out=XS[:, :], out_offset=bass.IndirectOffsetOnAxis(ap=dsti.t[:, ti, k:k + 1], axis=0),
                        in_=hb_.t[:, :], in_offset=None)), reads=[hb_, dsti])
            S.end_phase()

        with contextlib.ExitStack() as es:
            sb, ps = mk(es)
            S.begin_phase('p5')
            identb = sb('identb', [128, 128], BF16)
            S.dma('sp', DMA(identb.t[:, :], io['c_ident_bf']), writes=[identb])
            bef = sb('bef', [128, NB], F32)
            S.dma('sp', DMA(bef.t[:, :], BE[:, :]), writes=[bef])
            bei = sb('bei', [128, NB], I32)
            S.op('dve', CP(bei.t[:, :], bef.t[:, :]), reads=[bef], writes=[bei])
            wst2 = [[sb(f'wst{q}_{i}', [128, 4096], F32) for i in range(3)] for q in range(2)]
            wgb = [sb(f'wgb{i}', [128, 8, 512], BF16) for i in range(2)]
            wub = [sb(f'wub{i}', [128, 8, 512], BF16) for i in range(2)]
            wdb = [sb(f'wdb{i}', [128, 4, D], BF16) for i in range(2)]
            xs = [sb(f'xs{i}', [128, D], BF16) for i in range(4)]
            xsT = sb('xsT', [128, 8, 512], BF16)
            haT = sb('haT', [128, 4, 512], BF16)
            sg = sb('sg', [128, 512], F32)
            ysb = [sb(f'ysb{i}', [128, D], BF16) for i in range(2)]
            tp = [ps(f'tp{i}', [128, D], BF16) for i in range(2)]
            pg = [ps(f'pg{i}', [128, 512], F32) for i in range(2)]
            pu = [ps(f'pu{i}', [128, 512], F32) for i in range(2)]
            py = [ps(f'py{i}', [128, 512], F32) for i in range(2)]
            n_t = 0; n_g = 0; n_y = 0; n_ys = 0
            def gathers(b):
                for (src, stg) in zip((io['w_exp_gate'], io['w_exp_up'], io['w_exp_down']), wst2[b % 2]):
                    S.dma('pool', (lambda e, src=src, stg=stg, b=b: e.indirect_dma_start(
                        out=stg.t[:, :], out_offset=None, in_=src[:, :],
                        in_offset=bass.IndirectOffsetOnAxis(ap=bei.t[:, b:b + 1], axis=0))), reads=[bei], writes=[stg])

            gathers(0)
            for b in range(NB):
                wg_, wu_, wd_ = wgb[b % 2], wub[b % 2], wdb[b % 2]
                wst = wst2[b % 2]
                if b + 1 < NB:
                    gathers(b + 1)
                for (stg, dstb, eng) in ((wst[0], wg_, 'dve'), (wst[1], wu_, 'pool'), (wst[2], wd_, 'act')):
                    flat = dstb.t[:, :, :].rearrange("p c f -> p (c f)")
                    for hf in range(2):
                        S.op(eng, CP(flat[:, hf * 2048:(hf + 1) * 2048], stg.t[:, hf * 2048:(hf + 1) * 2048]), reads=[stg], writes=[dstb] if hf == 0 else [], pwrites=[] if hf == 0 else [dstb])
                for r in range(4):
                    S.dma('sp', DMA(xs[r].t[:, :], XS[b * MB + r * 128: b * MB + (r + 1) * 128, :]), writes=[xs[r]])
                for cp_ in range(4):
                    tpp = tp[n_t % 2]; n_t += 1
                    S.op('pe', [TR(tpp.t[:, ci * 512 + r * 128: ci * 512 + (r + 1) * 128], xs[r].t[:, bass.ds(cp_ * 2 + ci, 128, step=8)], identb.t[:, :])
                                for ci in range(2) for r in range(4)], reads=xs + [identb], writes=[tpp])
                    S.op('dve' if cp_ % 2 == 0 else 'act', CP(xsT.t[:, cp_ * 2:cp_ * 2 + 2, :], tpp.t[:, :].rearrange("p (c t) -> p c t", c=2)), reads=[tpp],
                         writes=[xsT] if cp_ == 0 else [], pwrites=[] if cp_ == 0 else [xsT])
                for j in range(4):
                    g_ = pg[n_g % 2]; u_ = pu[n_g % 2]; n_g += 1
                    S.op('pe', [MM(g_.t[:, :], wg_.t[:, c, bass.ds(j, 128, step=4)], xsT.t[:, c, :], c == 0, c == 7) for c in range(8)], reads=[wg_, xsT], writes=[g_])
                    S.op('pe', [MM(u_.t[:, :], wu_.t[:, c, bass.ds(j, 128, step=4)], xsT.t[:, c, :], c == 0, c == 7) for c in range(8)], reads=[wu_, xsT], writes=[u_])
                    S.op('act', ACTF(sg.t[:, :], g_.t[:, :], AF.Silu), reads=[g_], writes=[sg])
                    S.op('dve', TT(haT.t[:, j, :], u_.t[:, :], sg.t[:, :], ALU.mult), reads=[u_, sg], writes=[haT] if j == 0 else [], pwrites=[] if j == 0 else [haT])
                for r in range(4):
                    yb = ysb[n_ys % 2]; n_ys += 1
                    for half in range(2):
                        y_ = py[n_y % 2]; n_y += 1
                        S.op('pe', [MM(y_.t[:, :], haT.t[:, j, r * 128:(r + 1) * 128], wd_.t[:, j, half * 512:(half + 1) * 512], j == 0, j == 3) for j in range(4)],
                             reads=[haT, wd_], writes=[y_])
                        S.op('act' if half == 0 else 'dve', CP(yb.t[:, half * 512:(half + 1) * 512], y_.t[:, :]), reads=[y_], writes=[yb] if half == 0 else [], pwrites=[] if half == 0 else [yb])
                    S.dma('act', DMA(YS[b * MB + r * 128: b * MB + (r + 1) * 128, :], yb.t[:, :]), reads=[yb])
            S.end_phase()

        with contextlib.ExitStack() as es:
            sb, ps = mk(es)
            S.begin_phase('p6')
            rtf = sb('rtf', [128, 64, 8], F32)
            S.dma('sp', DMA(rtf.t[:, :, :], RT.rearrange("(t p) k -> p t k", p=128)), writes=[rtf])
            dsti = sb('dsti', [128, 64, 2], I32)
            S.op('dve', CP(dsti.t[:, :, :], rtf.t[:, :, 0:2]), reads=[rtf], writes=[dsti])
            gfin = sb('gfin', [128, D], F32)
            S.dma('sp', DMA(gfin.t[:, :], io['g_final'].partition_broadcast(128)), writes=[gfin])
            eps_t = sb('eps_t', [128, 1], F32)
            S.op('dve', MSET(eps_t.t[:, :], 1e-6), writes=[eps_t])
            xin = [sb(f'xin{i}', [128, D], F32) for i in range(2)]
            y1 = [sb(f'y1_{i}', [128, D], BF16) for i in range(2)]
            y2 = [sb(f'y2_{i}', [128, D], BF16) for i in range(2)]
            xa = sb('xa', [128, D], F32)
            junk = sb('junk', [128, D], BF16)
            ss = sb('ss', [128, 1], F32); rstd = sb('rstd', [128, 1], F32)
            ob = [sb(f'ob{i}', [128, D], F32) for i in range(2)]
            for ti in range(64):
                xb = xin[ti % 2]; ya_ = y1[ti % 2]; yb_ = y2[ti % 2]; o_ = ob[ti % 2]
                S.dma('sp', DMA(xb.t[:, :], X2[ti * 128:(ti + 1) * 128, :]), writes=[xb])
                for k, yy in enumerate((ya_, yb_)):
                    S.dma('pool', (lambda e, yy=yy, ti=ti, k=k: e.indirect_dma_start(
                        out=yy.t[:, :], out_offset=None, in_=YS[:, :],
                        in_offset=bass.IndirectOffsetOnAxis(ap=dsti.t[:, ti, k:k + 1], axis=0))), reads=[dsti], writes=[yy])
                S.op('dve', STT(xa.t[:, :], ya_.t[:, :], rtf.t[:, ti, 2:3], xb.t[:, :], ALU.mult, ALU.add), reads=[ya_, rtf, xb], writes=[xa])
                S.op('dve', STT(xa.t[:, :], yb_.t[:, :], rtf.t[:, ti, 3:4], xa.t[:, :], ALU.mult, ALU.add), reads=[yb_, rtf, xa], writes=[xa])
                rms_tok(xa, gfin, o_, junk, ss, rstd, eps_t)
                S.dma('act', DMA(out[ti * 128:(ti + 1) * 128, :], o_.t[:, :]), reads=[o_])
            S.end_phase()
        print("bass ops recorded:", S.nops)
    return nc


_CACHE = {}


def _consts():
    bf = ml_dtypes.bfloat16
    s = np.arange(128)[:, None]
    t = np.arange(128)[None, :]
    half = 32
    inv_freq = (10000.0 ** (-np.arange(half, dtype=np.float32) * 2.0 / 64)).astype(np.float32)
    return {
        'c_ident_bf': np.eye(128, dtype=np.float32).astype(bf),
        'c_ident_f': np.eye(128, dtype=np.float32),
        'c_tri': (s < t).astype(np.float32),
        'c_mask': np.where(s <= t, 0.0, -30000.0).astype(np.float32).astype(bf),
        'c_invf': np.concatenate([inv_freq, inv_freq]).reshape(64, 1).astype(np.float32),
        'c_bstart': np.broadcast_to((np.arange(NB, dtype=np.float32) * MB)[None, :], (128, NB)).copy(),
        'c_piota': np.arange(128, dtype=np.float32).reshape(128, 1),
    }


def kernel(**inputs):
    if 'nc' not in _CACHE:
        _CACHE['nc'] = build_program()
    nc = _CACHE['nc']
    a = {k: np.asarray(v) for k, v in inputs.items()}
    shared = {
        'g_mix': a['g_mix'][0], 'w_in': a['w_in'][0], 'b_fgate': a['b_fgate'][0],
        'w_branch_a': a['w_branch_a'][0], 'w_branch_b': a['w_branch_b'][0], 'w_out': a['w_out'][0],
        'lambda_q1': a['lambda_q1'][0], 'lambda_k1': a['lambda_k1'][0], 'lambda_q2': a['lambda_q2'][0], 'lambda_k2': a['lambda_k2'][0],
        'g_diff_sub': a['g_diff_sub'][0], 'g_cross': a['g_cross'][0], 'g_mem': a['g_mem'][0],
        'w_cq': a['w_cq'][0], 'w_ckv': a['w_ckv'][0], 'w_co': a['w_co'][0], 'g_ffn': a['g_ffn'][0],
        'w_group': a['w_group'][0], 'b_group': a['b_group'][0], 'w_expert': a['w_expert'][0], 'b_expert': a['b_expert'][0],
        'w_exp_gate': a['w_exp_gate'][0].reshape(NEXP * 128, 4096),
        'w_exp_up': a['w_exp_up'][0].reshape(NEXP * 128, 4096),
        'w_exp_down': a['w_exp_down'][0].reshape(NEXP * 128, 4096),
        'g_final': a['g_final'],
    }
    shared = {k: np.ascontiguousarray(v) for k, v in shared.items()}
    shared.update(_consts())
    in_maps = []
    for c in range(NCORES):
        m = dict(shared)
        m['x'] = np.ascontiguousarray(a['x'][c * NSEQ:(c + 1) * NSEQ].reshape(NT, D))
        m['mem'] = np.ascontiguousarray(a['mem'][c * NSEQ:(c + 1) * NSEQ].reshape(NSEQ * 256, D))
        m['positions'] = np.ascontiguousarray(a['positions'][c * NSEQ:(c + 1) * NSEQ].astype(np.int32))
        in_maps.append(m)
    res = run_bass_kernel_spmd(nc, in_maps, core_ids=list(range(NCORES)))
    outs = [np.asarray(r['out']).reshape(NSEQ, SEQ, D) for r in res.results]
    return np.concatenate(outs, axis=0).astype(np.float32)
```
